# Optimizing a Trainium2 kernel written in Bass

```python
import math
import jax, jax.numpy as jnp
from jax import lax
import numpy as np

D_MODEL = 2048
BATCH = 4
SEQ = 4096
DEPTH = 2

D_SSM = 1024
SSM_GROUP = 16
N_GROUPS = D_SSM // SSM_GROUP
STATE = 64
DT_MIN = 1e-3
DT_MAX = 1e-1
D_ATTN = 1024
HEAD_DIM = 128
N_HEADS = D_ATTN // HEAD_DIM
MOBA_BLOCK = 256
MOBA_TOPK = 3
Q_CHUNK = 16
D_FF = 5632
CONV_W = 3
EPS = 1e-6
NEG = -1e30
D_IN = D_SSM + 3 * D_ATTN + 2 * D_MODEL

kernel_name = "hybrid_s5_moba_convffn_block"


def rmsnorm(x, g):
    xf = x.astype(jnp.float32)
    y = xf * lax.rsqrt(jnp.mean(xf * xf, axis=-1, keepdims=True) + EPS)
    return (y * g.astype(jnp.float32)).astype(x.dtype)


def s5_mixer(u, lam_re, lam_im, log_dt, b_re, b_im, c_re, c_im, d_skip, w_glu):
    bsz, s, _ = u.shape
    f32 = jnp.float32
    lam = lax.complex(lam_re.astype(f32), lam_im.astype(f32))
    dt = jnp.exp(log_dt.astype(f32))[:, None]
    lam_bar = jnp.exp(lam * dt)
    b = lax.complex(b_re.astype(f32), b_im.astype(f32))
    b_bar = ((lam_bar - 1.0) / lam)[..., None] * b
    c = lax.complex(c_re.astype(f32), c_im.astype(f32))
    ug = u.astype(f32).reshape(bsz, s, N_GROUPS, SSM_GROUP)

    def combine(left, right):
        a_l, h_l = left
        a_r, h_r = right
        return a_r * a_l, a_r * h_l + h_r

    def scan_one(u_seq):
        bu = jnp.einsum('gpm,sgm->sgp', b_bar, u_seq.astype(jnp.complex64))
        a = jnp.broadcast_to(lam_bar, bu.shape)
        _, h = lax.associative_scan(combine, (a, bu), axis=0)
        return jnp.einsum('gmp,sgp->sgm', c, h).real

    y = lax.map(scan_one, ug)
    y = y + d_skip.astype(f32).reshape(N_GROUPS, SSM_GROUP) * ug
    y = jax.nn.gelu(y.reshape(bsz, s, D_SSM))
    y = y * jax.nn.sigmoid(y @ w_glu.astype(f32))
    return y.astype(u.dtype)


def moba_attention(q, k, v):
    f32 = jnp.float32
    bsz, s, nh, dh = q.shape
    s_pad = -(-s // MOBA_BLOCK) * MOBA_BLOCK
    pad = s_pad - s
    q, k, v = [jnp.pad(t, ((0, 0), (0, pad), (0, 0), (0, 0))).transpose(0, 2, 1, 3) for t in (q, k, v)]
    nb = s_pad // MOBA_BLOCK
    topk = min(MOBA_TOPK, nb)
    L = MOBA_BLOCK
    kb = k.reshape(bsz, nh, nb, L, dh)
    vb = v.reshape(bsz, nh, nb, L, dh)

    kmean = jnp.mean(kb.astype(f32), axis=3)
    gate = jnp.einsum('bhsd,bhnd->bhsn', q.astype(f32), kmean)
    qblk = jnp.arange(s_pad) // L
    past = jnp.arange(nb)[None, :] < qblk[:, None]
    gate = jnp.where(past, gate, NEG)
    _, sel = lax.top_k(gate, topk)
    valid = jnp.arange(topk)[None, :] < qblk[:, None]

    nc = s_pad // Q_CHUNK
    scale = 1.0 / math.sqrt(dh)

    def to_chunks(t):
        t = t.reshape(bsz, nh, nc, Q_CHUNK, *t.shape[3:])
        return jnp.moveaxis(t, 2, 0)

    q_c = to_chunks(q)
    sel_c = to_chunks(sel)
    valid_c = valid.reshape(nc, Q_CHUNK, topk)
    b_ix = jnp.arange(bsz)[:, None, None, None]
    h_ix = jnp.arange(nh)[None, :, None, None]

    def attend(args):
        ci, qc, selc, validc = args
        q0 = ci * Q_CHUNK
        qpos = q0 + jnp.arange(Q_CHUNK)
        own = q0 // L
        kg = kb[b_ix, h_ix, selc].astype(f32)
        vg = vb[b_ix, h_ix, selc].astype(f32)
        k_own = lax.dynamic_index_in_dim(kb, own, axis=2, keepdims=False).astype(f32)
        v_own = lax.dynamic_index_in_dim(vb, own, axis=2, keepdims=False).astype(f32)
        qf = qc.astype(f32) * scale
        s_sel = jnp.einsum('bhqd,bhqkld->bhqkl', qf, kg)
        s_sel = jnp.where(validc[None, None, :, :, None], s_sel, NEG)
        s_own = jnp.einsum('bhqd,bhld->bhql', qf, k_own)
        kpos = own * L + jnp.arange(L)
        s_own = jnp.where(kpos[None, :] <= qpos[:, None], s_own, NEG)
        scores = jnp.concatenate([s_sel.reshape(bsz, nh, Q_CHUNK, topk * L), s_own], axis=-1)
        p = jax.nn.softmax(scores, axis=-1)
        p_sel = p[..., :topk * L].reshape(bsz, nh, Q_CHUNK, topk, L)
        p_own = p[..., topk * L:]
        o = jnp.einsum('bhqkl,bhqkld->bhqd', p_sel, vg) + jnp.einsum('bhql,bhld->bhqd', p_own, v_own)
        return o.astype(qc.dtype)

    out = lax.map(attend, (jnp.arange(nc), q_c, sel_c, valid_c))
    out = jnp.moveaxis(out, 0, 2).reshape(bsz, nh, s_pad, dh)[:, :, :s]
    return out.transpose(0, 2, 1, 3).reshape(bsz, s, nh * dh)


def conv_ffn(h, w_up, conv_w, conv_b, w_down):
    z = h @ w_up
    z = lax.conv_general_dilated(
        z, conv_w[:, None, :], window_strides=(1,), padding=[(CONV_W - 1, 0)],
        dimension_numbers=('NWC', 'WIO', 'NWC'), feature_group_count=z.shape[-1]) + conv_b
    a, val = jnp.split(z, 2, axis=-1)
    return (jax.nn.silu(a) * val) @ w_down


def hybrid_layer(x, g_pre_mix, w_in, lam_re, lam_im, log_dt, b_re, b_im, c_re, c_im, d_skip, w_glu,
                 w_up_ssm, w_up_attn, w_out, g_post_mix, g_pre_ffn, w_ffn_up, conv_w, conv_b,
                 w_ffn_down, g_post_ffn):
    bsz, s, _ = x.shape
    h = rmsnorm(x, g_pre_mix)
    proj = h @ w_in
    cuts = [D_SSM, D_SSM + D_ATTN, D_SSM + 2 * D_ATTN, D_SSM + 3 * D_ATTN, D_SSM + 3 * D_ATTN + D_MODEL]
    u, q, k, v, ga, gb = jnp.split(proj, cuts, axis=-1)
    y_ssm = s5_mixer(u, lam_re, lam_im, log_dt, b_re, b_im, c_re, c_im, d_skip, w_glu) @ w_up_ssm
    hd = (bsz, s, N_HEADS, HEAD_DIM)
    y_att = moba_attention(q.reshape(hd), k.reshape(hd), v.reshape(hd)) @ w_up_attn
    m = jax.nn.sigmoid(ga) * y_ssm + jax.nn.sigmoid(gb) * y_att
    x = x + rmsnorm(m @ w_out, g_post_mix)
    f = conv_ffn(rmsnorm(x, g_pre_ffn), w_ffn_up, conv_w, conv_b, w_ffn_down)
    return x + rmsnorm(f, g_post_ffn)


def setup_inputs(seed: int = 0) -> dict:
    key = jax.random.key(seed)
    ks = jax.random.split(key, 24)
    f32 = jnp.float32
    nrm = lambda k, shp, sc: jax.random.normal(k, shp, f32) * sc
    gain = lambda k: 1.0 + 0.05 * jax.random.normal(k, (DEPTH, D_MODEL), f32)
    lam_im = jnp.broadcast_to(jnp.pi * jnp.arange(STATE, dtype=f32), (DEPTH, N_GROUPS, STATE))
    return {
        'x': jax.random.normal(ks[0], (BATCH, SEQ, D_MODEL), f32),
        'g_pre_mix': gain(ks[1]),
        'w_in': nrm(ks[2], (DEPTH, D_MODEL, D_IN), D_MODEL ** -0.5),
        'lam_re': -0.5 + nrm(ks[3], (DEPTH, N_GROUPS, STATE), 0.01),
        'lam_im': lam_im + nrm(ks[4], (DEPTH, N_GROUPS, STATE), 0.01),
        'log_dt': jax.random.uniform(ks[5], (DEPTH, N_GROUPS), f32, math.log(DT_MIN), math.log(DT_MAX)),
        'b_re': nrm(ks[6], (DEPTH, N_GROUPS, STATE, SSM_GROUP), (2 * SSM_GROUP) ** -0.5),
        'b_im': nrm(ks[7], (DEPTH, N_GROUPS, STATE, SSM_GROUP), (2 * SSM_GROUP) ** -0.5),
        'c_re': nrm(ks[8], (DEPTH, N_GROUPS, SSM_GROUP, STATE), (2 * STATE) ** -0.5),
        'c_im': nrm(ks[9], (DEPTH, N_GROUPS, SSM_GROUP, STATE), (2 * STATE) ** -0.5),
        'd_skip': nrm(ks[10], (DEPTH, D_SSM), 1.0),
        'w_glu': nrm(ks[11], (DEPTH, D_SSM, D_SSM), D_SSM ** -0.5),
        'w_up_ssm': nrm(ks[12], (DEPTH, D_SSM, D_MODEL), D_SSM ** -0.5),
        'w_up_attn': nrm(ks[13], (DEPTH, D_ATTN, D_MODEL), D_ATTN ** -0.5),
        'w_out': nrm(ks[14], (DEPTH, D_MODEL, D_MODEL), D_MODEL ** -0.5),
        'g_post_mix': gain(ks[15]),
        'g_pre_ffn': gain(ks[16]),
        'w_ffn_up': nrm(ks[17], (DEPTH, D_MODEL, 2 * D_FF), D_MODEL ** -0.5),
        'conv_w': nrm(ks[18], (DEPTH, CONV_W, 2 * D_FF), CONV_W ** -0.5),
        'conv_b': nrm(ks[19], (DEPTH, 2 * D_FF), 0.01),
        'w_ffn_down': nrm(ks[20], (DEPTH, D_FF, D_MODEL), D_FF ** -0.5),
        'g_post_ffn': gain(ks[21]),
    }


def reference(x, g_pre_mix, w_in, lam_re, lam_im, log_dt, b_re, b_im, c_re, c_im, d_skip, w_glu,
              w_up_ssm, w_up_attn, w_out, g_post_mix, g_pre_ffn, w_ffn_up, conv_w, conv_b,
              w_ffn_down, g_post_ffn):
    for l in range(DEPTH):
        x = hybrid_layer(x, g_pre_mix[l], w_in[l], lam_re[l], lam_im[l], log_dt[l], b_re[l], b_im[l],
                         c_re[l], c_im[l], d_skip[l], w_glu[l], w_up_ssm[l], w_up_attn[l], w_out[l],
                         g_post_mix[l], g_pre_ffn[l], w_ffn_up[l], conv_w[l], conv_b[l],
                         w_ffn_down[l], g_post_ffn[l])
    return x
```

```python
import numpy as np
import ml_dtypes
from contextlib import ExitStack
import concourse.bass as bass
import concourse.mybir as mybir
from concourse.bass_utils import run_bass_kernel_spmd

F32 = mybir.dt.float32
BF16 = mybir.dt.bfloat16
ALU = mybir.AluOpType
AF = mybir.ActivationFunctionType
AX = mybir.AxisListType
ENGS = ['tensor', 'vector', 'scalar', 'gpsimd', 'sync']


class _Op:
    __slots__ = ('eng', 'fn', 'dma', 'deps', 'signal', 'sigval', 'dsem', 'dval', 'pre')

    def __init__(self, eng, fn, dma):
        self.eng = eng
        self.fn = fn
        self.dma = dma
        self.deps = []
        self.signal = False
        self.sigval = 0
        self.dsem = None
        self.dval = 0
        self.pre = None


class Prog:
    def __init__(self, nc, ndma=12):
        self.nc = nc
        self.ops = {e: [] for e in ENGS}
        self.last_w = {}
        self.readers = {}
        self.ndma = ndma
        self.dma_count = {e: 0 for e in ENGS}
        self.es = ExitStack()
        self.scopes = []
        self._uid = 0
        self._par = {}
        self.nbar = 0

    def sb(self, name, shape, dt):
        st = self.scopes[-1] if self.scopes else self.es
        self._uid += 1
        return st.enter_context(self.nc.sbuf_tensor("%s_%d" % (name, self._uid), shape, dt))

    def parity(self, e):
        k = id(e)
        if k not in self._par:
            self._par[k] = e.partition_id() % 2
        return self._par[k]

    def push_scope(self):
        self.scopes.append(ExitStack())

    def pop_scope(self):
        self.barrier()
        self.scopes.pop().close()

    def barrier(self):
        self.nbar += 1
        for e in ENGS:
            o = _Op(e, None, False)
            o.pre = ('bar', self.nbar)
            self.ops[e].append(o)
        self.last_w = {}
        self.readers = {}

    def ps(self, name, shape, dt=F32):
        return self.es.enter_context(self.nc.psum_tensor(name, shape, dt))

    def _add(self, eng, fn, r, w, dma=False):
        o = _Op(eng, fn, dma)
        deps = {}
        for b in r:
            lw = self.last_w.get(b)
            if lw is not None:
                deps[id(lw)] = (lw, True)
        for b in w:
            lw = self.last_w.get(b)
            if lw is not None and id(lw) not in deps:
                deps[id(lw)] = (lw, False)
            rd = self.readers.get(b)
            if rd:
                for x in rd[0].values():
                    if id(x) not in deps:
                        deps[id(x)] = (x, False)
                for x in rd[1]:
                    if id(x) not in deps:
                        deps[id(x)] = (x, False)
        for d, raw in deps.values():
            if d is o:
                continue
            if not d.dma and not dma and d.eng == eng:
                if not raw or eng == 'tensor':
                    continue
            o.deps.append(d)
        for b in r:
            rd = self.readers.setdefault(b, ({}, []))
            if dma:
                rd[1].append(o)
            else:
                rd[0][eng] = o
        for b in w:
            self.last_w[b] = o
            self.readers[b] = ({}, [])
        self.ops[eng].append(o)
        return o

    def add(self, eng, meth, *args, r=(), w=(), **kw):
        return self._add(eng, lambda e: getattr(e, meth)(*args, **kw), r, w)

    def dma(self, out, in_, r=(), w=(), eng='sync', **kw):
        return self._add(eng, lambda e: e.dma_start(out=out, in_=in_, **kw), r, w, dma=True)

    def mm(self, out, lhsT, rhs, start=True, stop=True, r=(), w=(), **kw):
        return self.add('tensor', 'matmul', out, lhsT, rhs, start=start, stop=stop, r=r, w=w, **kw)

    def act(self, out, in_, func, r=(), w=(), **kw):
        return self.add('scalar', 'activation', out, in_, func, r=r, w=w, **kw)

    def emit(self):
        nc = self.nc
        es = self.es
        sem = {e: es.enter_context(nc.semaphore('s_' + e)) for e in ENGS}
        bsem = es.enter_context(nc.semaphore('s_bar'))
        dsems = {}
        for e in ENGS:
            if any(o.dma for o in self.ops[e]):
                dsems[e] = [es.enter_context(nc.semaphore('d_%s_%d' % (e, i))) for i in range(self.ndma)]
        for e in ENGS:
            for o in self.ops[e]:
                for d in o.deps:
                    if not d.dma:
                        d.signal = True
        for e in ENGS:
            c = 0
            j = 0
            for o in self.ops[e]:
                if o.fn is None:
                    continue
                if o.dma:
                    o.dsem = dsems[e][j % self.ndma]
                    o.dval = 16 * (j // self.ndma + 1)
                    o.pre = (o.dsem, o.dval - 16)
                    j += 1
                elif o.signal:
                    c += 1
                    o.sigval = c
        self.sig_counts = {e: sum(1 for o in self.ops[e] if o.signal) for e in ENGS}
        block = es.enter_context(nc.Block())
        all_dma_final = {}
        for e in ENGS:
            for o in self.ops[e]:
                if o.dma:
                    all_dma_final[id(o.dsem)] = (o.dsem, max(o.dval, all_dma_final.get(id(o.dsem), (None, 0))[1]))

        def run(e, engobj):
            waited = {}

            def wait(s, v):
                if v <= 0:
                    return
                if waited.get(id(s), 0) >= v:
                    return
                waited[id(s)] = v
                engobj.wait_ge(s, v)

            mydma = {}
            for o in self.ops[e]:
                if o.fn is None:
                    for s_, v_ in mydma.values():
                        wait(s_, v_)
                    engobj.drain().then_inc(bsem, 1)
                    engobj.wait_ge(bsem, len(ENGS) * o.pre[1])
                    continue
                if o.dma:
                    mydma[id(o.dsem)] = (o.dsem, o.dval)
                for d in o.deps:
                    if d.dma:
                        wait(d.dsem, d.dval)
                    else:
                        wait(sem[d.eng], d.sigval)
                if o.dma:
                    wait(*o.pre)
                    o.fn(engobj).then_inc(o.dsem, 16)
                else:
                    ins = o.fn(engobj)
                    if o.signal:
                        ins.then_inc(sem[e], 1)
            if e == 'sync':
                for s, v in all_dma_final.values():
                    wait(s, v)

        @block.sync
        def _(eng):
            run('sync', eng)

        @block.tensor
        def _(eng):
            run('tensor', eng)

        @block.vector
        def _(eng):
            run('vector', eng)

        @block.scalar
        def _(eng):
            run('scalar', eng)

        @block.gpsimd
        def _(eng):
            run('gpsimd', eng)

        es.close()


D = 2048
T = 4096
NT = 512
S = 4096
EPS = 1e-6
DFF = 5632
NEGM = -30000.0


class Ring:
    def __init__(self, p, name, n, shape, dt):
        self.t = [p.sb("%s%d" % (name, i), shape, dt) for i in range(n)]
        self.k = [(name, i) for i in range(n)]
        self.i = 0

    def next(self):
        j = self.i % len(self.t)
        self.i += 1
        return self.t[j], self.k[j]


class PRing:
    def __init__(self, p, name, n, shape=(128, NT)):
        self.t = [p.ps("%s%d" % (name, i), list(shape)) for i in range(n)]
        self.k = [(name, i) for i in range(n)]
        self.i = 0

    def next(self):
        j = self.i % len(self.t)
        self.i += 1
        return self.t[j], self.k[j]


class SubRing:
    def __init__(self, ring, idx):
        self.t = [ring.t[i] for i in idx]
        self.k = [ring.k[i] for i in idx]
        self.i = 0

    def next(self):
        j = self.i % len(self.t)
        self.i += 1
        return self.t[j], self.k[j]


def wview(t, kc, ncol):
    return t[:, 0:kc * ncol].rearrange("p (k o) -> p k o", o=ncol)


def rms_rstd(p, xs_list, rkeys, sqr, ps_ss, pk, onesf, rstd, n):
    m = len(xs_list)
    for i, xa in enumerate(xs_list):
        sq, sk = sqr.next()
        p.act(sq[:, 0:n], xa, AF.Square, r=rkeys, w=[sk])
        p.mm(ps_ss[:, 0:n], onesf[:], sq[:, 0:n], start=(i == 0), stop=(i == m - 1), r=[sk, 'onesf'], w=[pk])
    rstd_from_ss(p, ps_ss, pk, rstd, n)


def rstd_from_ss(p, ps_ss, pk, rstd, n):
    p.add('vector', 'tensor_scalar', rstd[:, 0:n], ps_ss[:, 0:n], 1.0 / D, EPS, ALU.mult, ALU.add, r=[pk], w=['rstd'])
    p.act(rstd[:, 0:n], rstd[:, 0:n], AF.Sqrt, r=['rstd'], w=['rstd'])
    p.add('vector', 'reciprocal', rstd[:, 0:n], rstd[:, 0:n], r=['rstd'], w=['rstd'])


def emit_A(p, E, l):
    xT = E['xcur']; g = E['g_pre_mix'][l]; w_in = E['w_in'][l]
    uP = E['uP']; qT = E['qT']; kT = E['kT']; v = E['v']; sg = E['sg']
    p.push_scope()
    onesf = p.sb("onesf", [128, 128], F32)
    p.add('vector', 'memset', onesf[:], 1.0, w=['onesf'])
    gt = p.sb("gt", [128, 16], F32)
    p.dma(gt[:], g, w=['gt'])
    xs = p.sb("xs", [128, 16, NT], F32)
    h = p.sb("h", [128, 16, 2048], BF16)
    sqr = Ring(p, "sq", 2, [128, NT], F32)
    rstd = p.sb("rstd", [128, NT], F32)
    wr = Ring(p, "w", 3, [128, 16 * 256], BF16)
    evf = Ring(p, "evf", 3, [128, NT], F32)
    evb = Ring(p, "evb", 3, [128, NT], BF16)
    ubr = Ring(p, "ub", 2, [128, 8, 64], BF16)
    ps_ss = E['pb'].t[7]
    pm = SubRing(E['pb'], [0, 1, 2, 3])
    w3 = w_in.rearrange("(kc p) o -> p kc o", p=128)
    xT3 = xT.rearrange("(kc p) t -> p kc t", p=128)
    tog = [0]

    def evac(out, in_, r, w):
        tog[0] ^= 1
        if tog[0]:
            p.add('vector', 'tensor_copy', out, in_, r=r, w=w)
        else:
            p.add('scalar', 'copy', out, in_, r=r, w=w)

    ST = 2048
    for st in range(T // ST):
        for sub in range(ST // NT):
            t0 = st * ST + sub * NT
            p.dma(xs[:], xT3[:, :, t0:t0 + NT], w=['xs'])
            rms_rstd(p, [xs[:, kc, :] for kc in range(16)], ['xs'], sqr, ps_ss, E['pb'].k[7], onesf, rstd, NT)
            for kc in range(16):
                p.add('vector', 'scalar_tensor_tensor', h[:, kc, sub * NT:(sub + 1) * NT], xs[:, kc, :], gt[:, kc:kc + 1], rstd[:],
                      ALU.mult, ALU.mult, r=['xs', 'gt', 'rstd'], w=['h'])
        for cg in range(32):
            wt, wk = wr.next()
            w4 = wview(wt, 16, 256)
            p.dma(w4, w3[:, :, cg * 256:(cg + 1) * 256], w=[wk], eng='gpsimd')
            if 12 <= cg < 16:
                for ts in range(ST // 128):
                    ps, pk = pm.next()
                    for kc in range(16):
                        p.mm(ps[:, 0:256], h[:, kc, ts * 128:(ts + 1) * 128], w4[:, kc, :], start=(kc == 0),
                             stop=(kc == 15), r=['h', wk], w=[pk])
                    eb, ek = evb.next()
                    evac(eb[:, 0:256], ps[:, 0:256], [pk], [ek])
                    r0 = st * ST + ts * 128
                    p.dma(v[r0:r0 + 128, (cg - 12) * 256:(cg - 11) * 256], eb[:, 0:256], r=[ek])
                continue
            for o2 in range(2):
                oc = cg * 2 + o2
                for sub in range(ST // NT):
                    t0 = st * ST + sub * NT
                    tt = t0 // NT
                    ps, pk = pm.next()
                    for kc in range(16):
                        p.mm(ps[:], w4[:, kc, o2 * 128:(o2 + 1) * 128], h[:, kc, sub * NT:(sub + 1) * NT], start=(kc == 0),
                             stop=(kc == 15), r=['h', wk], w=[pk])
                    if oc < 8:
                        ub, uk = ubr.next()
                        p.add('vector', 'tensor_copy', ub[:].rearrange("p s c -> p c s"),
                              ps[:].rearrange("p (c s) -> p c s", s=8), r=[pk], w=[uk])
                        for gl in range(8):
                            gg = oc * 8 + gl
                            p.dma(uP[gg].rearrange("(s m) c -> m s c", m=16)[:, :, tt * 64:(tt + 1) * 64],
                                  ub[gl * 16:(gl + 1) * 16, :, :], r=[uk])
                    elif oc < 24:
                        eb, ek = evb.next()
                        evac(eb[:], ps[:], [pk], [ek])
                        dst = qT if oc < 16 else kT
                        c = (oc - 8) % 8
                        p.dma(dst[c * 128:(c + 1) * 128, t0:t0 + NT], eb[:], r=[ek])
                    else:
                        ef, fk = evf.next()
                        p.act(ef[:], ps[:], AF.Sigmoid, r=[pk], w=[fk])
                        c = oc - 32
                        p.dma(sg[c * 128:(c + 1) * 128, t0:t0 + NT], ef[:], r=[fk])
    p.pop_scope()


NG = 32
NH = 4
KK_EXP = [0, -1, -2, -3, -4, -5, -6, -7] + list(range(8)) + [7, 6, 5, 4, 3, 2, 1, 0] + list(range(1, 9)) + [8]
I32 = mybir.dt.int32
TWO_PI = 6.283185307179586
ATT_SCALE = 1.0 / (128 ** 0.5)


def emit_B(p, E, l):
    uP_all = E['uP']; qT = E['qT']; kT = E['kT']; vv = E['v']; yP_all = E['yP']; aT = E['aT']
    c_identf = E['identf']; c_identb = E['identb']; c_tmask = E['tmask']; c_sel = E['selE']
    c_causal = E['causal']; c_pastm = E['pastm']; kk = E['kk']
    p.push_scope()

    def ld(name, src, shape, dt=F32, eng='sync'):
        t = p.sb(name, shape, dt)
        p.dma(t[:], src, w=[name], eng=eng)
        return t

    identf = ld("identf_t", c_identf, [128, 128]); identb = ld("identb_t", c_identb, [128, 128], BF16)
    tmask = ld("tmask_t", c_tmask, [128, 128]); selE = ld("selE_t", c_sel, [16, 16, 128], BF16)
    causal = ld("causal_t", c_causal, [128, 2, 256], BF16); pastm = ld("pastm_t", c_pastm, [128, 16, 16])
    onesb = p.sb("onesb", [128, 128], BF16)
    p.add('vector', 'memset', onesb[:], 1.0, w=['onesb'])
    pb = E['pb']

    for gh in range(2):
        sp = E['ssm'][l][gh]
        lamre = sp['lamre']; lamim = sp['lamim']; ldt = sp['ldt']; bre = sp['bre']; bim = sp['bim']
        cre = sp['cre']; cim = sp['cim']; dsk = sp['dsk']
        uP = uP_all[gh * NG:(gh + 1) * NG]; yP = yP_all[gh * NG:(gh + 1) * NG]
        p.push_scope()
        TT = p.sb("TT", [128, NG, 128], BF16); P1 = p.sb("P1", [128, NG, 128], BF16)
        QQre = p.sb("QQre", [64, NG, 128], BF16); QQnim = p.sb("QQnim", [64, NG, 128], BF16)
        C1 = p.sb("C1", [64, 64], F32); C2 = p.sb("C2", [64, 64], F32)
        p.push_scope()
        t_lre = ld("t_lre", lamre, [64, NG]); t_lim = ld("t_lim", lamim, [64, NG]); t_ldt = ld("t_ldt", ldt, [64, NG])
        t_bre = ld("t_bre", bre, [64, NG, 16]); t_bim = ld("t_bim", bim, [64, NG, 16])
        t_cre = ld("t_cre", cre, [64, NG, 16]); t_cim = ld("t_cim", cim, [64, NG, 16])
        t_dsk = ld("t_dsk", dsk, [128, NG]); t_kk = ld("t_kk", kk, [64, 33])
        V = 'vector'
        sh3 = [64, NG, 33]
        dt_ = p.sb("dt_", [64, NG], F32); ar = p.sb("ar", [64, NG], F32); ai = p.sb("ai", [64, NG], F32)
        p.act(dt_[:], t_ldt[:], AF.Exp, r=['t_ldt'], w=['dt_'])
        p.add(V, 'tensor_tensor', ar[:], t_lre[:], dt_[:], ALU.mult, r=['t_lre', 'dt_'], w=['ar'])
        p.add(V, 'tensor_tensor', ai[:], t_lim[:], dt_[:], ALU.mult, r=['t_lim', 'dt_'], w=['ai'])
        th = p.sb("th", sh3, F32); mag = p.sb("mag", sh3, F32)
        pwre = p.sb("pwre", sh3, F32); pwim = p.sb("pwim", sh3, F32)
        ni = p.sb("ni", sh3, I32); nf = p.sb("nf", sh3, F32); yy = p.sb("yy", sh3, F32)
        kkb = t_kk[:].unsqueeze(1).to_broadcast(sh3)
        p.add(V, 'tensor_tensor', mag[:], ar[:].unsqueeze(2).to_broadcast(sh3), kkb, ALU.mult, r=['ar', 't_kk'], w=['mag'])
        p.act(mag[:], mag[:], AF.Exp, r=['mag'], w=['mag'])
        p.add(V, 'tensor_tensor', th[:], ai[:].unsqueeze(2).to_broadcast(sh3), kkb, ALU.mult, r=['ai', 't_kk'], w=['th'])

        def sin_of(dst, dkey, off):
            p.add(V, 'tensor_scalar', yy[:], th[:], 1.0 / TWO_PI, 32.5 + off, ALU.mult, ALU.add, r=['th'], w=['yy'])
            p.add(V, 'tensor_copy', ni[:], yy[:], r=['yy'], w=['ni'])
            p.add(V, 'tensor_copy', nf[:], ni[:], r=['ni'], w=['nf'])
            p.add(V, 'tensor_tensor', yy[:], yy[:], nf[:], ALU.subtract, r=['yy', 'nf'], w=['yy'])
            p.add(V, 'scalar_tensor_tensor', nf[:], yy[:], 0.0, yy[:], ALU.is_lt, ALU.add, r=['yy'], w=['nf'])
            p.add(V, 'tensor_scalar', nf[:], nf[:], TWO_PI, -TWO_PI / 2, ALU.mult, ALU.add, r=['nf'], w=['nf'])
            p.act(nf[:], nf[:], AF.Sin, r=['nf'], w=['nf'])
            p.add(V, 'tensor_tensor', dst[:], nf[:], mag[:], ALU.mult, r=['nf', 'mag'], w=[dkey])

        sin_of(pwim, 'pwim', 0.0)
        sin_of(pwre, 'pwre', 0.25)
        nr = p.sb("nr", [64, NG], F32); dd = p.sb("dd", [64, NG], F32); t0_ = p.sb("t0_", [64, NG], F32)
        qre = p.sb("qre", [64, NG], F32); qim = p.sb("qim", [64, NG], F32)
        p.add(V, 'tensor_scalar', nr[:], pwre[:, :, 9], -1.0, None, ALU.add, r=['pwre'], w=['nr'])
        nim = pwim[:, :, 9]
        p.add(V, 'tensor_tensor', dd[:], t_lre[:], t_lre[:], ALU.mult, r=['t_lre'], w=['dd'])
        p.add(V, 'tensor_tensor', t0_[:], t_lim[:], t_lim[:], ALU.mult, r=['t_lim'], w=['t0_'])
        p.add(V, 'tensor_tensor', dd[:], dd[:], t0_[:], ALU.add, r=['dd', 't0_'], w=['dd'])
        p.add(V, 'reciprocal', dd[:], dd[:], r=['dd'], w=['dd'])
        p.add(V, 'tensor_tensor', qre[:], nr[:], t_lre[:], ALU.mult, r=['nr', 't_lre'], w=['qre'])
        p.add(V, 'tensor_tensor', t0_[:], nim, t_lim[:], ALU.mult, r=['pwim', 't_lim'], w=['t0_'])
        p.add(V, 'tensor_tensor', qre[:], qre[:], t0_[:], ALU.add, r=['qre', 't0_'], w=['qre'])
        p.add(V, 'tensor_tensor', qre[:], qre[:], dd[:], ALU.mult, r=['qre', 'dd'], w=['qre'])
        p.add(V, 'tensor_tensor', qim[:], nim, t_lre[:], ALU.mult, r=['pwim', 't_lre'], w=['qim'])
        p.add(V, 'tensor_tensor', t0_[:], nr[:], t_lim[:], ALU.mult, r=['nr', 't_lim'], w=['t0_'])
        p.add(V, 'tensor_tensor', qim[:], qim[:], t0_[:], ALU.subtract, r=['qim', 't0_'], w=['qim'])
        p.add(V, 'tensor_tensor', qim[:], qim[:], dd[:], ALU.mult, r=['qim', 'dd'], w=['qim'])
        sh16 = [64, NG, 16]
        bbre = p.sb("bbre", sh16, F32); bbim = p.sb("bbim", sh16, F32); t16 = p.sb("t16", sh16, F32)
        qreb = qre[:].unsqueeze(2).to_broadcast(sh16); qimb = qim[:].unsqueeze(2).to_broadcast(sh16)
        p.add(V, 'tensor_tensor', bbre[:], t_bre[:], qreb, ALU.mult, r=['t_bre', 'qre'], w=['bbre'])
        p.add(V, 'tensor_tensor', t16[:], t_bim[:], qimb, ALU.mult, r=['t_bim', 'qim'], w=['t16'])
        p.add(V, 'tensor_tensor', bbre[:], bbre[:], t16[:], ALU.subtract, r=['bbre', 't16'], w=['bbre'])
        p.add(V, 'tensor_tensor', bbim[:], t_bim[:], qreb, ALU.mult, r=['t_bim', 'qre'], w=['bbim'])
        p.add(V, 'tensor_tensor', t16[:], t_bre[:], qimb, ALU.mult, r=['t_bre', 'qim'], w=['t16'])
        p.add(V, 'tensor_tensor', bbim[:], bbim[:], t16[:], ALU.add, r=['bbim', 't16'], w=['bbim'])
        sh4 = [64, NG, 8, 16]
        LL = p.sb("LL", [128, NG, 128], BF16); RR = p.sb("RR", [128, NG, 128], BF16); PP = p.sb("PP", [128, NG, 128], BF16)
        imtmp = p.sb("imtmp", [64, NG, 128], BF16)
        ta = p.sb("ta", sh4, F32); tb = p.sb("tb", sh4, F32)

        def cprod(i0, vre, vim, vkeys, out_re, rekey, out_im, imkey, neg_im):
            pr = pwre[:, :, i0:i0 + 8].unsqueeze(3).to_broadcast(sh4)
            pi_ = pwim[:, :, i0:i0 + 8].unsqueeze(3).to_broadcast(sh4)
            vr = vre[:].unsqueeze(2).to_broadcast(sh4)
            vi = vim[:].unsqueeze(2).to_broadcast(sh4)
            o_re = out_re.rearrange("p g (j m) -> p g j m", m=16)
            o_im = out_im.rearrange("p g (j m) -> p g j m", m=16)
            p.add(V, 'tensor_tensor', ta[:], pr, vr, ALU.mult, r=['pwre'] + vkeys, w=['ta'])
            p.add('gpsimd', 'tensor_tensor', tb[:], pi_, vi, ALU.mult, r=['pwim'] + vkeys, w=['tb'])
            p.add(V, 'tensor_tensor', o_re, ta[:], tb[:], ALU.subtract, r=['ta', 'tb'], w=[rekey])
            p.add(V, 'tensor_tensor', ta[:], pr, vi, ALU.mult, r=['pwre'] + vkeys, w=['ta'])
            p.add('gpsimd', 'tensor_tensor', tb[:], pi_, vr, ALU.mult, r=['pwim'] + vkeys, w=['tb'])
            if neg_im:
                p.add(V, 'scalar_tensor_tensor', o_im, ta[:], -1.0, tb[:], ALU.mult, ALU.subtract, r=['ta', 'tb'], w=[imkey])
            else:
                p.add(V, 'tensor_tensor', o_im, ta[:], tb[:], ALU.add, r=['ta', 'tb'], w=[imkey])

        cprod(0, bbre, bbim, ['bbre', 'bbim'], LL[0:64], 'LL', imtmp[:], 'imtmp', False)
        p.dma(LL[64:128], imtmp[:], r=['imtmp'], w=['LL'])
        cprod(8, t_cre, t_cim, ['t_cre', 't_cim'], RR[0:64], 'RR', imtmp[:], 'imtmp', True)
        p.dma(RR[64:128], imtmp[:], r=['imtmp'], w=['RR'])
        cprod(16, bbre, bbim, ['bbre', 'bbim'], PP[0:64], 'PP', imtmp[:], 'imtmp', False)
        p.dma(PP[64:128], imtmp[:], r=['imtmp'], w=['PP'])
        cprod(24, t_cre, t_cim, ['t_cre', 't_cim'], QQre[:], 'QQre', QQnim[:], 'QQnim', True)
        p.add(V, 'tensor_copy', C1[:, 0:32], pwre[:, :, 32], r=['pwre'], w=['C1'])
        p.add(V, 'tensor_copy', C1[:, 32:64], pwre[:, :, 32], r=['pwre'], w=['C1'])
        p.add(V, 'tensor_scalar', C2[:, 0:32], pwim[:, :, 32], -1.0, None, ALU.mult, r=['pwim'], w=['C2'])
        p.add(V, 'tensor_copy', C2[:, 32:64], pwim[:, :, 32], r=['pwim'], w=['C2'])
        tfr = Ring(p, "tfr", 2, [128, 4, 128], F32)
        for g0 in range(0, NG, 4):
            ps, pk = pb.next()
            for gl in range(4):
                p.mm(ps[:, gl * 128:(gl + 1) * 128], LL[:, g0 + gl, :], RR[:, g0 + gl, :], r=['LL', 'RR'], w=[pk])
            tf, tk = tfr.next()
            p.add(V, 'tensor_tensor', tf[:], ps[:].rearrange("p (g c) -> p g c", c=128),
                  tmask[:].unsqueeze(1).to_broadcast([128, 4, 128]), ALU.mult, r=[pk, 'tmask_t'], w=[tk])
            for gl in range(4):
                p.add(V, 'scalar_tensor_tensor', TT[:, g0 + gl, :], identf[:], t_dsk[:, g0 + gl:g0 + gl + 1], tf[:, gl, :],
                      ALU.mult, ALU.add, r=['identf_t', 't_dsk', tk], w=['TT'])
            ps2, pk2 = pb.next()
            for gl in range(4):
                p.mm(ps2[:, gl * 128:(gl + 1) * 128], PP[:, g0 + gl, :], identb[:], r=['PP', 'identb_t'], w=[pk2])
            p.add('scalar', 'copy', P1[:, g0:g0 + 4, :], ps2[:].rearrange("p (g c) -> p g c", c=128), r=[pk2], w=['P1'])

        p.pop_scope()
        p.push_scope()
        U = p.sb("U", [128, NG, 512], BF16)
        for q4 in range(4):
            p.dma(U[:, q4 * 8:(q4 + 1) * 8, :], uP[q4 * 8:(q4 + 1) * 8].rearrange("g p c -> p g c"), w=['U'])
        hist = p.sb("hist", [64, 129, 96], F32)
        Gst = p.sb("Gst", [64, 128, 64], F32)
        Hre = p.sb("Hre", [64, NG, 128], BF16); Him = p.sb("Him", [64, NG, 128], BF16)
        t1 = p.sb("t1", [64, 64], F32); t2 = p.sb("t2", [64, 64], F32); t3 = p.sb("t3", [64, 64], F32)
        ybr = Ring(p, "yb", 2, [128, 4, 128], F32)
        p.add(V, 'memset', hist[:, 0, :], 0.0, w=['histA', 'histB'])
        for seg in range(4):
            cs = slice(seg * 128, (seg + 1) * 128)
            if seg > 0:
                p.add(V, 'tensor_copy', hist[:, 0, :], hist[:, 128, :], r=['histA', 'histB'], w=['histA', 'histB'])
            for g0 in range(0, NG, 4):
                for ri in range(2):
                    ps, pk = pb.next()
                    for gl in range(4):
                        p.mm(ps[0:64, gl * 128:(gl + 1) * 128], P1[:, g0 + gl, ri * 64:(ri + 1) * 64], U[:, g0 + gl, cs],
                             r=['P1', 'U'], w=[pk])
                    dst = Gst[:, :, ri * 32 + g0:ri * 32 + g0 + 4].rearrange("p c g -> p g c")
                    src = ps[0:64, :].rearrange("p (g c) -> p g c", c=128)
                    if ri == 0:
                        p.add(V, 'tensor_copy', dst, src, r=[pk], w=['Gst'])
                    else:
                        p.add('scalar', 'copy', dst, src, r=[pk], w=['Gst'])
            for c in range(128):
                p.add(V, 'tensor_tensor', t1[:], hist[:, c, 0:64], C1[:], ALU.mult, r=['histA', 'C1'], w=['t1'])
                p.add(V, 'tensor_tensor', t2[:], hist[:, c, 32:96], C2[:], ALU.mult, r=['histA', 'histB', 'C2'], w=['t2'])
                p.add(V, 'tensor_tensor', t3[:], t1[:], t2[:], ALU.add, r=['t1', 't2'], w=['t3'])
                p.add(V, 'tensor_tensor', hist[:, c + 1, 0:64], t3[:], Gst[:, c, :], ALU.add, r=['t3', 'Gst'], w=['histA'])
                p.add(V, 'tensor_tensor', hist[:, c + 1, 64:96], t3[:, 0:32], Gst[:, c, 0:32], ALU.add,
                      r=['t3', 'Gst'], w=['histB'])
            p.add(V, 'tensor_copy', Hre[:], hist[:, 0:128, 0:32].rearrange("p c g -> p g c"), r=['histA'], w=['Hre'])
            p.add('gpsimd', 'tensor_copy', Him[:], hist[:, 0:128, 32:64].rearrange("p c g -> p g c"), r=['histA'], w=['Him'])
            for g0 in range(0, NG, 4):
                ps, pk = pb.next()
                for gl in range(4):
                    g = g0 + gl
                    o = ps[:, gl * 128:(gl + 1) * 128]
                    p.mm(o, TT[:, g, :], U[:, g, cs], start=True, stop=False, r=['TT', 'U'], w=[pk])
                    p.mm(o, QQre[:, g, :], Hre[:, g, :], start=False, stop=False, r=['QQre', 'Hre'], w=[pk])
                    p.mm(o, QQnim[:, g, :], Him[:, g, :], start=False, stop=True, r=['QQnim', 'Him'], w=[pk])
                yb, yk = ybr.next()
                p.add('scalar', 'copy', yb[:], ps[:].rearrange("p (g c) -> p g c", c=128), r=[pk], w=[yk])
                p.dma(yP[g0:g0 + 4, :, cs].rearrange("g p c -> p g c"), yb[:], r=[yk])


        p.pop_scope()
        p.pop_scope()
    Kh = p.sb("Kh", [128, S], BF16); Qh = p.sb("Qh", [128, S], BF16); Vh = p.sb("Vh", [128, 32, 128], BF16)
    ksum = p.sb("ksum", [128, 16], F32); kmh = p.sb("kmh", [128, 16], BF16); kml = p.sb("kml", [128, 16], BF16)
    kmhf = p.sb("kmhf", [128, 16], F32)
    maskT = p.sb("maskT", [16, S], BF16)
    gmr = Ring(p, "gm", 2, [128, 16], F32); mxr = Ring(p, "mx", 2, [128, 8], F32); thr_r = Ring(p, "thr", 2, [128, 1], F32)
    bir = Ring(p, "bi", 2, [128, 16], F32)
    ptr = Ring(p, "pt", 3, [128, 512], BF16)
    rlr = Ring(p, "rl", 2, [128, 512], F32); obr = Ring(p, "ob", 2, [128, 512], BF16)
    psr = SubRing(pb, [0, 1, 2]); por = SubRing(pb, [3, 4]); plr = SubRing(pb, [5, 6]); pgr = SubRing(pb, [7])
    for hh in range(8):
        hs = slice(hh * 128, (hh + 1) * 128)
        p.dma(Kh[:], kT[hs, :], w=['Kh'])
        p.dma(Qh[:], qT[hs, :], w=['Qh'])
        p.dma(Vh[:], vv[:, hs].rearrange("(kt p) d -> p kt d", p=128), w=['Vh'])
        p.add(V, 'tensor_reduce', ksum[:], Kh[:].rearrange("p (j l) -> p j l", l=256), AX.X, ALU.add, r=['Kh'], w=['ksum'])
        p.add(V, 'tensor_scalar', ksum[:], ksum[:], 1.0 / 256, None, ALU.mult, r=['ksum'], w=['ksum'])
        p.add(V, 'tensor_copy', kmh[:], ksum[:], r=['ksum'], w=['kmh'])
        p.add(V, 'tensor_copy', kmhf[:], kmh[:], r=['kmh'], w=['kmhf'])
        p.add(V, 'tensor_tensor', kmhf[:], ksum[:], kmhf[:], ALU.subtract, r=['ksum', 'kmhf'], w=['kmhf'])
        p.add(V, 'tensor_copy', kml[:], kmhf[:], r=['kmhf'], w=['kml'])
        for qs in range(32):
            n = qs // 2
            ps, pk = pgr.next()
            p.mm(ps[:, 0:16], Qh[:, qs * 128:(qs + 1) * 128], kmh[:], start=True, stop=False, r=['Qh', 'kmh'], w=[pk])
            p.mm(ps[:, 0:16], Qh[:, qs * 128:(qs + 1) * 128], kml[:], start=False, stop=True, r=['Qh', 'kml'], w=[pk])
            gm, gk = gmr.next(); mx, mk = mxr.next(); th_, tk_ = thr_r.next(); bi, bk = bir.next()
            p.add(V, 'tensor_tensor', gm[:], ps[:, 0:16], pastm[:, n, :], ALU.add, r=[pk, 'pastm_t'], w=[gk])
            p.add(V, 'max', mx[:], gm[:], r=[gk], w=[mk])
            p.add(V, 'tensor_scalar_max', th_[:], mx[:, 2:3], -10000.0, r=[mk], w=[tk_])
            p.add(V, 'tensor_scalar', bi[:], gm[:], th_[:, 0:1], -NEGM, ALU.is_ge, ALU.mult, r=[gk, tk_], w=[bk])
            p.add(V, 'tensor_scalar', bi[:], bi[:], NEGM, None, ALU.add, r=[bk], w=[bk])
            p.add(V, 'memset', bi[:, n:n + 1], 0.0, w=[bk])
            ps2, pk2 = psr.next()
            p.mm(ps2[0:16, 0:128], bi[:], identf[:], r=[bk, 'identf_t'], w=[pk2])
            p.add('scalar', 'copy', maskT[:, qs * 128:(qs + 1) * 128], ps2[0:16, 0:128], r=[pk2], w=['maskT'])
        for np_ in range(8):
            n0 = 2 * np_
            qsl = slice(n0 * 256, (n0 + 2) * 256)
            po, pok = por.next()
            pl, plk = plr.next()
            nkc = 2 * n0 + 2
            tot = nkc + 2
            for kt in range(tot):
                j = kt // 2
                cs_ = slice(0, 512) if kt < nkc else slice(256, 512)
                qcols = slice(n0 * 256, (n0 + 2) * 256) if kt < nkc else slice((n0 + 1) * 256, (n0 + 2) * 256)
                ps, pk = psr.next()
                p.mm(ps[:, cs_], Kh[:, kt * 128:(kt + 1) * 128], Qh[:, qcols], start=True, stop=False, r=['Kh', 'Qh'], w=[pk])
                p.mm(ps[:, cs_], selE[:, j, :], maskT[:, qcols], start=False, stop=(j < n0), r=['selE_t', 'maskT'], w=[pk])
                dcol = slice(0, 256) if j == n0 else slice(256, 512)
                if j >= n0:
                    p.mm(ps[:, dcol], identb[:], causal[:, kt % 2, :], start=False, stop=True, r=['identb_t', 'causal_t'], w=[pk])
                pt, ptk = ptr.next()
                p.act(pt[:, cs_], ps[:, cs_], AF.Exp, scale=ATT_SCALE, r=[pk], w=[ptk])
                p.mm(po[:, cs_], Vh[:, kt, :], pt[:, cs_], start=(kt == 0), stop=(kt == tot - 1), r=['Vh', ptk], w=[pok])
                p.mm(pl[:, cs_], onesb[:], pt[:, cs_], start=(kt == 0), stop=(kt == tot - 1), r=['onesb', ptk], w=[plk])
            rl, rk = rlr.next(); ob, ok_ = obr.next()
            p.add(V, 'reciprocal', rl[:], pl[:], r=[plk], w=[rk])
            p.add(V, 'tensor_tensor', ob[:], po[:], rl[:], ALU.mult, r=[pok, rk], w=[ok_])
            p.dma(aT[hs, qsl], ob[:], r=[ok_])
    p.pop_scope()


def _bf(a):
    return np.ascontiguousarray(a).astype(ml_dtypes.bfloat16)


def consts_B():
    sm = np.arange(128) // 16
    tmask = (sm[None, :] >= sm[:, None]).astype(np.float32)
    selE = np.zeros((16, 16, 128), np.float32)
    for j in range(16):
        selE[j, j, :] = 1.0
    k = np.arange(128)[:, None, None]
    a = np.arange(2)[None, :, None]
    q = np.arange(256)[None, None, :]
    causal = np.where(a * 128 + k <= q, 0.0, NEGM).astype(np.float32)
    n = np.arange(16)[None, :, None]
    j = np.arange(16)[None, None, :]
    pastm = np.broadcast_to(np.where(j < n, 0.0, NEGM), (128, 16, 16)).astype(np.float32)
    return {
        "identf": np.eye(128, dtype=np.float32), "identb": _bf(np.eye(128, dtype=np.float32)),
        "tmask": tmask, "selE": _bf(selE), "causal": _bf(causal), "pastm": np.ascontiguousarray(pastm),
        "kk": np.ascontiguousarray(np.tile(np.array(KK_EXP, np.float32), (64, 1))),
    }


def ssm_params_B(inp, l, gh):
    gs = slice(gh * NG, (gh + 1) * NG)
    c = np.ascontiguousarray
    return {
        "lamre": c(inp['lam_re'][l][gs].T), "lamim": c(inp['lam_im'][l][gs].T),
        "ldt": c(np.broadcast_to(inp['log_dt'][l][gs][None, :], (64, NG))),
        "bre": c(inp['b_re'][l][gs].transpose(1, 0, 2)), "bim": c(inp['b_im'][l][gs].transpose(1, 0, 2)),
        "cre": c(inp['c_re'][l][gs].transpose(2, 0, 1)), "cim": c(inp['c_im'][l][gs].transpose(2, 0, 1)),
        "dsk": c(np.tile(inp['d_skip'][l].reshape(64, 16)[gs].T, (8, 1))),
    }


def _glay(gv):
    return np.ascontiguousarray(np.asarray(gv, np.float32).reshape(16, 128).T)


def emit_C(p, E, l):
    yP = E['yP']; aT = E['aT']; sg = E['sg']; xT = E['xcur']
    w_glu = E['w_glu'][l]; w_us = E['w_up_ssm'][l]; w_ua = E['w_up_attn'][l]; w_out = E['w_out'][l]
    g1 = E['g_post_mix'][l]; g2 = E['g_pre_ffn'][l]
    xm_o = E['xm']; hf_o = E['hfx'][:, 2:T + 2]
    p.push_scope()
    V = 'vector'; G = 'gpsimd'
    onesf = p.sb("onesf", [128, 128], F32)
    p.add(V, 'memset', onesf[:], 1.0, w=['onesf'])
    g1t = p.sb("g1t", [128, 16], F32); g2t = p.sb("g2t", [128, 16], F32)
    p.dma(g1t[:], g1, w=['g1t']); p.dma(g2t[:], g2, w=['g2t'])
    ybr = Ring(p, "ybuf", 2, [128, 8, 64], F32)
    yall = p.sb("yall", [128, 8, NT], F32); yb = p.sb("ybb", [128, 8, NT], BF16); y2 = p.sb("y2", [128, 8, NT], BF16)
    at = p.sb("at", [128, 8, NT], BF16); mb = p.sb("mb", [128, 16, NT], BF16)
    mo = p.sb("mo", [128, 16, NT], F32); xs = p.sb("xs", [128, 16, NT], F32); hfb = p.sb("hfb", [128, 16, NT], BF16)
    tr = Ring(p, "tmp", 4, [128, NT], F32)
    sgr = Ring(p, "sgt", 4, [128, NT], F32)
    sqr = Ring(p, "sq", 2, [128, NT], F32)
    rstd = p.sb("rstd", [128, NT], F32)
    wr = Ring(p, "w", 3, [128, 16 * 256], BF16)
    ps_ss = E['pb'].t[7]
    pm = SubRing(E['pb'], [0, 1, 2, 3, 4, 5])
    xT3 = xT.rearrange("(kc p) t -> p kc t", p=128)
    aT3 = aT.rearrange("(kc p) t -> p kc t", p=128)

    def wload(wd, kc, cg):
        wt, wk = wr.next()
        w4 = wview(wt, kc, 256)
        p.dma(w4, wd.rearrange("(kc p) o -> p kc o", p=128)[:, :, cg * 256:(cg + 1) * 256], w=[wk], eng=G)
        return w4, wk

    for tt in range(T // NT):
        t0 = tt * NT
        ts = slice(t0, t0 + NT)
        for kc in range(8):
            ybf, yk = ybr.next()
            for gl in range(8):
                p.dma(ybf[gl * 16:(gl + 1) * 16, :, :],
                      yP[kc * 8 + gl].rearrange("(t m) c -> m t c", m=16)[:, :, tt * 64:(tt + 1) * 64], w=[yk])
            xv = ybf[:].rearrange("p t c -> p c t")
            a, ak = tr.next()
            av = a[:].rearrange("p (c t) -> p c t", t=8)
            p.add(G, 'tensor_tensor', av, xv, xv, ALU.mult, r=[yk], w=[ak])
            p.add(G, 'tensor_scalar', a[:], a[:], 0.044715, 1.0, ALU.mult, ALU.add, r=[ak], w=[ak])
            p.add(G, 'tensor_tensor', av, av, xv, ALU.mult, r=[ak, yk], w=[ak])
            p.act(a[:], a[:], AF.Sigmoid, scale=1.5957691216057308, r=[ak], w=[ak])
            p.add(V, 'tensor_tensor', yall[:, kc, :].rearrange("p (c t) -> p c t", t=8), av, xv, ALU.mult, r=[ak, yk], w=['yall'])
            p.add(G, 'tensor_copy', yb[:, kc, :], yall[:, kc, :], r=['yall'], w=['yb'])
        for cg in range(4):
            w4, wk = wload(w_glu, 8, cg)
            for o2 in range(2):
                oc = cg * 2 + o2
                ps, pk = pm.next()
                for kc in range(8):
                    p.mm(ps[:], w4[:, kc, o2 * 128:(o2 + 1) * 128], yb[:, kc, :], start=(kc == 0), stop=(kc == 7), r=[wk, 'yb'], w=[pk])
                s_, sk = tr.next()
                p.act(s_[:], ps[:], AF.Sigmoid, r=[pk], w=[sk])
                p.add(V, 'tensor_tensor', y2[:, oc, :], yall[:, oc, :], s_[:], ALU.mult, r=['yall', sk], w=['y2'])
        p.dma(at[:], aT3[:, :, ts], w=['at'])
        for cg in range(8):
            w1, k1 = wload(w_us, 8, cg)
            w2, k2 = wload(w_ua, 8, cg)
            for o2 in range(2):
                oc = cg * 2 + o2
                osl = slice(o2 * 128, (o2 + 1) * 128)
                sa, sak = sgr.next(); sb_, sbk = sgr.next()
                p.dma(sa[:], sg[oc * 128:(oc + 1) * 128, ts], w=[sak])
                p.dma(sb_[:], sg[2048 + oc * 128:2048 + (oc + 1) * 128, ts], w=[sbk])
                ps1, pk1 = pm.next(); ps2, pk2 = pm.next()
                for kc in range(8):
                    p.mm(ps1[:], w1[:, kc, osl], y2[:, kc, :], start=(kc == 0), stop=(kc == 7), r=[k1, 'y2'], w=[pk1])
                for kc in range(8):
                    p.mm(ps2[:], w2[:, kc, osl], at[:, kc, :], start=(kc == 0), stop=(kc == 7), r=[k2, 'at'], w=[pk2])
                m1, mk1 = tr.next(); m2, mk2 = tr.next()
                p.add(V, 'tensor_tensor', m1[:], ps1[:], sa[:], ALU.mult, r=[pk1, sak], w=[mk1])
                p.add(V, 'tensor_tensor', m2[:], ps2[:], sb_[:], ALU.mult, r=[pk2, sbk], w=[mk2])
                p.add(G, 'tensor_tensor', mb[:, oc, :], m1[:], m2[:], ALU.add, r=[mk1, mk2], w=['mb'])
        p.dma(xs[:], xT3[:, :, ts], w=['xs'])
        for cg in range(8):
            w4, wk = wload(w_out, 16, cg)
            for o2 in range(2):
                oc = cg * 2 + o2
                ps, pk = pm.next()
                for kc in range(16):
                    p.mm(ps[:], w4[:, kc, o2 * 128:(o2 + 1) * 128], mb[:, kc, :], start=(kc == 0), stop=(kc == 15), r=[wk, 'mb'], w=[pk])
                p.act(mo[:, oc, :], ps[:], AF.Copy, r=[pk], w=['mo'])
        rms_rstd(p, [mo[:, kc, :] for kc in range(16)], ['mo'], sqr, ps_ss, E['pb'].k[7], onesf, rstd, NT)
        for oc in range(16):
            p.add(V, 'scalar_tensor_tensor', mo[:, oc, :], mo[:, oc, :], g1t[:, oc:oc + 1], rstd[:], ALU.mult, ALU.mult,
                  r=['mo', 'g1t', 'rstd'], w=['mo'])
            p.add(G, 'tensor_tensor', mo[:, oc, :], mo[:, oc, :], xs[:, oc, :], ALU.add, r=['mo', 'xs'], w=['mo'])
        p.dma(xm_o.rearrange("(kc p) t -> p kc t", p=128)[:, :, ts], mo[:], r=['mo'])
        rms_rstd(p, [mo[:, kc, :] for kc in range(16)], ['mo'], sqr, ps_ss, E['pb'].k[7], onesf, rstd, NT)
        for oc in range(16):
            p.add(V, 'scalar_tensor_tensor', hfb[:, oc, :], mo[:, oc, :], g2t[:, oc:oc + 1], rstd[:], ALU.mult, ALU.mult,
                  r=['mo', 'g2t', 'rstd'], w=['hfb'])
        p.dma(hf_o.rearrange("(kc p) t -> p kc t", p=128)[:, :, ts], hfb[:], r=['hfb'])
    p.pop_scope()


def emit_D(p, E, l):
    hfx = E['hfx']; xm = E['xm']; w_up = E['w_ffn_up'][l]; w_dn = E['w_ffn_down'][l]
    cw = E['cw'][l]; cb = E['cb'][l]; g3 = E['g_post_ffn'][l]
    xo = E['xout'] if l == 1 else E['xnext']
    p.push_scope()
    V = 'vector'; G = 'gpsimd'
    onesf = p.sb("onesf", [128, 128], F32)
    p.add(V, 'memset', onesf[:], 1.0, w=['onesf'])
    g3t = p.sb("g3t", [128, 16], F32); cwt = p.sb("cwt", [128, 88, 3], F32); cbt = p.sb("cbt", [128, 88], F32)
    p.dma(g3t[:], g3, w=['g3t']); p.dma(cwt[:], cw, w=['cwt']); p.dma(cbt[:], cb, w=['cbt'])
    hx = p.sb("hx", [128, 16, NT + 2], BF16)
    ztail = p.sb("ztail", [128, 88, 2], F32)
    p.add(V, 'memset', ztail[:], 0.0, w=['ztail'])
    actb = p.sb("actb", [128, 44, NT], BF16)
    f = p.sb("f", [128, 16, NT], F32)
    zr = Ring(p, "z", 4, [128, NT + 2], F32)
    cr = Ring(p, "cv", 4, [128, NT], F32)
    xr = Ring(p, "xm", 2, [128, NT], F32)
    orr = Ring(p, "or", 2, [128, NT], F32)
    sqr = Ring(p, "sq", 2, [128, NT], F32)
    rstd = p.sb("rstd", [128, NT], F32)
    wr = Ring(p, "w", 3, [128, 2 * 16 * 256], BF16)
    ps_ss = E['pb'].t[7]
    pm = SubRing(E['pb'], [0, 1, 2, 3, 4])
    ph = SubRing(E['pb'], [5, 6])
    hfx3 = hfx.rearrange("(kc p) t -> p kc t", p=128)
    wup3 = w_up.rearrange("(kc p) o -> p kc o", p=128)
    wdn3 = w_dn.rearrange("(kc p) o -> p kc o", p=128)
    for tt in range(T // NT):
        t0 = tt * NT
        ts = slice(t0, t0 + NT)
        p.dma(hx[:], hfx3[:, :, t0:t0 + NT + 2], w=['hx'])
        for jj in range(22):
            wt, wk = wr.next()
            w5 = wt[:].rearrange("p (a k o) -> p a k o", a=2, o=256)
            p.dma(w5[:, 0], wup3[:, :, jj * 256:(jj + 1) * 256], w=[wk], eng=G)
            p.dma(w5[:, 1], wup3[:, :, DFF + jj * 256:DFF + (jj + 1) * 256], w=[wk], eng=G)
            for o2 in range(2):
                j = jj * 2 + o2
                osl = slice(o2 * 128, (o2 + 1) * 128)
                cres = []
                for which in range(2):
                    ch = which * 44 + j
                    ps, pk = pm.next()
                    for kc in range(16):
                        p.mm(ps[:], w5[:, which, kc, osl], hx[:, kc, 2:NT + 2], start=(kc == 0), stop=(kc == 15), r=[wk, 'hx'], w=[pk])
                    zt, zk = zr.next()
                    p.act(zt[:, 2:NT + 2], ps[:], AF.Copy, r=[pk], w=[zk])
                    p.add(G, 'tensor_copy', zt[:, 0:2], ztail[:, ch, :], r=['ztail'], w=[zk])
                    p.add(G, 'tensor_copy', ztail[:, ch, :], zt[:, NT:NT + 2], r=[zk], w=['ztail'])
                    c1, ck = cr.next()
                    p.add(V, 'tensor_scalar', c1[:], zt[:, 2:NT + 2], cwt[:, ch, 2:3], cbt[:, ch:ch + 1], ALU.mult, ALU.add,
                          r=[zk, 'cwt', 'cbt'], w=[ck])
                    p.add(V, 'scalar_tensor_tensor', c1[:], zt[:, 1:NT + 1], cwt[:, ch, 1:2], c1[:], ALU.mult, ALU.add,
                          r=[zk, 'cwt', ck], w=[ck])
                    p.add(V, 'scalar_tensor_tensor', c1[:], zt[:, 0:NT], cwt[:, ch, 0:1], c1[:], ALU.mult, ALU.add,
                          r=[zk, 'cwt', ck], w=[ck])
                    cres.append((c1, ck))
                (ca, cak), (cv_, cvk) = cres
                p.act(ca[:], ca[:], AF.Silu, r=[cak], w=[cak])
                p.add(V, 'tensor_tensor', actb[:, j, :], ca[:], cv_[:], ALU.mult, r=[cak, cvk], w=['actb'])
        for oc in range(16):
            wt, wk = wr.next()
            w4 = wview(wt, 44, 128)
            p.dma(w4, wdn3[:, :, oc * 128:(oc + 1) * 128], w=[wk], eng=G)
            ps, pk = pm.next()
            for j in range(44):
                p.mm(ps[:], w4[:, j, :], actb[:, j, :], start=(j == 0), stop=(j == 43), r=[wk, 'actb'], w=[pk])
            p.act(f[:, oc, :], ps[:], AF.Copy, r=[pk], w=['f'])
        rms_rstd(p, [f[:, kc, :] for kc in range(16)], ['f'], sqr, ps_ss, E['pb'].k[7], onesf, rstd, NT)
        for oc in range(16):
            xt, xk = xr.next(); ot, ok_ = orr.next()
            p.dma(xt[:], xm[oc * 128:(oc + 1) * 128, ts], w=[xk])
            p.add(V, 'scalar_tensor_tensor', ot[:], f[:, oc, :], g3t[:, oc:oc + 1], rstd[:], ALU.mult, ALU.mult,
                  r=['f', 'g3t', 'rstd'], w=[ok_])
            p.add(G, 'tensor_tensor', ot[:], ot[:], xt[:], ALU.add, r=[ok_, xk], w=[ok_])
            p.dma(xo[oc * 128:(oc + 1) * 128, ts], ot[:], r=[ok_])
    p.pop_scope()


def build_fused():
    nc = bass.Bass("TRN2", target_bir_lowering=False)
    E = {}

    def din(name, shape, dt=F32):
        return nc.dram_tensor(name, shape, dt, kind="ExternalInput").ap()

    def dscr(name, shape, dt=F32):
        return nc.dram_tensor(name, shape, dt, kind="Internal").ap()

    E['xin'] = din("xT", [D, T])
    for nm in ['g_pre_mix', 'g_post_mix', 'g_pre_ffn', 'g_post_ffn']:
        E[nm] = din(nm, [2, 128, 16])
    E['w_in'] = din("w_in", [2, D, 8192]); E['w_glu'] = din("w_glu", [2, 1024, 1024])
    E['w_up_ssm'] = din("w_up_ssm", [2, 1024, D]); E['w_up_attn'] = din("w_up_attn", [2, 1024, D])
    E['w_out'] = din("w_out", [2, D, D]); E['w_ffn_up'] = din("w_ffn_up", [2, D, 2 * DFF]); E['w_ffn_down'] = din("w_ffn_down", [2, DFF, D])
    E['cw'] = din("cw", [2, 128, 88, 3]); E['cb'] = din("cb", [2, 128, 88])
    E['ssm'] = [[{k: din("%s_%d_%d" % (k, l, gh), shp) for k, shp in
                  [('lamre', [64, NG]), ('lamim', [64, NG]), ('ldt', [64, NG]), ('bre', [64, NG, 16]), ('bim', [64, NG, 16]),
                   ('cre', [64, NG, 16]), ('cim', [64, NG, 16]), ('dsk', [128, NG])]} for gh in range(2)] for l in range(2)]
    E['kk'] = din("kk", [64, 33]); E['identf'] = din("identf", [128, 128]); E['identb'] = din("identb", [128, 128], BF16)
    E['tmask'] = din("tmask", [128, 128]); E['selE'] = din("selE", [16, 16, 128], BF16)
    E['causal'] = din("causal", [128, 2, 256], BF16); E['pastm'] = din("pastm", [128, 16, 16])
    E['xout'] = nc.dram_tensor("xo", [D, T], F32, kind="ExternalOutput").ap()
    E['xnext'] = dscr("xnext", [D, T])
    E['uP'] = dscr("uP", [64, 128, 512], BF16); E['qT'] = dscr("qT", [1024, T], BF16); E['kT'] = dscr("kT", [1024, T], BF16)
    E['v'] = dscr("v", [T, 1024], BF16); E['sg'] = dscr("sg", [4096, T]); E['yP'] = dscr("yP", [64, 128, 512])
    E['aT'] = dscr("aT", [1024, T], BF16); E['xm'] = dscr("xm", [D, T]); E['hfx'] = dscr("hfx", [D, T + 2], BF16)
    p = Prog(nc)
    E['pb'] = PRing(p, "pb", 8)
    zt = p.sb("zt", [128, 16, 2], BF16)
    p.add('vector', 'memset', zt[:], 0.0, w=['zt'])
    p.dma(E['hfx'].rearrange("(kc p) t -> p kc t", p=128)[:, :, 0:2], zt[:], r=['zt'])
    for l in range(2):
        E['xcur'] = E['xin'] if l == 0 else E['xnext']
        emit_A(p, E, l)
        emit_B(p, E, l)
        emit_C(p, E, l)
        emit_D(p, E, l)
    p.emit()
    return nc


_NC = []


def kernel(**inp):
    c_ = np.ascontiguousarray
    f32 = lambda a: c_(np.asarray(a, np.float32))
    if not _NC:
        _NC.append(build_fused())
    nc = _NC[0]
    x = f32(inp['x'])
    shared = dict(consts_B())
    for nm in ['g_pre_mix', 'g_post_mix', 'g_pre_ffn', 'g_post_ffn']:
        shared[nm] = c_(np.stack([_glay(inp[nm][l]) for l in range(2)]))
    shared['w_in'] = f32(inp['w_in']); shared['w_glu'] = f32(inp['w_glu'])
    shared['w_up_ssm'] = f32(inp['w_up_ssm']); shared['w_up_attn'] = f32(inp['w_up_attn'])
    shared['w_out'] = f32(inp['w_out']); shared['w_ffn_up'] = f32(inp['w_ffn_up']); shared['w_ffn_down'] = f32(inp['w_ffn_down'])
    shared['cw'] = c_(np.stack([f32(inp['conv_w'][l]).reshape(3, 88, 128).transpose(2, 1, 0) for l in range(2)]))
    shared['cb'] = c_(np.stack([f32(inp['conv_b'][l]).reshape(88, 128).T for l in range(2)]))
    for l in range(2):
        for gh in range(2):
            for k, v in ssm_params_B(inp, l, gh).items():
                shared["%s_%d_%d" % (k, l, gh)] = v
    nb = x.shape[0]
    maps = []
    for b in range(nb):
        m = dict(shared)
        m['xT'] = c_(x[b].T)
        maps.append(m)
    res = run_bass_kernel_spmd(nc, maps, core_ids=list(range(nb))).results
    return c_(np.stack([res[b]['xo'].T for b in range(nb)]))
```

```python
import numpy as np
import ml_dtypes
from contextlib import ExitStack
import concourse.bass as bass
import concourse.mybir as mybir
from concourse.bass_utils import run_bass_kernel_spmd

F32 = mybir.dt.float32
BF16 = mybir.dt.bfloat16
ALU = mybir.AluOpType
AF = mybir.ActivationFunctionType
AX = mybir.AxisListType
ENGS = ['tensor', 'vector', 'scalar', 'gpsimd', 'sync']


class _Op:
    __slots__ = ('eng', 'fn', 'dma', 'deps', 'signal', 'sigval', 'dsem', 'dval', 'pre')

    def __init__(self, eng, fn, dma):
        self.eng = eng
        self.fn = fn
        self.dma = dma
        self.deps = []
        self.signal = False
        self.sigval = 0
        self.dsem = None
        self.dval = 0
        self.pre = None


class Prog:
    def __init__(self, nc, ndma=12):
        self.nc = nc
        self.ops = {e: [] for e in ENGS}
        self.last_w = {}
        self.readers = {}
        self.ndma = ndma
        self.dma_count = {e: 0 for e in ENGS}
        self.es = ExitStack()
        self.scopes = []
        self._uid = 0
        self._par = {}
        self.nbar = 0

    def sb(self, name, shape, dt):
        st = self.scopes[-1] if self.scopes else self.es
        self._uid += 1
        return st.enter_context(self.nc.sbuf_tensor("%s_%d" % (name, self._uid), shape, dt))

    def parity(self, e):
        k = id(e)
        if k not in self._par:
            self._par[k] = e.partition_id() % 2
        return self._par[k]

    def push_scope(self):
        self.scopes.append(ExitStack())

    def pop_scope(self):
        self.barrier()
        self.scopes.pop().close()

    def barrier(self):
        self.nbar += 1
        for e in ENGS:
            o = _Op(e, None, False)
            o.pre = ('bar', self.nbar)
            self.ops[e].append(o)
        self.last_w = {}
        self.readers = {}

    def ps(self, name, shape, dt=F32):
        return self.es.enter_context(self.nc.psum_tensor(name, shape, dt))

    def _add(self, eng, fn, r, w, dma=False):
        o = _Op(eng, fn, dma)
        deps = {}
        for b in r:
            lw = self.last_w.get(b)
            if lw is not None:
                deps[id(lw)] = (lw, True)
        for b in w:
            lw = self.last_w.get(b)
            if lw is not None and id(lw) not in deps:
                deps[id(lw)] = (lw, False)
            rd = self.readers.get(b)
            if rd:
                for x in rd[0].values():
                    if id(x) not in deps:
                        deps[id(x)] = (x, False)
                for x in rd[1]:
                    if id(x) not in deps:
                        deps[id(x)] = (x, False)
        for d, raw in deps.values():
            if d is o:
                continue
            if not d.dma and not dma and d.eng == eng:
                if not raw or eng == 'tensor':
                    continue
            o.deps.append(d)
        for b in r:
            rd = self.readers.setdefault(b, ({}, []))
            if dma:
                rd[1].append(o)
            else:
                rd[0][eng] = o
        for b in w:
            self.last_w[b] = o
            self.readers[b] = ({}, [])
        self.ops[eng].append(o)
        return o

    def add(self, eng, meth, *args, r=(), w=(), **kw):
        return self._add(eng, lambda e: getattr(e, meth)(*args, **kw), r, w)

    def dma(self, out, in_, r=(), w=(), eng='sync', **kw):
        return self._add(eng, lambda e: e.dma_start(out=out, in_=in_, **kw), r, w, dma=True)

    def mm(self, out, lhsT, rhs, start=True, stop=True, r=(), w=(), **kw):
        return self.add('tensor', 'matmul', out, lhsT, rhs, start=start, stop=stop, r=r, w=w, **kw)

    def act(self, out, in_, func, r=(), w=(), **kw):
        return self.add('scalar', 'activation', out, in_, func, r=r, w=w, **kw)

    def emit(self):
        nc = self.nc
        es = self.es
        sem = {e: es.enter_context(nc.semaphore('s_' + e)) for e in ENGS}
        bsem = es.enter_context(nc.semaphore('s_bar'))
        dsems = {}
        for e in ENGS:
            if any(o.dma for o in self.ops[e]):
                dsems[e] = [es.enter_context(nc.semaphore('d_%s_%d' % (e, i))) for i in range(self.ndma)]
        for e in ENGS:
            for o in self.ops[e]:
                for d in o.deps:
                    if not d.dma:
                        d.signal = True
        for e in ENGS:
            c = 0
            j = 0
            for o in self.ops[e]:
                if o.fn is None:
                    continue
                if o.dma:
                    o.dsem = dsems[e][j % self.ndma]
                    o.dval = 16 * (j // self.ndma + 1)
                    o.pre = (o.dsem, o.dval - 16)
                    j += 1
                elif o.signal:
                    c += 1
                    o.sigval = c
        self.sig_counts = {e: sum(1 for o in self.ops[e] if o.signal) for e in ENGS}
        block = es.enter_context(nc.Block())
        all_dma_final = {}
        for e in ENGS:
            for o in self.ops[e]:
                if o.dma:
                    all_dma_final[id(o.dsem)] = (o.dsem, max(o.dval, all_dma_final.get(id(o.dsem), (None, 0))[1]))

        def run(e, engobj):
            waited = {}

            def wait(s, v):
                if v <= 0:
                    return
                if waited.get(id(s), 0) >= v:
                    return
                waited[id(s)] = v
                engobj.wait_ge(s, v)

            mydma = {}
            for o in self.ops[e]:
                if o.fn is None:
                    for s_, v_ in mydma.values():
                        wait(s_, v_)
                    engobj.drain().then_inc(bsem, 1)
                    engobj.wait_ge(bsem, len(ENGS) * o.pre[1])
                    continue
                if o.dma:
                    mydma[id(o.dsem)] = (o.dsem, o.dval)
                for d in o.deps:
                    if d.dma:
                        wait(d.dsem, d.dval)
                    else:
                        wait(sem[d.eng], d.sigval)
                if o.dma:
                    wait(*o.pre)
                    o.fn(engobj).then_inc(o.dsem, 16)
                else:
                    ins = o.fn(engobj)
                    if o.signal:
                        ins.then_inc(sem[e], 1)
            if e == 'sync':
                for s, v in all_dma_final.values():
                    wait(s, v)

        @block.sync
        def _(eng):
            run('sync', eng)

        @block.tensor
        def _(eng):
            run('tensor', eng)

        @block.vector
        def _(eng):
            run('vector', eng)

        @block.scalar
        def _(eng):
            run('scalar', eng)

        @block.gpsimd
        def _(eng):
            run('gpsimd', eng)

        es.close()


D = 2048
T = 4096
NT = 512
S = 4096
EPS = 1e-6
DFF = 5632
NEGM = -30000.0


class Ring:
    def __init__(self, p, name, n, shape, dt):
        self.t = [p.sb("%s%d" % (name, i), shape, dt) for i in range(n)]
        self.k = [(name, i) for i in range(n)]
        self.i = 0

    def next(self):
        j = self.i % len(self.t)
        self.i += 1
        return self.t[j], self.k[j]


class PRing:
    def __init__(self, p, name, n, shape=(128, NT)):
        self.t = [p.ps("%s%d" % (name, i), list(shape)) for i in range(n)]
        self.k = [(name, i) for i in range(n)]
        self.i = 0

    def next(self):
        j = self.i % len(self.t)
        self.i += 1
        return self.t[j], self.k[j]


class SubRing:
    def __init__(self, ring, idx):
        self.t = [ring.t[i] for i in idx]
        self.k = [ring.k[i] for i in idx]
        self.i = 0

    def next(self):
        j = self.i % len(self.t)
        self.i += 1
        return self.t[j], self.k[j]


def wview(t, kc, ncol):
    return t[:, 0:kc * ncol].rearrange("p (k o) -> p k o", o=ncol)


def rms_rstd(p, xs_list, rkeys, sqr, ps_ss, pk, onesf, rstd, n):
    m = len(xs_list)
    for i, xa in enumerate(xs_list):
        sq, sk = sqr.next()
        p.act(sq[:, 0:n], xa, AF.Square, r=rkeys, w=[sk])
        p.mm(ps_ss[:, 0:n], onesf[:], sq[:, 0:n], start=(i == 0), stop=(i == m - 1), r=[sk, 'onesf'], w=[pk])
    rstd_from_ss(p, ps_ss, pk, rstd, n)


def rstd_from_ss(p, ps_ss, pk, rstd, n):
    p.add('vector', 'tensor_scalar', rstd[:, 0:n], ps_ss[:, 0:n], 1.0 / D, EPS, ALU.mult, ALU.add, r=[pk], w=['rstd'])
    p.act(rstd[:, 0:n], rstd[:, 0:n], AF.Sqrt, r=['rstd'], w=['rstd'])
    p.add('vector', 'reciprocal', rstd[:, 0:n], rstd[:, 0:n], r=['rstd'], w=['rstd'])


def emit_A(p, E, l):
    xT = E['xcur']; g = E['g_pre_mix'][l]; w_in = E['w_in'][l]
    uP = E['uP']; qT = E['qT']; kT = E['kT']; v = E['v']; sg = E['sg']
    p.push_scope()
    onesf = p.sb("onesf", [128, 128], F32)
    p.add('vector', 'memset', onesf[:], 1.0, w=['onesf'])
    gt = p.sb("gt", [128, 16], F32)
    p.dma(gt[:], g, w=['gt'])
    xs = p.sb("xs", [128, 16, NT], F32)
    h = p.sb("h", [128, 16, 2048], BF16)
    sqr = Ring(p, "sq", 2, [128, NT], F32)
    rstd = p.sb("rstd", [128, NT], F32)
    wr = Ring(p, "w", 4, [128, 16 * 256], BF16)
    evf = Ring(p, "evf", 3, [128, NT], F32)
    evb = Ring(p, "evb", 3, [128, NT], BF16)
    ubr = Ring(p, "ub", 2, [128, 8, 64], BF16)
    ps_ss = E['pb'].t[7]
    pm = SubRing(E['pb'], [0, 1, 2, 3])
    w3 = w_in.rearrange("(kc p) o -> p kc o", p=128)
    xT3 = xT.rearrange("(kc p) t -> p kc t", p=128)
    tog = [0]

    def evac(out, in_, r, w):
        tog[0] ^= 1
        if tog[0]:
            p.add('vector', 'tensor_copy', out, in_, r=r, w=w)
        else:
            p.add('scalar', 'copy', out, in_, r=r, w=w)

    ST = 2048
    for st in range(T // ST):
        for sub in range(ST // NT):
            t0 = st * ST + sub * NT
            p.dma(xs[:], xT3[:, :, t0:t0 + NT], w=['xs'])
            rms_rstd(p, [xs[:, kc, :] for kc in range(16)], ['xs'], sqr, ps_ss, E['pb'].k[7], onesf, rstd, NT)
            for kc in range(16):
                p.add('vector', 'scalar_tensor_tensor', h[:, kc, sub * NT:(sub + 1) * NT], xs[:, kc, :], gt[:, kc:kc + 1], rstd[:],
                      ALU.mult, ALU.mult, r=['xs', 'gt', 'rstd'], w=['h'])
        for cg in range(32):
            wt, wk = wr.next()
            w4 = wview(wt, 16, 256)
            p.dma(w4, w3[:, :, cg * 256:(cg + 1) * 256], w=[wk], eng='gpsimd')
            if 12 <= cg < 16:
                for ts in range(ST // 128):
                    ps, pk = pm.next()
                    for kc in range(16):
                        p.mm(ps[:, 0:256], h[:, kc, ts * 128:(ts + 1) * 128], w4[:, kc, :], start=(kc == 0),
                             stop=(kc == 15), r=['h', wk], w=[pk])
                    eb, ek = evb.next()
                    evac(eb[:, 0:256], ps[:, 0:256], [pk], [ek])
                    r0 = st * ST + ts * 128
                    p.dma(v[r0:r0 + 128, (cg - 12) * 256:(cg - 11) * 256], eb[:, 0:256], r=[ek])
                continue
            for o2 in range(2):
                oc = cg * 2 + o2
                for sub in range(ST // NT):
                    t0 = st * ST + sub * NT
                    tt = t0 // NT
                    ps, pk = pm.next()
                    for kc in range(16):
                        p.mm(ps[:], w4[:, kc, o2 * 128:(o2 + 1) * 128], h[:, kc, sub * NT:(sub + 1) * NT], start=(kc == 0),
                             stop=(kc == 15), r=['h', wk], w=[pk])
                    if oc < 8:
                        ub, uk = ubr.next()
                        p.add('vector', 'tensor_copy', ub[:].rearrange("p s c -> p c s"),
                              ps[:].rearrange("p (c s) -> p c s", s=8), r=[pk], w=[uk])
                        for gl in range(8):
                            gg = oc * 8 + gl
                            p.dma(uP[gg].rearrange("(s m) c -> m s c", m=16)[:, :, tt * 64:(tt + 1) * 64],
                                  ub[gl * 16:(gl + 1) * 16, :, :], r=[uk])
                    elif oc < 24:
                        eb, ek = evb.next()
                        evac(eb[:], ps[:], [pk], [ek])
                        dst = qT if oc < 16 else kT
                        c = (oc - 8) % 8
                        p.dma(dst[c * 128:(c + 1) * 128, t0:t0 + NT], eb[:], r=[ek])
                    else:
                        ef, fk = evf.next()
                        p.act(ef[:], ps[:], AF.Sigmoid, r=[pk], w=[fk])
                        c = oc - 32
                        p.dma(sg[c * 128:(c + 1) * 128, t0:t0 + NT], ef[:], r=[fk])
    p.pop_scope()


NG = 32
NH = 4
KK_EXP = [0, -1, -2, -3, -4, -5, -6, -7] + list(range(8)) + [7, 6, 5, 4, 3, 2, 1, 0] + list(range(1, 9)) + [8]
I32 = mybir.dt.int32
TWO_PI = 6.283185307179586
ATT_SCALE = 1.0 / (128 ** 0.5)


def emit_B(p, E, l):
    uP_all = E['uP']; qT = E['qT']; kT = E['kT']; vv = E['v']; yP_all = E['yP']; aT = E['aT']
    c_identf = E['identf']; c_identb = E['identb']; c_tmask = E['tmask']; c_sel = E['selE']
    c_causal = E['causal']; c_pastm = E['pastm']; kk = E['kk']
    p.push_scope()

    def ld(name, src, shape, dt=F32, eng='sync'):
        t = p.sb(name, shape, dt)
        p.dma(t[:], src, w=[name], eng=eng)
        return t

    identf = ld("identf_t", c_identf, [128, 128]); identb = ld("identb_t", c_identb, [128, 128], BF16)
    tmask = ld("tmask_t", c_tmask, [128, 128]); selE = ld("selE_t", c_sel, [16, 16, 128], BF16)
    causal = ld("causal_t", c_causal, [128, 2, 256], BF16); pastm = ld("pastm_t", c_pastm, [128, 16, 16])
    onesb = p.sb("onesb", [128, 128], BF16)
    p.add('vector', 'memset', onesb[:], 1.0, w=['onesb'])
    pb = E['pb']

    for gh in range(2):
        sp = E['ssm'][l][gh]
        lamre = sp['lamre']; lamim = sp['lamim']; ldt = sp['ldt']; bre = sp['bre']; bim = sp['bim']
        cre = sp['cre']; cim = sp['cim']; dsk = sp['dsk']
        uP = uP_all[gh * NG:(gh + 1) * NG]; yP = yP_all[gh * NG:(gh + 1) * NG]
        p.push_scope()
        TT = p.sb("TT", [128, NG, 128], BF16); P1 = p.sb("P1", [128, NG, 128], BF16)
        QQre = p.sb("QQre", [64, NG, 128], BF16); QQnim = p.sb("QQnim", [64, NG, 128], BF16)
        C1 = p.sb("C1", [64, 64], F32); C2 = p.sb("C2", [64, 64], F32)
        p.push_scope()
        t_lre = ld("t_lre", lamre, [64, NG]); t_lim = ld("t_lim", lamim, [64, NG]); t_ldt = ld("t_ldt", ldt, [64, NG])
        t_bre = ld("t_bre", bre, [64, NG, 16]); t_bim = ld("t_bim", bim, [64, NG, 16])
        t_cre = ld("t_cre", cre, [64, NG, 16]); t_cim = ld("t_cim", cim, [64, NG, 16])
        t_dsk = ld("t_dsk", dsk, [128, NG]); t_kk = ld("t_kk", kk, [64, 33])
        V = 'vector'
        sh3 = [64, NG, 33]
        dt_ = p.sb("dt_", [64, NG], F32); ar = p.sb("ar", [64, NG], F32); ai = p.sb("ai", [64, NG], F32)
        p.act(dt_[:], t_ldt[:], AF.Exp, r=['t_ldt'], w=['dt_'])
        p.add(V, 'tensor_tensor', ar[:], t_lre[:], dt_[:], ALU.mult, r=['t_lre', 'dt_'], w=['ar'])
        p.add(V, 'tensor_tensor', ai[:], t_lim[:], dt_[:], ALU.mult, r=['t_lim', 'dt_'], w=['ai'])
        th = p.sb("th", sh3, F32); mag = p.sb("mag", sh3, F32)
        pwre = p.sb("pwre", sh3, F32); pwim = p.sb("pwim", sh3, F32)
        ni = p.sb("ni", sh3, I32); nf = p.sb("nf", sh3, F32); yy = p.sb("yy", sh3, F32)
        kkb = t_kk[:].unsqueeze(1).to_broadcast(sh3)
        p.add(V, 'tensor_tensor', mag[:], ar[:].unsqueeze(2).to_broadcast(sh3), kkb, ALU.mult, r=['ar', 't_kk'], w=['mag'])
        p.act(mag[:], mag[:], AF.Exp, r=['mag'], w=['mag'])
        p.add(V, 'tensor_tensor', th[:], ai[:].unsqueeze(2).to_broadcast(sh3), kkb, ALU.mult, r=['ai', 't_kk'], w=['th'])

        def sin_of(dst, dkey, off):
            p.add(V, 'tensor_scalar', yy[:], th[:], 1.0 / TWO_PI, 32.5 + off, ALU.mult, ALU.add, r=['th'], w=['yy'])
            p.add(V, 'tensor_copy', ni[:], yy[:], r=['yy'], w=['ni'])
            p.add(V, 'tensor_copy', nf[:], ni[:], r=['ni'], w=['nf'])
            p.add(V, 'tensor_tensor', yy[:], yy[:], nf[:], ALU.subtract, r=['yy', 'nf'], w=['yy'])
            p.add(V, 'scalar_tensor_tensor', nf[:], yy[:], 0.0, yy[:], ALU.is_lt, ALU.add, r=['yy'], w=['nf'])
            p.add(V, 'tensor_scalar', nf[:], nf[:], TWO_PI, -TWO_PI / 2, ALU.mult, ALU.add, r=['nf'], w=['nf'])
            p.act(nf[:], nf[:], AF.Sin, r=['nf'], w=['nf'])
            p.add(V, 'tensor_tensor', dst[:], nf[:], mag[:], ALU.mult, r=['nf', 'mag'], w=[dkey])

        sin_of(pwim, 'pwim', 0.0)
        sin_of(pwre, 'pwre', 0.25)
        nr = p.sb("nr", [64, NG], F32); dd = p.sb("dd", [64, NG], F32); t0_ = p.sb("t0_", [64, NG], F32)
        qre = p.sb("qre", [64, NG], F32); qim = p.sb("qim", [64, NG], F32)
        p.add(V, 'tensor_scalar', nr[:], pwre[:, :, 9], -1.0, None, ALU.add, r=['pwre'], w=['nr'])
        nim = pwim[:, :, 9]
        p.add(V, 'tensor_tensor', dd[:], t_lre[:], t_lre[:], ALU.mult, r=['t_lre'], w=['dd'])
        p.add(V, 'tensor_tensor', t0_[:], t_lim[:], t_lim[:], ALU.mult, r=['t_lim'], w=['t0_'])
        p.add(V, 'tensor_tensor', dd[:], dd[:], t0_[:], ALU.add, r=['dd', 't0_'], w=['dd'])
        p.add(V, 'reciprocal', dd[:], dd[:], r=['dd'], w=['dd'])
        p.add(V, 'tensor_tensor', qre[:], nr[:], t_lre[:], ALU.mult, r=['nr', 't_lre'], w=['qre'])
        p.add(V, 'tensor_tensor', t0_[:], nim, t_lim[:], ALU.mult, r=['pwim', 't_lim'], w=['t0_'])
        p.add(V, 'tensor_tensor', qre[:], qre[:], t0_[:], ALU.add, r=['qre', 't0_'], w=['qre'])
        p.add(V, 'tensor_tensor', qre[:], qre[:], dd[:], ALU.mult, r=['qre', 'dd'], w=['qre'])
        p.add(V, 'tensor_tensor', qim[:], nim, t_lre[:], ALU.mult, r=['pwim', 't_lre'], w=['qim'])
        p.add(V, 'tensor_tensor', t0_[:], nr[:], t_lim[:], ALU.mult, r=['nr', 't_lim'], w=['t0_'])
        p.add(V, 'tensor_tensor', qim[:], qim[:], t0_[:], ALU.subtract, r=['qim', 't0_'], w=['qim'])
        p.add(V, 'tensor_tensor', qim[:], qim[:], dd[:], ALU.mult, r=['qim', 'dd'], w=['qim'])
        sh16 = [64, NG, 16]
        bbre = p.sb("bbre", sh16, F32); bbim = p.sb("bbim", sh16, F32); t16 = p.sb("t16", sh16, F32)
        qreb = qre[:].unsqueeze(2).to_broadcast(sh16); qimb = qim[:].unsqueeze(2).to_broadcast(sh16)
        p.add(V, 'tensor_tensor', bbre[:], t_bre[:], qreb, ALU.mult, r=['t_bre', 'qre'], w=['bbre'])
        p.add(V, 'tensor_tensor', t16[:], t_bim[:], qimb, ALU.mult, r=['t_bim', 'qim'], w=['t16'])
        p.add(V, 'tensor_tensor', bbre[:], bbre[:], t16[:], ALU.subtract, r=['bbre', 't16'], w=['bbre'])
        p.add(V, 'tensor_tensor', bbim[:], t_bim[:], qreb, ALU.mult, r=['t_bim', 'qre'], w=['bbim'])
        p.add(V, 'tensor_tensor', t16[:], t_bre[:], qimb, ALU.mult, r=['t_bre', 'qim'], w=['t16'])
        p.add(V, 'tensor_tensor', bbim[:], bbim[:], t16[:], ALU.add, r=['bbim', 't16'], w=['bbim'])
        sh4 = [64, NG, 8, 16]
        LL = p.sb("LL", [128, NG, 128], BF16); RR = p.sb("RR", [128, NG, 128], BF16); PP = p.sb("PP", [128, NG, 128], BF16)
        imtmp = p.sb("imtmp", [64, NG, 128], BF16)
        ta = p.sb("ta", sh4, F32); tb = p.sb("tb", sh4, F32)

        def cprod(i0, vre, vim, vkeys, out_re, rekey, out_im, imkey, neg_im):
            pr = pwre[:, :, i0:i0 + 8].unsqueeze(3).to_broadcast(sh4)
            pi_ = pwim[:, :, i0:i0 + 8].unsqueeze(3).to_broadcast(sh4)
            vr = vre[:].unsqueeze(2).to_broadcast(sh4)
            vi = vim[:].unsqueeze(2).to_broadcast(sh4)
            o_re = out_re.rearrange("p g (j m) -> p g j m", m=16)
            o_im = out_im.rearrange("p g (j m) -> p g j m", m=16)
            p.add(V, 'tensor_tensor', ta[:], pr, vr, ALU.mult, r=['pwre'] + vkeys, w=['ta'])
            p.add('gpsimd', 'tensor_tensor', tb[:], pi_, vi, ALU.mult, r=['pwim'] + vkeys, w=['tb'])
            p.add(V, 'tensor_tensor', o_re, ta[:], tb[:], ALU.subtract, r=['ta', 'tb'], w=[rekey])
            p.add(V, 'tensor_tensor', ta[:], pr, vi, ALU.mult, r=['pwre'] + vkeys, w=['ta'])
            p.add('gpsimd', 'tensor_tensor', tb[:], pi_, vr, ALU.mult, r=['pwim'] + vkeys, w=['tb'])
            if neg_im:
                p.add(V, 'scalar_tensor_tensor', o_im, ta[:], -1.0, tb[:], ALU.mult, ALU.subtract, r=['ta', 'tb'], w=[imkey])
            else:
                p.add(V, 'tensor_tensor', o_im, ta[:], tb[:], ALU.add, r=['ta', 'tb'], w=[imkey])

        cprod(0, bbre, bbim, ['bbre', 'bbim'], LL[0:64], 'LL', imtmp[:], 'imtmp', False)
        p.dma(LL[64:128], imtmp[:], r=['imtmp'], w=['LL'])
        cprod(8, t_cre, t_cim, ['t_cre', 't_cim'], RR[0:64], 'RR', imtmp[:], 'imtmp', True)
        p.dma(RR[64:128], imtmp[:], r=['imtmp'], w=['RR'])
        cprod(16, bbre, bbim, ['bbre', 'bbim'], PP[0:64], 'PP', imtmp[:], 'imtmp', False)
        p.dma(PP[64:128], imtmp[:], r=['imtmp'], w=['PP'])
        cprod(24, t_cre, t_cim, ['t_cre', 't_cim'], QQre[:], 'QQre', QQnim[:], 'QQnim', True)
        p.add(V, 'tensor_copy', C1[:, 0:32], pwre[:, :, 32], r=['pwre'], w=['C1'])
        p.add(V, 'tensor_copy', C1[:, 32:64], pwre[:, :, 32], r=['pwre'], w=['C1'])
        p.add(V, 'tensor_scalar', C2[:, 0:32], pwim[:, :, 32], -1.0, None, ALU.mult, r=['pwim'], w=['C2'])
        p.add(V, 'tensor_copy', C2[:, 32:64], pwim[:, :, 32], r=['pwim'], w=['C2'])
        tfr = Ring(p, "tfr", 2, [128, 4, 128], F32)
        for g0 in range(0, NG, 4):
            ps, pk = pb.next()
            for gl in range(4):
                p.mm(ps[:, gl * 128:(gl + 1) * 128], LL[:, g0 + gl, :], RR[:, g0 + gl, :], r=['LL', 'RR'], w=[pk])
            tf, tk = tfr.next()
            p.add(V, 'tensor_tensor', tf[:], ps[:].rearrange("p (g c) -> p g c", c=128),
                  tmask[:].unsqueeze(1).to_broadcast([128, 4, 128]), ALU.mult, r=[pk, 'tmask_t'], w=[tk])
            for gl in range(4):
                p.add(V, 'scalar_tensor_tensor', TT[:, g0 + gl, :], identf[:], t_dsk[:, g0 + gl:g0 + gl + 1], tf[:, gl, :],
                      ALU.mult, ALU.add, r=['identf_t', 't_dsk', tk], w=['TT'])
            ps2, pk2 = pb.next()
            for gl in range(4):
                p.mm(ps2[:, gl * 128:(gl + 1) * 128], PP[:, g0 + gl, :], identb[:], r=['PP', 'identb_t'], w=[pk2])
            p.add('scalar', 'copy', P1[:, g0:g0 + 4, :], ps2[:].rearrange("p (g c) -> p g c", c=128), r=[pk2], w=['P1'])

        p.pop_scope()
        p.push_scope()
        U = p.sb("U", [128, NG, 512], BF16)
        for q4 in range(4):
            p.dma(U[:, q4 * 8:(q4 + 1) * 8, :], uP[q4 * 8:(q4 + 1) * 8].rearrange("g p c -> p g c"), w=['U'])
        hist = p.sb("hist", [64, 129, 96], F32)
        Gst = p.sb("Gst", [64, 128, 64], F32)
        Hre = p.sb("Hre", [64, NG, 128], BF16); Him = p.sb("Him", [64, NG, 128], BF16)
        t1 = p.sb("t1", [64, 64], F32); t2 = p.sb("t2", [64, 64], F32); t3 = p.sb("t3", [64, 64], F32)
        ybr = Ring(p, "yb", 2, [128, 4, 128], F32)
        p.add(V, 'memset', hist[:, 0, :], 0.0, w=['histA', 'histB'])
        for seg in range(4):
            cs = slice(seg * 128, (seg + 1) * 128)
            if seg > 0:
                p.add(V, 'tensor_copy', hist[:, 0, :], hist[:, 128, :], r=['histA', 'histB'], w=['histA', 'histB'])
            for g0 in range(0, NG, 4):
                for ri in range(2):
                    ps, pk = pb.next()
                    for gl in range(4):
                        p.mm(ps[0:64, gl * 128:(gl + 1) * 128], P1[:, g0 + gl, ri * 64:(ri + 1) * 64], U[:, g0 + gl, cs],
                             r=['P1', 'U'], w=[pk])
                    dst = Gst[:, :, ri * 32 + g0:ri * 32 + g0 + 4].rearrange("p c g -> p g c")
                    src = ps[0:64, :].rearrange("p (g c) -> p g c", c=128)
                    if ri == 0:
                        p.add(V, 'tensor_copy', dst, src, r=[pk], w=['Gst'])
                    else:
                        p.add('scalar', 'copy', dst, src, r=[pk], w=['Gst'])
            for c in range(128):
                p.add(V, 'tensor_tensor', t1[:], hist[:, c, 0:64], C1[:], ALU.mult, r=['histA', 'C1'], w=['t1'])
                p.add(V, 'tensor_tensor', t2[:], hist[:, c, 32:96], C2[:], ALU.mult, r=['histA', 'histB', 'C2'], w=['t2'])
                p.add(V, 'tensor_tensor', t3[:], t1[:], t2[:], ALU.add, r=['t1', 't2'], w=['t3'])
                p.add(V, 'tensor_tensor', hist[:, c + 1, 0:64], t3[:], Gst[:, c, :], ALU.add, r=['t3', 'Gst'], w=['histA'])
                p.add(V, 'tensor_tensor', hist[:, c + 1, 64:96], t3[:, 0:32], Gst[:, c, 0:32], ALU.add,
                      r=['t3', 'Gst'], w=['histB'])
            p.add(V, 'tensor_copy', Hre[:], hist[:, 0:128, 0:32].rearrange("p c g -> p g c"), r=['histA'], w=['Hre'])
            p.add('gpsimd', 'tensor_copy', Him[:], hist[:, 0:128, 32:64].rearrange("p c g -> p g c"), r=['histA'], w=['Him'])
            for g0 in range(0, NG, 4):
                ps, pk = pb.next()
                for gl in range(4):
                    g = g0 + gl
                    o = ps[:, gl * 128:(gl + 1) * 128]
                    p.mm(o, TT[:, g, :], U[:, g, cs], start=True, stop=False, r=['TT', 'U'], w=[pk])
                    p.mm(o, QQre[:, g, :], Hre[:, g, :], start=False, stop=False, r=['QQre', 'Hre'], w=[pk])
                    p.mm(o, QQnim[:, g, :], Him[:, g, :], start=False, stop=True, r=['QQnim', 'Him'], w=[pk])
                yb, yk = ybr.next()
                p.add('scalar', 'copy', yb[:], ps[:].rearrange("p (g c) -> p g c", c=128), r=[pk], w=[yk])
                p.dma(yP[g0:g0 + 4, :, cs].rearrange("g p c -> p g c"), yb[:], r=[yk])


        p.pop_scope()
        p.pop_scope()
    Kh = p.sb("Kh", [128, S], BF16); Qh = p.sb("Qh", [128, S], BF16); Vh = p.sb("Vh", [128, 32, 128], BF16)
    ksum = p.sb("ksum", [128, 16], F32); kmh = p.sb("kmh", [128, 16], BF16); kml = p.sb("kml", [128, 16], BF16)
    kmhf = p.sb("kmhf", [128, 16], F32)
    maskT = p.sb("maskT", [16, S], BF16)
    gmr = Ring(p, "gm", 2, [128, 16], F32); mxr = Ring(p, "mx", 2, [128, 8], F32); thr_r = Ring(p, "thr", 2, [128, 1], F32)
    bir = Ring(p, "bi", 2, [128, 16], F32)
    ptr = Ring(p, "pt", 4, [128, 512], BF16)
    rlr = Ring(p, "rl", 2, [128, 512], F32); obr = Ring(p, "ob", 2, [128, 512], BF16)
    psr = SubRing(pb, [0, 1, 2]); por = SubRing(pb, [3, 4]); plr = SubRing(pb, [5, 6]); pgr = SubRing(pb, [7])
    for hh in range(8):
        hs = slice(hh * 128, (hh + 1) * 128)
        p.dma(Kh[:], kT[hs, :], w=['Kh'])
        p.dma(Qh[:], qT[hs, :], w=['Qh'])
        p.dma(Vh[:], vv[:, hs].rearrange("(kt p) d -> p kt d", p=128), w=['Vh'])
        p.add(V, 'tensor_reduce', ksum[:], Kh[:].rearrange("p (j l) -> p j l", l=256), AX.X, ALU.add, r=['Kh'], w=['ksum'])
        p.add(V, 'tensor_scalar', ksum[:], ksum[:], 1.0 / 256, None, ALU.mult, r=['ksum'], w=['ksum'])
        p.add(V, 'tensor_copy', kmh[:], ksum[:], r=['ksum'], w=['kmh'])
        p.add(V, 'tensor_copy', kmhf[:], kmh[:], r=['kmh'], w=['kmhf'])
        p.add(V, 'tensor_tensor', kmhf[:], ksum[:], kmhf[:], ALU.subtract, r=['ksum', 'kmhf'], w=['kmhf'])
        p.add(V, 'tensor_copy', kml[:], kmhf[:], r=['kmhf'], w=['kml'])
        for qs in range(32):
            n = qs // 2
            ps, pk = pgr.next()
            p.mm(ps[:, 0:16], Qh[:, qs * 128:(qs + 1) * 128], kmh[:], start=True, stop=False, r=['Qh', 'kmh'], w=[pk])
            p.mm(ps[:, 0:16], Qh[:, qs * 128:(qs + 1) * 128], kml[:], start=False, stop=True, r=['Qh', 'kml'], w=[pk])
            gm, gk = gmr.next(); mx, mk = mxr.next(); th_, tk_ = thr_r.next(); bi, bk = bir.next()
            p.add(V, 'tensor_tensor', gm[:], ps[:, 0:16], pastm[:, n, :], ALU.add, r=[pk, 'pastm_t'], w=[gk])
            p.add(V, 'max', mx[:], gm[:], r=[gk], w=[mk])
            p.add(V, 'tensor_scalar_max', th_[:], mx[:, 2:3], -10000.0, r=[mk], w=[tk_])
            p.add(V, 'tensor_scalar', bi[:], gm[:], th_[:, 0:1], -NEGM, ALU.is_ge, ALU.mult, r=[gk, tk_], w=[bk])
            p.add(V, 'tensor_scalar', bi[:], bi[:], NEGM, None, ALU.add, r=[bk], w=[bk])
            p.add(V, 'memset', bi[:, n:n + 1], 0.0, w=[bk])
            ps2, pk2 = psr.next()
            p.mm(ps2[0:16, 0:128], bi[:], identf[:], r=[bk, 'identf_t'], w=[pk2])
            p.add('scalar', 'copy', maskT[:, qs * 128:(qs + 1) * 128], ps2[0:16, 0:128], r=[pk2], w=['maskT'])
        for np_ in range(8):
            n0 = 2 * np_
            qsl = slice(n0 * 256, (n0 + 2) * 256)
            po, pok = por.next()
            pl, plk = plr.next()
            nkc = 2 * n0 + 2
            tot = nkc + 2
            pend = []

            def stage1(kt):
                j = kt // 2
                cs_ = slice(0, 512) if kt < nkc else slice(256, 512)
                qcols = slice(n0 * 256, (n0 + 2) * 256) if kt < nkc else slice((n0 + 1) * 256, (n0 + 2) * 256)
                ps, pk = psr.next()
                p.mm(ps[:, cs_], Kh[:, kt * 128:(kt + 1) * 128], Qh[:, qcols], start=True, stop=False, r=['Kh', 'Qh'], w=[pk])
                p.mm(ps[:, cs_], selE[:, j, :], maskT[:, qcols], start=False, stop=(j < n0), r=['selE_t', 'maskT'], w=[pk])
                dcol = slice(0, 256) if j == n0 else slice(256, 512)
                if j >= n0:
                    p.mm(ps[:, dcol], identb[:], causal[:, kt % 2, :], start=False, stop=True, r=['identb_t', 'causal_t'], w=[pk])
                pt, ptk = ptr.next()
                p.act(pt[:, cs_], ps[:, cs_], AF.Exp, scale=ATT_SCALE, r=[pk], w=[ptk])
                pend.append((kt, cs_, pt, ptk))

            def stage2():
                kt, cs_, pt, ptk = pend.pop(0)
                p.mm(po[:, cs_], Vh[:, kt, :], pt[:, cs_], start=(kt == 0), stop=(kt == tot - 1), r=['Vh', ptk], w=[pok])
                p.mm(pl[:, cs_], onesb[:], pt[:, cs_], start=(kt == 0), stop=(kt == tot - 1), r=['onesb', ptk], w=[plk])

            for kt in range(tot):
                stage1(kt)
                if len(pend) > 2:
                    stage2()
            while pend:
                stage2()
            rl, rk = rlr.next(); ob, ok_ = obr.next()
            p.add(V, 'reciprocal', rl[:], pl[:], r=[plk], w=[rk])
            p.add(V, 'tensor_tensor', ob[:], po[:], rl[:], ALU.mult, r=[pok, rk], w=[ok_])
            p.dma(aT[hs, qsl], ob[:], r=[ok_])
    p.pop_scope()


def _bf(a):
    return np.ascontiguousarray(a).astype(ml_dtypes.bfloat16)


def consts_B():
    sm = np.arange(128) // 16
    tmask = (sm[None, :] >= sm[:, None]).astype(np.float32)
    selE = np.zeros((16, 16, 128), np.float32)
    for j in range(16):
        selE[j, j, :] = 1.0
    k = np.arange(128)[:, None, None]
    a = np.arange(2)[None, :, None]
    q = np.arange(256)[None, None, :]
    causal = np.where(a * 128 + k <= q, 0.0, NEGM).astype(np.float32)
    n = np.arange(16)[None, :, None]
    j = np.arange(16)[None, None, :]
    pastm = np.broadcast_to(np.where(j < n, 0.0, NEGM), (128, 16, 16)).astype(np.float32)
    return {
        "identf": np.eye(128, dtype=np.float32), "identb": _bf(np.eye(128, dtype=np.float32)),
        "tmask": tmask, "selE": _bf(selE), "causal": _bf(causal), "pastm": np.ascontiguousarray(pastm),
        "kk": np.ascontiguousarray(np.tile(np.array(KK_EXP, np.float32), (64, 1))),
    }


def ssm_params_B(inp, l, gh):
    gs = slice(gh * NG, (gh + 1) * NG)
    c = np.ascontiguousarray
    return {
        "lamre": c(inp['lam_re'][l][gs].T), "lamim": c(inp['lam_im'][l][gs].T),
        "ldt": c(np.broadcast_to(inp['log_dt'][l][gs][None, :], (64, NG))),
        "bre": c(inp['b_re'][l][gs].transpose(1, 0, 2)), "bim": c(inp['b_im'][l][gs].transpose(1, 0, 2)),
        "cre": c(inp['c_re'][l][gs].transpose(2, 0, 1)), "cim": c(inp['c_im'][l][gs].transpose(2, 0, 1)),
        "dsk": c(np.tile(inp['d_skip'][l].reshape(64, 16)[gs].T, (8, 1))),
    }


def _glay(gv):
    return np.ascontiguousarray(np.asarray(gv, np.float32).reshape(16, 128).T)


def emit_C(p, E, l):
    yP = E['yP']; aT = E['aT']; sg = E['sg']; xT = E['xcur']
    w_glu = E['w_glu'][l]; w_us = E['w_up_ssm'][l]; w_ua = E['w_up_attn'][l]; w_out = E['w_out'][l]
    g1 = E['g_post_mix'][l]; g2 = E['g_pre_ffn'][l]
    xm_o = E['xm']; hf_o = E['hfx'][:, 2:T + 2]
    p.push_scope()
    V = 'vector'; G = 'gpsimd'
    onesf = p.sb("onesf", [128, 128], F32)
    p.add(V, 'memset', onesf[:], 1.0, w=['onesf'])
    g1t = p.sb("g1t", [128, 16], F32); g2t = p.sb("g2t", [128, 16], F32)
    p.dma(g1t[:], g1, w=['g1t']); p.dma(g2t[:], g2, w=['g2t'])
    ybr = Ring(p, "ybuf", 2, [128, 8, 64], F32)
    yall = p.sb("yall", [128, 8, NT], F32); yb = p.sb("ybb", [128, 8, NT], BF16); y2 = p.sb("y2", [128, 8, NT], BF16)
    at = p.sb("at", [128, 8, NT], BF16); mb = p.sb("mb", [128, 16, NT], BF16)
    mo = p.sb("mo", [128, 16, NT], F32); xs = p.sb("xs", [128, 16, NT], F32); hfb = p.sb("hfb", [128, 16, NT], BF16)
    tr = Ring(p, "tmp", 4, [128, NT], F32)
    sgr = Ring(p, "sgt", 4, [128, NT], F32)
    sqr = Ring(p, "sq", 2, [128, NT], F32)
    rstd = p.sb("rstd", [128, NT], F32)
    wr = Ring(p, "w", 4, [128, 16 * 256], BF16)
    ps_ss = E['pb'].t[7]
    pm = SubRing(E['pb'], [0, 1, 2, 3, 4, 5])
    xT3 = xT.rearrange("(kc p) t -> p kc t", p=128)
    aT3 = aT.rearrange("(kc p) t -> p kc t", p=128)

    def wload(wd, kc, cg):
        wt, wk = wr.next()
        w4 = wview(wt, kc, 256)
        p.dma(w4, wd.rearrange("(kc p) o -> p kc o", p=128)[:, :, cg * 256:(cg + 1) * 256], w=[wk], eng=G)
        return w4, wk

    for tt in range(T // NT):
        t0 = tt * NT
        ts = slice(t0, t0 + NT)
        for kc in range(8):
            ybf, yk = ybr.next()
            for gl in range(8):
                p.dma(ybf[gl * 16:(gl + 1) * 16, :, :],
                      yP[kc * 8 + gl].rearrange("(t m) c -> m t c", m=16)[:, :, tt * 64:(tt + 1) * 64], w=[yk])
            xv = ybf[:].rearrange("p t c -> p c t")
            a, ak = tr.next()
            av = a[:].rearrange("p (c t) -> p c t", t=8)
            p.add(V, 'tensor_tensor', av, xv, xv, ALU.mult, r=[yk], w=[ak])
            p.add(V, 'tensor_scalar', a[:], a[:], 0.044715, 1.0, ALU.mult, ALU.add, r=[ak], w=[ak])
            p.add(V, 'tensor_tensor', av, av, xv, ALU.mult, r=[ak, yk], w=[ak])
            p.act(a[:], a[:], AF.Sigmoid, scale=1.5957691216057308, r=[ak], w=[ak])
            p.add(V, 'tensor_tensor', yall[:, kc, :].rearrange("p (c t) -> p c t", t=8), av, xv, ALU.mult, r=[ak, yk], w=['yall'])
            p.add(V, 'tensor_copy', yb[:, kc, :], yall[:, kc, :], r=['yall'], w=['yb'])
        for cg in range(4):
            w4, wk = wload(w_glu, 8, cg)
            for o2 in range(2):
                oc = cg * 2 + o2
                ps, pk = pm.next()
                for kc in range(8):
                    p.mm(ps[:], w4[:, kc, o2 * 128:(o2 + 1) * 128], yb[:, kc, :], start=(kc == 0), stop=(kc == 7), r=[wk, 'yb'], w=[pk])
                s_, sk = tr.next()
                p.act(s_[:], ps[:], AF.Sigmoid, r=[pk], w=[sk])
                p.add(V, 'tensor_tensor', y2[:, oc, :], yall[:, oc, :], s_[:], ALU.mult, r=['yall', sk], w=['y2'])
        p.dma(at[:], aT3[:, :, ts], w=['at'])
        for cg in range(8):
            w1, k1 = wload(w_us, 8, cg)
            w2, k2 = wload(w_ua, 8, cg)
            for o2 in range(2):
                oc = cg * 2 + o2
                osl = slice(o2 * 128, (o2 + 1) * 128)
                sa, sak = sgr.next(); sb_, sbk = sgr.next()
                p.dma(sa[:], sg[oc * 128:(oc + 1) * 128, ts], w=[sak])
                p.dma(sb_[:], sg[2048 + oc * 128:2048 + (oc + 1) * 128, ts], w=[sbk])
                ps1, pk1 = pm.next(); ps2, pk2 = pm.next()
                for kc in range(8):
                    p.mm(ps1[:], w1[:, kc, osl], y2[:, kc, :], start=(kc == 0), stop=(kc == 7), r=[k1, 'y2'], w=[pk1])
                for kc in range(8):
                    p.mm(ps2[:], w2[:, kc, osl], at[:, kc, :], start=(kc == 0), stop=(kc == 7), r=[k2, 'at'], w=[pk2])
                m1, mk1 = tr.next(); m2, mk2 = tr.next()
                p.add(V, 'tensor_tensor', m1[:], ps1[:], sa[:], ALU.mult, r=[pk1, sak], w=[mk1])
                p.add(V, 'tensor_tensor', m2[:], ps2[:], sb_[:], ALU.mult, r=[pk2, sbk], w=[mk2])
                p.add(V, 'tensor_tensor', mb[:, oc, :], m1[:], m2[:], ALU.add, r=[mk1, mk2], w=['mb'])
        p.dma(xs[:], xT3[:, :, ts], w=['xs'])
        for cg in range(8):
            w4, wk = wload(w_out, 16, cg)
            for o2 in range(2):
                oc = cg * 2 + o2
                ps, pk = pm.next()
                for kc in range(16):
                    p.mm(ps[:], w4[:, kc, o2 * 128:(o2 + 1) * 128], mb[:, kc, :], start=(kc == 0), stop=(kc == 15), r=[wk, 'mb'], w=[pk])
                p.act(mo[:, oc, :], ps[:], AF.Copy, r=[pk], w=['mo'])
        rms_rstd(p, [mo[:, kc, :] for kc in range(16)], ['mo'], sqr, ps_ss, E['pb'].k[7], onesf, rstd, NT)
        for oc in range(16):
            p.add(V, 'scalar_tensor_tensor', mo[:, oc, :], mo[:, oc, :], g1t[:, oc:oc + 1], rstd[:], ALU.mult, ALU.mult,
                  r=['mo', 'g1t', 'rstd'], w=['mo'])
            p.add(V, 'tensor_tensor', mo[:, oc, :], mo[:, oc, :], xs[:, oc, :], ALU.add, r=['mo', 'xs'], w=['mo'])
        p.dma(xm_o.rearrange("(kc p) t -> p kc t", p=128)[:, :, ts], mo[:], r=['mo'])
        rms_rstd(p, [mo[:, kc, :] for kc in range(16)], ['mo'], sqr, ps_ss, E['pb'].k[7], onesf, rstd, NT)
        for oc in range(16):
            p.add(V, 'scalar_tensor_tensor', hfb[:, oc, :], mo[:, oc, :], g2t[:, oc:oc + 1], rstd[:], ALU.mult, ALU.mult,
                  r=['mo', 'g2t', 'rstd'], w=['hfb'])
        p.dma(hf_o.rearrange("(kc p) t -> p kc t", p=128)[:, :, ts], hfb[:], r=['hfb'])
    p.pop_scope()


def emit_D(p, E, l):
    hfx = E['hfx']; xm = E['xm']; w_up = E['w_ffn_up'][l]; w_dn = E['w_ffn_down'][l]
    cw = E['cw'][l]; cb = E['cb'][l]; g3 = E['g_post_ffn'][l]
    xo = E['xout'] if l == 1 else E['xnext']
    TS = 1024
    NH_ = TS // NT
    p.push_scope()
    V = 'vector'; G = 'gpsimd'
    onesf = p.sb("onesf", [128, 128], F32)
    p.add(V, 'memset', onesf[:], 1.0, w=['onesf'])
    g3t = p.sb("g3t", [128, 16], F32); cwt = p.sb("cwt", [128, 88, 3], F32); cbt = p.sb("cbt", [128, 88], F32)
    p.dma(g3t[:], g3, w=['g3t']); p.dma(cwt[:], cw, w=['cwt']); p.dma(cbt[:], cb, w=['cbt'])
    ztail = p.sb("ztail", [128, 88, 2], F32)
    p.add(V, 'memset', ztail[:], 0.0, w=['ztail'])
    actb = p.sb("actb", [128, 44, TS], BF16)
    ps_ss = E['pb'].t[7]
    pm = SubRing(E['pb'], [0, 1, 2, 3, 4, 5, 6])
    hfx3 = hfx.rearrange("(kc p) t -> p kc t", p=128)
    wup3 = w_up.rearrange("(kc p) o -> p kc o", p=128)
    wdn3 = w_dn.rearrange("(kc p) o -> p kc o", p=128)
    for st in range(T // TS):
        s0 = st * TS
        p.push_scope()
        hx = p.sb("hx", [128, 16, TS + 2], BF16)
        zr = Ring(p, "z", 4, [128, NT + 2], F32)
        cr = Ring(p, "cv", 4, [128, NT], F32)
        wr = Ring(p, "w", 3, [128, 2 * 16 * 256], BF16)
        p.dma(hx[:], hfx3[:, :, s0:s0 + TS + 2], w=['hx'])
        for jj in range(22):
            wt, wk = wr.next()
            w5 = wt[:].rearrange("p (a k o) -> p a k o", a=2, o=256)
            p.dma(w5[:, 0], wup3[:, :, jj * 256:(jj + 1) * 256], w=[wk], eng=G)
            p.dma(w5[:, 1], wup3[:, :, DFF + jj * 256:DFF + (jj + 1) * 256], w=[wk], eng=G)
            for o2 in range(2):
                j = jj * 2 + o2
                osl = slice(o2 * 128, (o2 + 1) * 128)
                for hb in range(NH_):
                    cres = []
                    for which in range(2):
                        ch = which * 44 + j
                        ps, pk = pm.next()
                        for kc in range(16):
                            p.mm(ps[:], w5[:, which, kc, osl], hx[:, kc, 2 + hb * NT:2 + (hb + 1) * NT], start=(kc == 0),
                                 stop=(kc == 15), r=[wk, 'hx'], w=[pk])
                        zt, zk = zr.next()
                        p.act(zt[:, 2:NT + 2], ps[:], AF.Copy, r=[pk], w=[zk])
                        p.add('scalar', 'copy', zt[:, 0:2], ztail[:, ch, :], r=['ztail'], w=[zk])
                        p.add('scalar', 'copy', ztail[:, ch, :], zt[:, NT:NT + 2], r=[zk], w=['ztail'])
                        c1, ck = cr.next()
                        p.add(V, 'tensor_scalar', c1[:], zt[:, 2:NT + 2], cwt[:, ch, 2:3], cbt[:, ch:ch + 1], ALU.mult, ALU.add,
                              r=[zk, 'cwt', 'cbt'], w=[ck])
                        p.add(V, 'scalar_tensor_tensor', c1[:], zt[:, 1:NT + 1], cwt[:, ch, 1:2], c1[:], ALU.mult, ALU.add,
                              r=[zk, 'cwt', ck], w=[ck])
                        p.add(V, 'scalar_tensor_tensor', c1[:], zt[:, 0:NT], cwt[:, ch, 0:1], c1[:], ALU.mult, ALU.add,
                              r=[zk, 'cwt', ck], w=[ck])
                        cres.append((c1, ck))
                    (ca, cak), (cv_, cvk) = cres
                    p.act(ca[:], ca[:], AF.Silu, r=[cak], w=[cak])
                    p.add(V, 'tensor_tensor', actb[:, j, hb * NT:(hb + 1) * NT], ca[:], cv_[:], ALU.mult, r=[cak, cvk], w=['actb'])
        p.pop_scope()
        p.push_scope()
        f = p.sb("f", [128, 16, TS], F32)
        xr = Ring(p, "xm", 2, [128, NT], F32)
        orr = Ring(p, "or", 2, [128, NT], F32)
        sqr = Ring(p, "sq", 2, [128, NT], F32)
        rstd = p.sb("rstd", [128, NT], F32)
        wr = Ring(p, "wd", 3, [128, 44 * 128], BF16)
        for oc in range(16):
            wt, wk = wr.next()
            w4 = wview(wt, 44, 128)
            p.dma(w4, wdn3[:, :, oc * 128:(oc + 1) * 128], w=[wk], eng=G)
            for hb in range(NH_):
                ps, pk = pm.next()
                for j in range(44):
                    p.mm(ps[:], w4[:, j, :], actb[:, j, hb * NT:(hb + 1) * NT], start=(j == 0), stop=(j == 43), r=[wk, 'actb'], w=[pk])
                p.act(f[:, oc, hb * NT:(hb + 1) * NT], ps[:], AF.Copy, r=[pk], w=['f'])
        for hb in range(NH_):
            hs_ = slice(hb * NT, (hb + 1) * NT)
            ts = slice(s0 + hb * NT, s0 + (hb + 1) * NT)
            rms_rstd(p, [f[:, kc, hs_] for kc in range(16)], ['f'], sqr, ps_ss, E['pb'].k[7], onesf, rstd, NT)
            for oc in range(16):
                xt, xk = xr.next(); ot, ok_ = orr.next()
                p.dma(xt[:], xm[oc * 128:(oc + 1) * 128, ts], w=[xk])
                p.add(V, 'scalar_tensor_tensor', ot[:], f[:, oc, hs_], g3t[:, oc:oc + 1], rstd[:], ALU.mult, ALU.mult,
                      r=['f', 'g3t', 'rstd'], w=[ok_])
                p.add(V, 'tensor_tensor', ot[:], ot[:], xt[:], ALU.add, r=[ok_, xk], w=[ok_])
                p.dma(xo[oc * 128:(oc + 1) * 128, ts], ot[:], r=[ok_])
        p.pop_scope()
    p.pop_scope()


def build_fused():
    nc = bass.Bass("TRN2", target_bir_lowering=False)
    E = {}

    def din(name, shape, dt=F32):
        return nc.dram_tensor(name, shape, dt, kind="ExternalInput").ap()

    def dscr(name, shape, dt=F32):
        return nc.dram_tensor(name, shape, dt, kind="Internal").ap()

    E['xin'] = din("xT", [D, T])
    for nm in ['g_pre_mix', 'g_post_mix', 'g_pre_ffn', 'g_post_ffn']:
        E[nm] = din(nm, [2, 128, 16])
    E['w_in'] = din("w_in", [2, D, 8192]); E['w_glu'] = din("w_glu", [2, 1024, 1024])
    E['w_up_ssm'] = din("w_up_ssm", [2, 1024, D]); E['w_up_attn'] = din("w_up_attn", [2, 1024, D])
    E['w_out'] = din("w_out", [2, D, D]); E['w_ffn_up'] = din("w_ffn_up", [2, D, 2 * DFF]); E['w_ffn_down'] = din("w_ffn_down", [2, DFF, D])
    E['cw'] = din("cw", [2, 128, 88, 3]); E['cb'] = din("cb", [2, 128, 88])
    E['ssm'] = [[{k: din("%s_%d_%d" % (k, l, gh), shp) for k, shp in
                  [('lamre', [64, NG]), ('lamim', [64, NG]), ('ldt', [64, NG]), ('bre', [64, NG, 16]), ('bim', [64, NG, 16]),
                   ('cre', [64, NG, 16]), ('cim', [64, NG, 16]), ('dsk', [128, NG])]} for gh in range(2)] for l in range(2)]
    E['kk'] = din("kk", [64, 33]); E['identf'] = din("identf", [128, 128]); E['identb'] = din("identb", [128, 128], BF16)
    E['tmask'] = din("tmask", [128, 128]); E['selE'] = din("selE", [16, 16, 128], BF16)
    E['causal'] = din("causal", [128, 2, 256], BF16); E['pastm'] = din("pastm", [128, 16, 16])
    E['xout'] = nc.dram_tensor("xo", [D, T], F32, kind="ExternalOutput").ap()
    E['xnext'] = dscr("xnext", [D, T])
    E['uP'] = dscr("uP", [64, 128, 512], BF16); E['qT'] = dscr("qT", [1024, T], BF16); E['kT'] = dscr("kT", [1024, T], BF16)
    E['v'] = dscr("v", [T, 1024], BF16); E['sg'] = dscr("sg", [4096, T]); E['yP'] = dscr("yP", [64, 128, 512])
    E['aT'] = dscr("aT", [1024, T], BF16); E['xm'] = dscr("xm", [D, T]); E['hfx'] = dscr("hfx", [D, T + 2], BF16)
    p = Prog(nc)
    E['pb'] = PRing(p, "pb", 8)
    zt = p.sb("zt", [128, 16, 2], BF16)
    p.add('vector', 'memset', zt[:], 0.0, w=['zt'])
    p.dma(E['hfx'].rearrange("(kc p) t -> p kc t", p=128)[:, :, 0:2], zt[:], r=['zt'])
    for l in range(2):
        E['xcur'] = E['xin'] if l == 0 else E['xnext']
        emit_A(p, E, l)
        emit_B(p, E, l)
        emit_C(p, E, l)
        emit_D(p, E, l)
    p.emit()
    return nc


_NC = []


def kernel(**inp):
    c_ = np.ascontiguousarray
    f32 = lambda a: c_(np.asarray(a, np.float32))
    if not _NC:
        _NC.append(build_fused())
    nc = _NC[0]
    x = f32(inp['x'])
    shared = dict(consts_B())
    for nm in ['g_pre_mix', 'g_post_mix', 'g_pre_ffn', 'g_post_ffn']:
        shared[nm] = c_(np.stack([_glay(inp[nm][l]) for l in range(2)]))
    shared['w_in'] = f32(inp['w_in']); shared['w_glu'] = f32(inp['w_glu'])
    shared['w_up_ssm'] = f32(inp['w_up_ssm']); shared['w_up_attn'] = f32(inp['w_up_attn'])
    shared['w_out'] = f32(inp['w_out']); shared['w_ffn_up'] = f32(inp['w_ffn_up']); shared['w_ffn_down'] = f32(inp['w_ffn_down'])
    shared['cw'] = c_(np.stack([f32(inp['conv_w'][l]).reshape(3, 88, 128).transpose(2, 1, 0) for l in range(2)]))
    shared['cb'] = c_(np.stack([f32(inp['conv_b'][l]).reshape(88, 128).T for l in range(2)]))
    for l in range(2):
        for gh in range(2):
            for k, v in ssm_params_B(inp, l, gh).items():
                shared["%s_%d_%d" % (k, l, gh)] = v
    nb = x.shape[0]
    maps = []
    for b in range(nb):
        m = dict(shared)
        m['xT'] = c_(x[b].T)
        maps.append(m)
    res = run_bass_kernel_spmd(nc, maps, core_ids=list(range(nb))).results
    return c_(np.stack([res[b]['xo'].T for b in range(nb)]))
```

```python
import numpy as np
import ml_dtypes
from contextlib import ExitStack
import concourse.bass as bass
import concourse.mybir as mybir
from concourse.bass_utils import run_bass_kernel_spmd

F32 = mybir.dt.float32
BF16 = mybir.dt.bfloat16
ALU = mybir.AluOpType
AF = mybir.ActivationFunctionType
AX = mybir.AxisListType
ENGS = ['tensor', 'vector', 'scalar', 'gpsimd', 'sync']


class _Op:
    __slots__ = ('eng', 'fn', 'dma', 'deps', 'signal', 'sigval', 'dsem', 'dval', 'pre')

    def __init__(self, eng, fn, dma):
        self.eng = eng
        self.fn = fn
        self.dma = dma
        self.deps = []
        self.signal = False
        self.sigval = 0
        self.dsem = None
        self.dval = 0
        self.pre = None


class Prog:
    def __init__(self, nc, ndma=12):
        self.nc = nc
        self.ops = {e: [] for e in ENGS}
        self.last_w = {}
        self.readers = {}
        self.ndma = ndma
        self.dma_count = {e: 0 for e in ENGS}
        self.es = ExitStack()
        self.scopes = []
        self._uid = 0
        self._par = {}
        self.nbar = 0

    def sb(self, name, shape, dt):
        st = self.scopes[-1] if self.scopes else self.es
        self._uid += 1
        return st.enter_context(self.nc.sbuf_tensor("%s_%d" % (name, self._uid), shape, dt))

    def parity(self, e):
        k = id(e)
        if k not in self._par:
            self._par[k] = e.partition_id() % 2
        return self._par[k]

    def push_scope(self):
        self.scopes.append(ExitStack())

    def pop_scope(self):
        self.barrier()
        self.scopes.pop().close()

    def barrier(self):
        self.nbar += 1
        for e in ENGS:
            o = _Op(e, None, False)
            o.pre = ('bar', self.nbar)
            self.ops[e].append(o)
        self.last_w = {}
        self.readers = {}

    def ps(self, name, shape, dt=F32):
        return self.es.enter_context(self.nc.psum_tensor(name, shape, dt))

    def _add(self, eng, fn, r, w, dma=False):
        o = _Op(eng, fn, dma)
        deps = {}
        for b in r:
            lw = self.last_w.get(b)
            if lw is not None:
                deps[id(lw)] = (lw, True)
        for b in w:
            lw = self.last_w.get(b)
            if lw is not None and id(lw) not in deps:
                deps[id(lw)] = (lw, False)
            rd = self.readers.get(b)
            if rd:
                for x in rd[0].values():
                    if id(x) not in deps:
                        deps[id(x)] = (x, False)
                for x in rd[1]:
                    if id(x) not in deps:
                        deps[id(x)] = (x, False)
        for d, raw in deps.values():
            if d is o:
                continue
            if not d.dma and not dma and d.eng == eng:
                if not raw or eng == 'tensor':
                    continue
            o.deps.append(d)
        for b in r:
            rd = self.readers.setdefault(b, ({}, []))
            if dma:
                rd[1].append(o)
            else:
                rd[0][eng] = o
        for b in w:
            self.last_w[b] = o
            self.readers[b] = ({}, [])
        self.ops[eng].append(o)
        return o

    def add(self, eng, meth, *args, r=(), w=(), **kw):
        return self._add(eng, lambda e: getattr(e, meth)(*args, **kw), r, w)

    def dma(self, out, in_, r=(), w=(), eng='sync', **kw):
        return self._add(eng, lambda e: e.dma_start(out=out, in_=in_, **kw), r, w, dma=True)

    def mm(self, out, lhsT, rhs, start=True, stop=True, r=(), w=(), **kw):
        return self.add('tensor', 'matmul', out, lhsT, rhs, start=start, stop=stop, r=r, w=w, **kw)

    def act(self, out, in_, func, r=(), w=(), **kw):
        return self.add('scalar', 'activation', out, in_, func, r=r, w=w, **kw)

    def emit(self):
        nc = self.nc
        es = self.es
        sem = {e: es.enter_context(nc.semaphore('s_' + e)) for e in ENGS}
        bsem = es.enter_context(nc.semaphore('s_bar'))
        dsems = {}
        for e in ENGS:
            if any(o.dma for o in self.ops[e]):
                dsems[e] = [es.enter_context(nc.semaphore('d_%s_%d' % (e, i))) for i in range(self.ndma)]
        for e in ENGS:
            for o in self.ops[e]:
                for d in o.deps:
                    if not d.dma:
                        d.signal = True
        for e in ENGS:
            c = 0
            j = 0
            for o in self.ops[e]:
                if o.fn is None:
                    continue
                if o.dma:
                    o.dsem = dsems[e][j % self.ndma]
                    o.dval = 16 * (j // self.ndma + 1)
                    o.pre = (o.dsem, o.dval - 16)
                    j += 1
                elif o.signal:
                    c += 1
                    o.sigval = c
        self.sig_counts = {e: sum(1 for o in self.ops[e] if o.signal) for e in ENGS}
        block = es.enter_context(nc.Block())
        all_dma_final = {}
        for e in ENGS:
            for o in self.ops[e]:
                if o.dma:
                    all_dma_final[id(o.dsem)] = (o.dsem, max(o.dval, all_dma_final.get(id(o.dsem), (None, 0))[1]))

        def run(e, engobj):
            waited = {}

            def wait(s, v):
                if v <= 0:
                    return
                if waited.get(id(s), 0) >= v:
                    return
                waited[id(s)] = v
                engobj.wait_ge(s, v)

            mydma = {}
            for o in self.ops[e]:
                if o.fn is None:
                    for s_, v_ in mydma.values():
                        wait(s_, v_)
                    engobj.drain().then_inc(bsem, 1)
                    engobj.wait_ge(bsem, len(ENGS) * o.pre[1])
                    continue
                if o.dma:
                    mydma[id(o.dsem)] = (o.dsem, o.dval)
                for d in o.deps:
                    if d.dma:
                        wait(d.dsem, d.dval)
                    else:
                        wait(sem[d.eng], d.sigval)
                if o.dma:
                    wait(*o.pre)
                    o.fn(engobj).then_inc(o.dsem, 16)
                else:
                    ins = o.fn(engobj)
                    if o.signal:
                        ins.then_inc(sem[e], 1)
            if e == 'sync':
                for s, v in all_dma_final.values():
                    wait(s, v)

        @block.sync
        def _(eng):
            run('sync', eng)

        @block.tensor
        def _(eng):
            run('tensor', eng)

        @block.vector
        def _(eng):
            run('vector', eng)

        @block.scalar
        def _(eng):
            run('scalar', eng)

        @block.gpsimd
        def _(eng):
            run('gpsimd', eng)

        es.close()


D = 2048
T = 4096
NT = 512
S = 4096
EPS = 1e-6
DFF = 5632
NEGM = -30000.0


class Ring:
    def __init__(self, p, name, n, shape, dt):
        self.t = [p.sb("%s%d" % (name, i), shape, dt) for i in range(n)]
        self.k = [(name, i) for i in range(n)]
        self.i = 0

    def next(self):
        j = self.i % len(self.t)
        self.i += 1
        return self.t[j], self.k[j]


class PRing:
    def __init__(self, p, name, n, shape=(128, NT)):
        self.t = [p.ps("%s%d" % (name, i), list(shape)) for i in range(n)]
        self.k = [(name, i) for i in range(n)]
        self.i = 0

    def next(self):
        j = self.i % len(self.t)
        self.i += 1
        return self.t[j], self.k[j]


class SubRing:
    def __init__(self, ring, idx):
        self.t = [ring.t[i] for i in idx]
        self.k = [ring.k[i] for i in idx]
        self.i = 0

    def next(self):
        j = self.i % len(self.t)
        self.i += 1
        return self.t[j], self.k[j]


def wview(t, kc, ncol):
    return t[:, 0:kc * ncol].rearrange("p (k o) -> p k o", o=ncol)


def rms_rstd(p, xs_list, rkeys, sqr, ps_ss, pk, onesf, rstd, n):
    m = len(xs_list)
    for i, xa in enumerate(xs_list):
        sq, sk = sqr.next()
        p.act(sq[:, 0:n], xa, AF.Square, r=rkeys, w=[sk])
        p.mm(ps_ss[:, 0:n], onesf[:], sq[:, 0:n], start=(i == 0), stop=(i == m - 1), r=[sk, 'onesf'], w=[pk])
    rstd_from_ss(p, ps_ss, pk, rstd, n)


def rstd_from_ss(p, ps_ss, pk, rstd, n):
    p.add('vector', 'tensor_scalar', rstd[:, 0:n], ps_ss[:, 0:n], 1.0 / D, EPS, ALU.mult, ALU.add, r=[pk], w=['rstd'])
    p.act(rstd[:, 0:n], rstd[:, 0:n], AF.Sqrt, r=['rstd'], w=['rstd'])
    p.add('vector', 'reciprocal', rstd[:, 0:n], rstd[:, 0:n], r=['rstd'], w=['rstd'])


def emit_A(p, E, l):
    xT = E['xcur']; g = E['g_pre_mix'][l]; w_in = E['w_in'][l]
    uP = E['uP']; qT = E['qT']; kT = E['kT']; v = E['v']; sg = E['sg']
    p.push_scope()
    onesf = p.sb("onesf", [128, 128], F32)
    p.add('vector', 'memset', onesf[:], 1.0, w=['onesf'])
    gt = p.sb("gt", [128, 16], F32)
    p.dma(gt[:], g, w=['gt'])
    xs = p.sb("xs", [128, 16, NT], F32)
    h = p.sb("h", [128, 16, 2048], BF16)
    sqr = Ring(p, "sq", 2, [128, NT], F32)
    rstd = p.sb("rstd", [128, NT], F32)
    wr = Ring(p, "w", 4, [128, 16 * 256], BF16)
    evf = Ring(p, "evf", 3, [128, NT], F32)
    evb = Ring(p, "evb", 3, [128, NT], BF16)
    ubr = Ring(p, "ub", 2, [128, 8, 64], BF16)
    ps_ss = E['pb'].t[7]
    pm = SubRing(E['pb'], [0, 1, 2, 3])
    w3 = w_in.rearrange("(kc p) o -> p kc o", p=128)
    xT3 = xT.rearrange("(kc p) t -> p kc t", p=128)
    tog = [0]

    def evac(out, in_, r, w):
        tog[0] ^= 1
        if tog[0]:
            p.add('vector', 'tensor_copy', out, in_, r=r, w=w)
        else:
            p.add('scalar', 'copy', out, in_, r=r, w=w)

    ST = 2048
    for st in range(T // ST):
        for sub in range(ST // NT):
            t0 = st * ST + sub * NT
            p.dma(xs[:], xT3[:, :, t0:t0 + NT], w=['xs'])
            rms_rstd(p, [xs[:, kc, :] for kc in range(16)], ['xs'], sqr, ps_ss, E['pb'].k[7], onesf, rstd, NT)
            for kc in range(16):
                p.add('vector', 'scalar_tensor_tensor', h[:, kc, sub * NT:(sub + 1) * NT], xs[:, kc, :], gt[:, kc:kc + 1], rstd[:],
                      ALU.mult, ALU.mult, r=['xs', 'gt', 'rstd'], w=['h'])
        for cg in range(32):
            wt, wk = wr.next()
            w4 = wview(wt, 16, 256)
            p.dma(w4, w3[:, :, cg * 256:(cg + 1) * 256], w=[wk], eng='gpsimd')
            if 12 <= cg < 16:
                for ts in range(ST // 128):
                    ps, pk = pm.next()
                    for kc in range(16):
                        p.mm(ps[:, 0:256], h[:, kc, ts * 128:(ts + 1) * 128], w4[:, kc, :], start=(kc == 0),
                             stop=(kc == 15), r=['h', wk], w=[pk])
                    eb, ek = evb.next()
                    evac(eb[:, 0:256], ps[:, 0:256], [pk], [ek])
                    r0 = st * ST + ts * 128
                    p.dma(v[r0:r0 + 128, (cg - 12) * 256:(cg - 11) * 256], eb[:, 0:256], r=[ek])
                continue
            for o2 in range(2):
                oc = cg * 2 + o2
                for sub in range(ST // NT):
                    t0 = st * ST + sub * NT
                    tt = t0 // NT
                    ps, pk = pm.next()
                    for kc in range(16):
                        p.mm(ps[:], w4[:, kc, o2 * 128:(o2 + 1) * 128], h[:, kc, sub * NT:(sub + 1) * NT], start=(kc == 0),
                             stop=(kc == 15), r=['h', wk], w=[pk])
                    if oc < 8:
                        ub, uk = ubr.next()
                        p.add('vector', 'tensor_copy', ub[:].rearrange("p s c -> p c s"),
                              ps[:].rearrange("p (c s) -> p c s", s=8), r=[pk], w=[uk])
                        for gl in range(8):
                            gg = oc * 8 + gl
                            p.dma(uP[gg].rearrange("(s m) c -> m s c", m=16)[:, :, tt * 64:(tt + 1) * 64],
                                  ub[gl * 16:(gl + 1) * 16, :, :], r=[uk])
                    elif oc < 24:
                        eb, ek = evb.next()
                        evac(eb[:], ps[:], [pk], [ek])
                        dst = qT if oc < 16 else kT
                        c = (oc - 8) % 8
                        p.dma(dst[c * 128:(c + 1) * 128, t0:t0 + NT], eb[:], r=[ek])
                    else:
                        ef, fk = evf.next()
                        p.act(ef[:], ps[:], AF.Sigmoid, r=[pk], w=[fk])
                        c = oc - 32
                        p.dma(sg[c * 128:(c + 1) * 128, t0:t0 + NT], ef[:], r=[fk])
    p.pop_scope()


NG = 32
NH = 4
KK_EXP = [0, -1, -2, -3, -4, -5, -6, -7] + list(range(8)) + [7, 6, 5, 4, 3, 2, 1, 0] + list(range(1, 9)) + [8]
I32 = mybir.dt.int32
TWO_PI = 6.283185307179586
ATT_SCALE = 1.0 / (128 ** 0.5)


def emit_B(p, E, l):
    uP_all = E['uP']; qT = E['qT']; kT = E['kT']; vv = E['v']; yP_all = E['yP']; aT = E['aT']
    c_identf = E['identf']; c_identb = E['identb']; c_tmask = E['tmask']; c_sel = E['selE']
    c_causal = E['causal']; c_pastm = E['pastm']; kk = E['kk']
    p.push_scope()

    def ld(name, src, shape, dt=F32, eng='sync'):
        t = p.sb(name, shape, dt)
        p.dma(t[:], src, w=[name], eng=eng)
        return t

    identf = ld("identf_t", c_identf, [128, 128]); identb = ld("identb_t", c_identb, [128, 128], BF16)
    tmask = ld("tmask_t", c_tmask, [128, 128]); selE = ld("selE_t", c_sel, [16, 16, 128], BF16)
    causal = ld("causal_t", c_causal, [128, 2, 256], BF16); pastm = ld("pastm_t", c_pastm, [128, 16, 16])
    onesb = p.sb("onesb", [128, 128], BF16)
    p.add('vector', 'memset', onesb[:], 1.0, w=['onesb'])
    pb = E['pb']

    PERS = []
    for gh in range(2):
        sp = E['ssm'][l][gh]
        lamre = sp['lamre']; lamim = sp['lamim']; ldt = sp['ldt']; bre = sp['bre']; bim = sp['bim']
        cre = sp['cre']; cim = sp['cim']; dsk = sp['dsk']
        TT = p.sb("TT", [128, NG, 128], BF16); P1 = p.sb("P1", [128, NG, 128], BF16)
        QQre = p.sb("QQre", [64, NG, 128], BF16); QQnim = p.sb("QQnim", [64, NG, 128], BF16)
        C1 = p.sb("C1", [64, 64], F32); C2 = p.sb("C2", [64, 64], F32)
        p.push_scope()
        t_lre = ld("t_lre", lamre, [64, NG]); t_lim = ld("t_lim", lamim, [64, NG]); t_ldt = ld("t_ldt", ldt, [64, NG])
        t_bre = ld("t_bre", bre, [64, NG, 16]); t_bim = ld("t_bim", bim, [64, NG, 16])
        t_cre = ld("t_cre", cre, [64, NG, 16]); t_cim = ld("t_cim", cim, [64, NG, 16])
        t_dsk = ld("t_dsk", dsk, [128, NG]); t_kk = ld("t_kk", kk, [64, 33])
        V = 'vector'
        sh3 = [64, NG, 33]
        dt_ = p.sb("dt_", [64, NG], F32); ar = p.sb("ar", [64, NG], F32); ai = p.sb("ai", [64, NG], F32)
        p.act(dt_[:], t_ldt[:], AF.Exp, r=['t_ldt'], w=['dt_'])
        p.add(V, 'tensor_tensor', ar[:], t_lre[:], dt_[:], ALU.mult, r=['t_lre', 'dt_'], w=['ar'])
        p.add(V, 'tensor_tensor', ai[:], t_lim[:], dt_[:], ALU.mult, r=['t_lim', 'dt_'], w=['ai'])
        th = p.sb("th", sh3, F32); mag = p.sb("mag", sh3, F32)
        pwre = p.sb("pwre", sh3, F32); pwim = p.sb("pwim", sh3, F32)
        ni = p.sb("ni", sh3, I32); nf = p.sb("nf", sh3, F32); yy = p.sb("yy", sh3, F32)
        kkb = t_kk[:].unsqueeze(1).to_broadcast(sh3)
        p.add(V, 'tensor_tensor', mag[:], ar[:].unsqueeze(2).to_broadcast(sh3), kkb, ALU.mult, r=['ar', 't_kk'], w=['mag'])
        p.act(mag[:], mag[:], AF.Exp, r=['mag'], w=['mag'])
        p.add(V, 'tensor_tensor', th[:], ai[:].unsqueeze(2).to_broadcast(sh3), kkb, ALU.mult, r=['ai', 't_kk'], w=['th'])

        def sin_of(dst, dkey, off):
            p.add(V, 'tensor_scalar', yy[:], th[:], 1.0 / TWO_PI, 32.5 + off, ALU.mult, ALU.add, r=['th'], w=['yy'])
            p.add(V, 'tensor_copy', ni[:], yy[:], r=['yy'], w=['ni'])
            p.add(V, 'tensor_copy', nf[:], ni[:], r=['ni'], w=['nf'])
            p.add(V, 'tensor_tensor', yy[:], yy[:], nf[:], ALU.subtract, r=['yy', 'nf'], w=['yy'])
            p.add(V, 'scalar_tensor_tensor', nf[:], yy[:], 0.0, yy[:], ALU.is_lt, ALU.add, r=['yy'], w=['nf'])
            p.add(V, 'tensor_scalar', nf[:], nf[:], TWO_PI, -TWO_PI / 2, ALU.mult, ALU.add, r=['nf'], w=['nf'])
            p.act(nf[:], nf[:], AF.Sin, r=['nf'], w=['nf'])
            p.add(V, 'tensor_tensor', dst[:], nf[:], mag[:], ALU.mult, r=['nf', 'mag'], w=[dkey])

        sin_of(pwim, 'pwim', 0.0)
        sin_of(pwre, 'pwre', 0.25)
        nr = p.sb("nr", [64, NG], F32); dd = p.sb("dd", [64, NG], F32); t0_ = p.sb("t0_", [64, NG], F32)
        qre = p.sb("qre", [64, NG], F32); qim = p.sb("qim", [64, NG], F32)
        p.add(V, 'tensor_scalar', nr[:], pwre[:, :, 9], -1.0, None, ALU.add, r=['pwre'], w=['nr'])
        nim = pwim[:, :, 9]
        p.add(V, 'tensor_tensor', dd[:], t_lre[:], t_lre[:], ALU.mult, r=['t_lre'], w=['dd'])
        p.add(V, 'tensor_tensor', t0_[:], t_lim[:], t_lim[:], ALU.mult, r=['t_lim'], w=['t0_'])
        p.add(V, 'tensor_tensor', dd[:], dd[:], t0_[:], ALU.add, r=['dd', 't0_'], w=['dd'])
        p.add(V, 'reciprocal', dd[:], dd[:], r=['dd'], w=['dd'])
        p.add(V, 'tensor_tensor', qre[:], nr[:], t_lre[:], ALU.mult, r=['nr', 't_lre'], w=['qre'])
        p.add(V, 'tensor_tensor', t0_[:], nim, t_lim[:], ALU.mult, r=['pwim', 't_lim'], w=['t0_'])
        p.add(V, 'tensor_tensor', qre[:], qre[:], t0_[:], ALU.add, r=['qre', 't0_'], w=['qre'])
        p.add(V, 'tensor_tensor', qre[:], qre[:], dd[:], ALU.mult, r=['qre', 'dd'], w=['qre'])
        p.add(V, 'tensor_tensor', qim[:], nim, t_lre[:], ALU.mult, r=['pwim', 't_lre'], w=['qim'])
        p.add(V, 'tensor_tensor', t0_[:], nr[:], t_lim[:], ALU.mult, r=['nr', 't_lim'], w=['t0_'])
        p.add(V, 'tensor_tensor', qim[:], qim[:], t0_[:], ALU.subtract, r=['qim', 't0_'], w=['qim'])
        p.add(V, 'tensor_tensor', qim[:], qim[:], dd[:], ALU.mult, r=['qim', 'dd'], w=['qim'])
        sh16 = [64, NG, 16]
        bbre = p.sb("bbre", sh16, F32); bbim = p.sb("bbim", sh16, F32); t16 = p.sb("t16", sh16, F32)
        qreb = qre[:].unsqueeze(2).to_broadcast(sh16); qimb = qim[:].unsqueeze(2).to_broadcast(sh16)
        p.add(V, 'tensor_tensor', bbre[:], t_bre[:], qreb, ALU.mult, r=['t_bre', 'qre'], w=['bbre'])
        p.add(V, 'tensor_tensor', t16[:], t_bim[:], qimb, ALU.mult, r=['t_bim', 'qim'], w=['t16'])
        p.add(V, 'tensor_tensor', bbre[:], bbre[:], t16[:], ALU.subtract, r=['bbre', 't16'], w=['bbre'])
        p.add(V, 'tensor_tensor', bbim[:], t_bim[:], qreb, ALU.mult, r=['t_bim', 'qre'], w=['bbim'])
        p.add(V, 'tensor_tensor', t16[:], t_bre[:], qimb, ALU.mult, r=['t_bre', 'qim'], w=['t16'])
        p.add(V, 'tensor_tensor', bbim[:], bbim[:], t16[:], ALU.add, r=['bbim', 't16'], w=['bbim'])
        sh4 = [64, NG, 8, 16]
        LL = p.sb("LL", [128, NG, 128], BF16); RR = p.sb("RR", [128, NG, 128], BF16); PP = p.sb("PP", [128, NG, 128], BF16)
        imtmp = p.sb("imtmp", [64, NG, 128], BF16)
        ta = p.sb("ta", sh4, F32); tb = p.sb("tb", sh4, F32)

        def cprod(i0, vre, vim, vkeys, out_re, rekey, out_im, imkey, neg_im):
            pr = pwre[:, :, i0:i0 + 8].unsqueeze(3).to_broadcast(sh4)
            pi_ = pwim[:, :, i0:i0 + 8].unsqueeze(3).to_broadcast(sh4)
            vr = vre[:].unsqueeze(2).to_broadcast(sh4)
            vi = vim[:].unsqueeze(2).to_broadcast(sh4)
            o_re = out_re.rearrange("p g (j m) -> p g j m", m=16)
            o_im = out_im.rearrange("p g (j m) -> p g j m", m=16)
            p.add(V, 'tensor_tensor', ta[:], pr, vr, ALU.mult, r=['pwre'] + vkeys, w=['ta'])
            p.add('gpsimd', 'tensor_tensor', tb[:], pi_, vi, ALU.mult, r=['pwim'] + vkeys, w=['tb'])
            p.add(V, 'tensor_tensor', o_re, ta[:], tb[:], ALU.subtract, r=['ta', 'tb'], w=[rekey])
            p.add(V, 'tensor_tensor', ta[:], pr, vi, ALU.mult, r=['pwre'] + vkeys, w=['ta'])
            p.add('gpsimd', 'tensor_tensor', tb[:], pi_, vr, ALU.mult, r=['pwim'] + vkeys, w=['tb'])
            if neg_im:
                p.add(V, 'scalar_tensor_tensor', o_im, ta[:], -1.0, tb[:], ALU.mult, ALU.subtract, r=['ta', 'tb'], w=[imkey])
            else:
                p.add(V, 'tensor_tensor', o_im, ta[:], tb[:], ALU.add, r=['ta', 'tb'], w=[imkey])

        cprod(0, bbre, bbim, ['bbre', 'bbim'], LL[0:64], 'LL', imtmp[:], 'imtmp', False)
        p.dma(LL[64:128], imtmp[:], r=['imtmp'], w=['LL'])
        cprod(8, t_cre, t_cim, ['t_cre', 't_cim'], RR[0:64], 'RR', imtmp[:], 'imtmp', True)
        p.dma(RR[64:128], imtmp[:], r=['imtmp'], w=['RR'])
        cprod(16, bbre, bbim, ['bbre', 'bbim'], PP[0:64], 'PP', imtmp[:], 'imtmp', False)
        p.dma(PP[64:128], imtmp[:], r=['imtmp'], w=['PP'])
        cprod(24, t_cre, t_cim, ['t_cre', 't_cim'], QQre[:], 'QQre', QQnim[:], 'QQnim', True)
        p.add(V, 'tensor_copy', C1[:, 0:32], pwre[:, :, 32], r=['pwre'], w=['C1'])
        p.add(V, 'tensor_copy', C1[:, 32:64], pwre[:, :, 32], r=['pwre'], w=['C1'])
        p.add(V, 'tensor_scalar', C2[:, 0:32], pwim[:, :, 32], -1.0, None, ALU.mult, r=['pwim'], w=['C2'])
        p.add(V, 'tensor_copy', C2[:, 32:64], pwim[:, :, 32], r=['pwim'], w=['C2'])
        tfr = Ring(p, "tfr", 2, [128, 4, 128], F32)
        for g0 in range(0, NG, 4):
            ps, pk = pb.next()
            for gl in range(4):
                p.mm(ps[:, gl * 128:(gl + 1) * 128], LL[:, g0 + gl, :], RR[:, g0 + gl, :], r=['LL', 'RR'], w=[pk])
            tf, tk = tfr.next()
            p.add(V, 'tensor_tensor', tf[:], ps[:].rearrange("p (g c) -> p g c", c=128),
                  tmask[:].unsqueeze(1).to_broadcast([128, 4, 128]), ALU.mult, r=[pk, 'tmask_t'], w=[tk])
            for gl in range(4):
                p.add(V, 'scalar_tensor_tensor', TT[:, g0 + gl, :], identf[:], t_dsk[:, g0 + gl:g0 + gl + 1], tf[:, gl, :],
                      ALU.mult, ALU.add, r=['identf_t', 't_dsk', tk], w=['TT'])
            ps2, pk2 = pb.next()
            for gl in range(4):
                p.mm(ps2[:, gl * 128:(gl + 1) * 128], PP[:, g0 + gl, :], identb[:], r=['PP', 'identb_t'], w=[pk2])
            p.add('scalar', 'copy', P1[:, g0:g0 + 4, :], ps2[:].rearrange("p (g c) -> p g c", c=128), r=[pk2], w=['P1'])

        p.pop_scope()
        PERS.append((TT, P1, QQre, QQnim, C1, C2))
    SEGC = 64
    U = p.sb("U", [128, NG, 512], BF16)
    hist = p.sb("hist", [64, SEGC + 1, 96], F32)
    Gst = p.sb("Gst", [64, SEGC, 64], F32)
    Hre = p.sb("Hre", [64, NG, SEGC], BF16); Him = p.sb("Him", [64, NG, SEGC], BF16)
    t1 = p.sb("t1", [64, 64], F32); t2 = p.sb("t2", [64, 64], F32); t3 = p.sb("t3", [64, 64], F32)
    ybr = Ring(p, "yb", 2, [128, 4, SEGC], F32)
    pbs = SubRing(pb, [6, 7])

    def ssm_main():
        for gh in range(2):
            TT, P1, QQre, QQnim, C1, C2 = PERS[gh]
            uP = uP_all[gh * NG:(gh + 1) * NG]; yP = yP_all[gh * NG:(gh + 1) * NG]
            for q4 in range(4):
                p.dma(U[:, q4 * 8:(q4 + 1) * 8, :], uP[q4 * 8:(q4 + 1) * 8].rearrange("g p c -> p g c"), w=['U'])
            p.add(V, 'memset', hist[:, 0, :], 0.0, w=['histA', 'histB'])
            yield
            for seg in range(512 // SEGC):
                cs = slice(seg * SEGC, (seg + 1) * SEGC)
                if seg > 0:
                    p.add(V, 'tensor_copy', hist[:, 0, :], hist[:, SEGC, :], r=['histA', 'histB'], w=['histA', 'histB'])
                for g0 in range(0, NG, 4):
                    for ri in range(2):
                        ps, pk = pbs.next()
                        for gl in range(4):
                            p.mm(ps[0:64, gl * SEGC:(gl + 1) * SEGC], P1[:, g0 + gl, ri * 64:(ri + 1) * 64], U[:, g0 + gl, cs],
                                 r=['P1', 'U'], w=[pk])
                        dst = Gst[:, :, ri * 32 + g0:ri * 32 + g0 + 4].rearrange("p c g -> p g c")
                        src = ps[0:64, 0:4 * SEGC].rearrange("p (g c) -> p g c", c=SEGC)
                        if ri == 0:
                            p.add(V, 'tensor_copy', dst, src, r=[pk], w=['Gst'])
                        else:
                            p.add('scalar', 'copy', dst, src, r=[pk], w=['Gst'])
                        yield
                for c in range(SEGC):
                    p.add(V, 'tensor_tensor', t1[:], hist[:, c, 0:64], C1[:], ALU.mult, r=['histA', 'C1'], w=['t1'])
                    p.add(V, 'tensor_tensor', t2[:], hist[:, c, 32:96], C2[:], ALU.mult, r=['histA', 'histB', 'C2'], w=['t2'])
                    p.add(V, 'tensor_tensor', t3[:], t1[:], t2[:], ALU.add, r=['t1', 't2'], w=['t3'])
                    p.add(V, 'tensor_tensor', hist[:, c + 1, 0:64], t3[:], Gst[:, c, :], ALU.add, r=['t3', 'Gst'], w=['histA'])
                    p.add(V, 'tensor_tensor', hist[:, c + 1, 64:96], t3[:, 0:32], Gst[:, c, 0:32], ALU.add,
                          r=['t3', 'Gst'], w=['histB'])
                    yield
                p.add(V, 'tensor_copy', Hre[:], hist[:, 0:SEGC, 0:32].rearrange("p c g -> p g c"), r=['histA'], w=['Hre'])
                p.add('gpsimd', 'tensor_copy', Him[:], hist[:, 0:SEGC, 32:64].rearrange("p c g -> p g c"), r=['histA'], w=['Him'])
                yield
                for g0 in range(0, NG, 4):
                    ps, pk = pbs.next()
                    for gl in range(4):
                        g = g0 + gl
                        o = ps[:, gl * SEGC:(gl + 1) * SEGC]
                        p.mm(o, TT[:, g, :], U[:, g, cs], start=True, stop=False, r=['TT', 'U'], w=[pk])
                        p.mm(o, QQre[:, g, :], Hre[:, g, :], start=False, stop=False, r=['QQre', 'Hre'], w=[pk])
                        p.mm(o, QQnim[:, g, :], Him[:, g, :], start=False, stop=True, r=['QQnim', 'Him'], w=[pk])
                    yb, yk = ybr.next()
                    p.add('scalar', 'copy', yb[:], ps[:, 0:4 * SEGC].rearrange("p (g c) -> p g c", c=SEGC), r=[pk], w=[yk])
                    p.dma(yP[g0:g0 + 4, :, cs].rearrange("g p c -> p g c"), yb[:], r=[yk])
                    yield

    ssm_it = ssm_main()
    Kh = p.sb("Kh", [128, S], BF16); Qh = p.sb("Qh", [128, S], BF16); Vh = p.sb("Vh", [128, 32, 128], BF16)
    ksum = p.sb("ksum", [128, 16], F32); kmh = p.sb("kmh", [128, 16], BF16); kml = p.sb("kml", [128, 16], BF16)
    kmhf = p.sb("kmhf", [128, 16], F32)
    maskT = p.sb("maskT", [16, S], BF16)
    gmr = Ring(p, "gm", 2, [128, 16], F32); mxr = Ring(p, "mx", 2, [128, 8], F32); thr_r = Ring(p, "thr", 2, [128, 1], F32)
    bir = Ring(p, "bi", 2, [128, 16], F32)
    ptr = Ring(p, "pt", 4, [128, 512], BF16)
    rlr = Ring(p, "rl", 2, [128, 512], F32); obr = Ring(p, "ob", 2, [128, 512], BF16)
    psr = SubRing(pb, [0, 1, 2]); por = SubRing(pb, [3]); plr = SubRing(pb, [4]); pgr = SubRing(pb, [5])
    for hh in range(8):
        hs = slice(hh * 128, (hh + 1) * 128)
        p.dma(Kh[:], kT[hs, :], w=['Kh'])
        p.dma(Qh[:], qT[hs, :], w=['Qh'])
        p.dma(Vh[:], vv[:, hs].rearrange("(kt p) d -> p kt d", p=128), w=['Vh'])
        p.add(V, 'tensor_reduce', ksum[:], Kh[:].rearrange("p (j l) -> p j l", l=256), AX.X, ALU.add, r=['Kh'], w=['ksum'])
        p.add(V, 'tensor_scalar', ksum[:], ksum[:], 1.0 / 256, None, ALU.mult, r=['ksum'], w=['ksum'])
        p.add(V, 'tensor_copy', kmh[:], ksum[:], r=['ksum'], w=['kmh'])
        p.add(V, 'tensor_copy', kmhf[:], kmh[:], r=['kmh'], w=['kmhf'])
        p.add(V, 'tensor_tensor', kmhf[:], ksum[:], kmhf[:], ALU.subtract, r=['ksum', 'kmhf'], w=['kmhf'])
        p.add(V, 'tensor_copy', kml[:], kmhf[:], r=['kmhf'], w=['kml'])
        for qs in range(32):
            n = qs // 2
            ps, pk = pgr.next()
            p.mm(ps[:, 0:16], Qh[:, qs * 128:(qs + 1) * 128], kmh[:], start=True, stop=False, r=['Qh', 'kmh'], w=[pk])
            p.mm(ps[:, 0:16], Qh[:, qs * 128:(qs + 1) * 128], kml[:], start=False, stop=True, r=['Qh', 'kml'], w=[pk])
            gm, gk = gmr.next(); mx, mk = mxr.next(); th_, tk_ = thr_r.next(); bi, bk = bir.next()
            p.add(V, 'tensor_tensor', gm[:], ps[:, 0:16], pastm[:, n, :], ALU.add, r=[pk, 'pastm_t'], w=[gk])
            p.add(V, 'max', mx[:], gm[:], r=[gk], w=[mk])
            p.add(V, 'tensor_scalar_max', th_[:], mx[:, 2:3], -10000.0, r=[mk], w=[tk_])
            p.add(V, 'tensor_scalar', bi[:], gm[:], th_[:, 0:1], -NEGM, ALU.is_ge, ALU.mult, r=[gk, tk_], w=[bk])
            p.add(V, 'tensor_scalar', bi[:], bi[:], NEGM, None, ALU.add, r=[bk], w=[bk])
            p.add(V, 'memset', bi[:, n:n + 1], 0.0, w=[bk])
            ps2, pk2 = psr.next()
            p.mm(ps2[0:16, 0:128], bi[:], identf[:], r=[bk, 'identf_t'], w=[pk2])
            p.add('scalar', 'copy', maskT[:, qs * 128:(qs + 1) * 128], ps2[0:16, 0:128], r=[pk2], w=['maskT'])
            next(ssm_it, None)
        for np_ in range(8):
            n0 = 2 * np_
            qsl = slice(n0 * 256, (n0 + 2) * 256)
            po, pok = por.next()
            pl, plk = plr.next()
            nkc = 2 * n0 + 2
            tot = nkc + 2
            pend = []

            def stage1(kt):
                j = kt // 2
                cs_ = slice(0, 512) if kt < nkc else slice(256, 512)
                qcols = slice(n0 * 256, (n0 + 2) * 256) if kt < nkc else slice((n0 + 1) * 256, (n0 + 2) * 256)
                ps, pk = psr.next()
                p.mm(ps[:, cs_], Kh[:, kt * 128:(kt + 1) * 128], Qh[:, qcols], start=True, stop=False, r=['Kh', 'Qh'], w=[pk])
                p.mm(ps[:, cs_], selE[:, j, :], maskT[:, qcols], start=False, stop=(j < n0), r=['selE_t', 'maskT'], w=[pk])
                dcol = slice(0, 256) if j == n0 else slice(256, 512)
                if j >= n0:
                    p.mm(ps[:, dcol], identb[:], causal[:, kt % 2, :], start=False, stop=True, r=['identb_t', 'causal_t'], w=[pk])
                pt, ptk = ptr.next()
                p.act(pt[:, cs_], ps[:, cs_], AF.Exp, scale=ATT_SCALE, r=[pk], w=[ptk])
                pend.append((kt, cs_, pt, ptk))

            def stage2():
                kt, cs_, pt, ptk = pend.pop(0)
                p.mm(po[:, cs_], Vh[:, kt, :], pt[:, cs_], start=(kt == 0), stop=(kt == tot - 1), r=['Vh', ptk], w=[pok])
                p.mm(pl[:, cs_], onesb[:], pt[:, cs_], start=(kt == 0), stop=(kt == tot - 1), r=['onesb', ptk], w=[plk])

            for kt in range(tot):
                stage1(kt)
                next(ssm_it, None)
                if len(pend) > 2:
                    stage2()
            while pend:
                stage2()
            rl, rk = rlr.next(); ob, ok_ = obr.next()
            p.add(V, 'reciprocal', rl[:], pl[:], r=[plk], w=[rk])
            p.add(V, 'tensor_tensor', ob[:], po[:], rl[:], ALU.mult, r=[pok, rk], w=[ok_])
            p.dma(aT[hs, qsl], ob[:], r=[ok_])
    for _ in ssm_it:
        pass
    p.pop_scope()


def _bf(a):
    return np.ascontiguousarray(a).astype(ml_dtypes.bfloat16)


def consts_B():
    sm = np.arange(128) // 16
    tmask = (sm[None, :] >= sm[:, None]).astype(np.float32)
    selE = np.zeros((16, 16, 128), np.float32)
    for j in range(16):
        selE[j, j, :] = 1.0
    k = np.arange(128)[:, None, None]
    a = np.arange(2)[None, :, None]
    q = np.arange(256)[None, None, :]
    causal = np.where(a * 128 + k <= q, 0.0, NEGM).astype(np.float32)
    n = np.arange(16)[None, :, None]
    j = np.arange(16)[None, None, :]
    pastm = np.broadcast_to(np.where(j < n, 0.0, NEGM), (128, 16, 16)).astype(np.float32)
    return {
        "identf": np.eye(128, dtype=np.float32), "identb": _bf(np.eye(128, dtype=np.float32)),
        "tmask": tmask, "selE": _bf(selE), "causal": _bf(causal), "pastm": np.ascontiguousarray(pastm),
        "kk": np.ascontiguousarray(np.tile(np.array(KK_EXP, np.float32), (64, 1))),
    }


def ssm_params_B(inp, l, gh):
    gs = slice(gh * NG, (gh + 1) * NG)
    c = np.ascontiguousarray
    return {
        "lamre": c(inp['lam_re'][l][gs].T), "lamim": c(inp['lam_im'][l][gs].T),
        "ldt": c(np.broadcast_to(inp['log_dt'][l][gs][None, :], (64, NG))),
        "bre": c(inp['b_re'][l][gs].transpose(1, 0, 2)), "bim": c(inp['b_im'][l][gs].transpose(1, 0, 2)),
        "cre": c(inp['c_re'][l][gs].transpose(2, 0, 1)), "cim": c(inp['c_im'][l][gs].transpose(2, 0, 1)),
        "dsk": c(np.tile(inp['d_skip'][l].reshape(64, 16)[gs].T, (8, 1))),
    }


def _glay(gv):
    return np.ascontiguousarray(np.asarray(gv, np.float32).reshape(16, 128).T)


def emit_C(p, E, l):
    yP = E['yP']; aT = E['aT']; sg = E['sg']; xT = E['xcur']
    w_glu = E['w_glu'][l]; w_us = E['w_up_ssm'][l]; w_ua = E['w_up_attn'][l]; w_out = E['w_out'][l]
    g1 = E['g_post_mix'][l]; g2 = E['g_pre_ffn'][l]
    xm_o = E['xm']; hf_o = E['hfx'][:, 2:T + 2]
    p.push_scope()
    V = 'vector'; G = 'gpsimd'
    onesf = p.sb("onesf", [128, 128], F32)
    p.add(V, 'memset', onesf[:], 1.0, w=['onesf'])
    g1t = p.sb("g1t", [128, 16], F32); g2t = p.sb("g2t", [128, 16], F32)
    p.dma(g1t[:], g1, w=['g1t']); p.dma(g2t[:], g2, w=['g2t'])
    ybr = Ring(p, "ybuf", 2, [128, 8, 64], F32)
    yall = p.sb("yall", [128, 8, NT], F32); yb = p.sb("ybb", [128, 8, NT], BF16); y2 = p.sb("y2", [128, 8, NT], BF16)
    at = p.sb("at", [128, 8, NT], BF16); mb = p.sb("mb", [128, 16, NT], BF16)
    mo = p.sb("mo", [128, 16, NT], F32); xs = p.sb("xs", [128, 16, NT], F32); hfb = p.sb("hfb", [128, 16, NT], BF16)
    tr = Ring(p, "tmp", 4, [128, NT], F32)
    sgr = Ring(p, "sgt", 4, [128, NT], F32)
    sqr = Ring(p, "sq", 2, [128, NT], F32)
    rstd = p.sb("rstd", [128, NT], F32)
    wr = Ring(p, "w", 4, [128, 16 * 256], BF16)
    ps_ss = E['pb'].t[7]
    pm = SubRing(E['pb'], [0, 1, 2, 3, 4, 5])
    xT3 = xT.rearrange("(kc p) t -> p kc t", p=128)
    aT3 = aT.rearrange("(kc p) t -> p kc t", p=128)

    def wload(wd, kc, cg):
        wt, wk = wr.next()
        w4 = wview(wt, kc, 256)
        p.dma(w4, wd.rearrange("(kc p) o -> p kc o", p=128)[:, :, cg * 256:(cg + 1) * 256], w=[wk], eng=G)
        return w4, wk

    for tt in range(T // NT):
        t0 = tt * NT
        ts = slice(t0, t0 + NT)
        for kc in range(8):
            ybf, yk = ybr.next()
            for gl in range(8):
                p.dma(ybf[gl * 16:(gl + 1) * 16, :, :],
                      yP[kc * 8 + gl].rearrange("(t m) c -> m t c", m=16)[:, :, tt * 64:(tt + 1) * 64], w=[yk])
            xv = ybf[:].rearrange("p t c -> p c t")
            a, ak = tr.next()
            av = a[:].rearrange("p (c t) -> p c t", t=8)
            p.add(V, 'tensor_tensor', av, xv, xv, ALU.mult, r=[yk], w=[ak])
            p.add(V, 'tensor_scalar', a[:], a[:], 0.044715, 1.0, ALU.mult, ALU.add, r=[ak], w=[ak])
            p.add(V, 'tensor_tensor', av, av, xv, ALU.mult, r=[ak, yk], w=[ak])
            p.act(a[:], a[:], AF.Sigmoid, scale=1.5957691216057308, r=[ak], w=[ak])
            p.add(V, 'tensor_tensor', yall[:, kc, :].rearrange("p (c t) -> p c t", t=8), av, xv, ALU.mult, r=[ak, yk], w=['yall'])
            p.add(V, 'tensor_copy', yb[:, kc, :], yall[:, kc, :], r=['yall'], w=['yb'])
        for cg in range(4):
            w4, wk = wload(w_glu, 8, cg)
            for o2 in range(2):
                oc = cg * 2 + o2
                ps, pk = pm.next()
                for kc in range(8):
                    p.mm(ps[:], w4[:, kc, o2 * 128:(o2 + 1) * 128], yb[:, kc, :], start=(kc == 0), stop=(kc == 7), r=[wk, 'yb'], w=[pk])
                s_, sk = tr.next()
                p.act(s_[:], ps[:], AF.Sigmoid, r=[pk], w=[sk])
                p.add(V, 'tensor_tensor', y2[:, oc, :], yall[:, oc, :], s_[:], ALU.mult, r=['yall', sk], w=['y2'])
        p.dma(at[:], aT3[:, :, ts], w=['at'])
        for cg in range(8):
            w1, k1 = wload(w_us, 8, cg)
            w2, k2 = wload(w_ua, 8, cg)
            for o2 in range(2):
                oc = cg * 2 + o2
                osl = slice(o2 * 128, (o2 + 1) * 128)
                sa, sak = sgr.next(); sb_, sbk = sgr.next()
                p.dma(sa[:], sg[oc * 128:(oc + 1) * 128, ts], w=[sak])
                p.dma(sb_[:], sg[2048 + oc * 128:2048 + (oc + 1) * 128, ts], w=[sbk])
                ps1, pk1 = pm.next(); ps2, pk2 = pm.next()
                for kc in range(8):
                    p.mm(ps1[:], w1[:, kc, osl], y2[:, kc, :], start=(kc == 0), stop=(kc == 7), r=[k1, 'y2'], w=[pk1])
                for kc in range(8):
                    p.mm(ps2[:], w2[:, kc, osl], at[:, kc, :], start=(kc == 0), stop=(kc == 7), r=[k2, 'at'], w=[pk2])
                m1, mk1 = tr.next(); m2, mk2 = tr.next()
                p.add(V, 'tensor_tensor', m1[:], ps1[:], sa[:], ALU.mult, r=[pk1, sak], w=[mk1])
                p.add(V, 'tensor_tensor', m2[:], ps2[:], sb_[:], ALU.mult, r=[pk2, sbk], w=[mk2])
                p.add(V, 'tensor_tensor', mb[:, oc, :], m1[:], m2[:], ALU.add, r=[mk1, mk2], w=['mb'])
        p.dma(xs[:], xT3[:, :, ts], w=['xs'])
        for cg in range(8):
            w4, wk = wload(w_out, 16, cg)
            for o2 in range(2):
                oc = cg * 2 + o2
                ps, pk = pm.next()
                for kc in range(16):
                    p.mm(ps[:], w4[:, kc, o2 * 128:(o2 + 1) * 128], mb[:, kc, :], start=(kc == 0), stop=(kc == 15), r=[wk, 'mb'], w=[pk])
                p.act(mo[:, oc, :], ps[:], AF.Copy, r=[pk], w=['mo'])
        rms_rstd(p, [mo[:, kc, :] for kc in range(16)], ['mo'], sqr, ps_ss, E['pb'].k[7], onesf, rstd, NT)
        for oc in range(16):
            p.add(V, 'scalar_tensor_tensor', mo[:, oc, :], mo[:, oc, :], g1t[:, oc:oc + 1], rstd[:], ALU.mult, ALU.mult,
                  r=['mo', 'g1t', 'rstd'], w=['mo'])
            p.add(V, 'tensor_tensor', mo[:, oc, :], mo[:, oc, :], xs[:, oc, :], ALU.add, r=['mo', 'xs'], w=['mo'])
        p.dma(xm_o.rearrange("(kc p) t -> p kc t", p=128)[:, :, ts], mo[:], r=['mo'])
        rms_rstd(p, [mo[:, kc, :] for kc in range(16)], ['mo'], sqr, ps_ss, E['pb'].k[7], onesf, rstd, NT)
        for oc in range(16):
            p.add(V, 'scalar_tensor_tensor', hfb[:, oc, :], mo[:, oc, :], g2t[:, oc:oc + 1], rstd[:], ALU.mult, ALU.mult,
                  r=['mo', 'g2t', 'rstd'], w=['hfb'])
        p.dma(hf_o.rearrange("(kc p) t -> p kc t", p=128)[:, :, ts], hfb[:], r=['hfb'])
    p.pop_scope()


def emit_D(p, E, l):
    hfx = E['hfx']; xm = E['xm']; w_up = E['w_ffn_up'][l]; w_dn = E['w_ffn_down'][l]
    cw = E['cw'][l]; cb = E['cb'][l]; g3 = E['g_post_ffn'][l]
    xo = E['xout'] if l == 1 else E['xnext']
    TS = 1024
    NH_ = TS // NT
    p.push_scope()
    V = 'vector'; G = 'gpsimd'
    onesf = p.sb("onesf", [128, 128], F32)
    p.add(V, 'memset', onesf[:], 1.0, w=['onesf'])
    g3t = p.sb("g3t", [128, 16], F32); cwt = p.sb("cwt", [128, 88, 3], F32); cbt = p.sb("cbt", [128, 88], F32)
    p.dma(g3t[:], g3, w=['g3t']); p.dma(cwt[:], cw, w=['cwt']); p.dma(cbt[:], cb, w=['cbt'])
    ztail = p.sb("ztail", [128, 88, 2], F32)
    p.add(V, 'memset', ztail[:], 0.0, w=['ztail'])
    actb = p.sb("actb", [128, 44, TS], BF16)
    ps_ss = E['pb'].t[7]
    pm = SubRing(E['pb'], [0, 1, 2, 3, 4, 5, 6])
    hfx3 = hfx.rearrange("(kc p) t -> p kc t", p=128)
    wup3 = w_up.rearrange("(kc p) o -> p kc o", p=128)
    wdn3 = w_dn.rearrange("(kc p) o -> p kc o", p=128)
    for st in range(T // TS):
        s0 = st * TS
        p.push_scope()
        hx = p.sb("hx", [128, 16, TS + 2], BF16)
        zr = Ring(p, "z", 4, [128, NT + 2], F32)
        cr = Ring(p, "cv", 4, [128, NT], F32)
        wr = Ring(p, "w", 3, [128, 2 * 16 * 256], BF16)
        p.dma(hx[:], hfx3[:, :, s0:s0 + TS + 2], w=['hx'])
        for jj in range(22):
            wt, wk = wr.next()
            w5 = wt[:].rearrange("p (a k o) -> p a k o", a=2, o=256)
            p.dma(w5[:, 0], wup3[:, :, jj * 256:(jj + 1) * 256], w=[wk], eng=G)
            p.dma(w5[:, 1], wup3[:, :, DFF + jj * 256:DFF + (jj + 1) * 256], w=[wk], eng=G)
            for o2 in range(2):
                j = jj * 2 + o2
                osl = slice(o2 * 128, (o2 + 1) * 128)
                for hb in range(NH_):
                    cres = []
                    for which in range(2):
                        ch = which * 44 + j
                        ps, pk = pm.next()
                        for kc in range(16):
                            p.mm(ps[:], w5[:, which, kc, osl], hx[:, kc, 2 + hb * NT:2 + (hb + 1) * NT], start=(kc == 0),
                                 stop=(kc == 15), r=[wk, 'hx'], w=[pk])
                        zt, zk = zr.next()
                        p.act(zt[:, 2:NT + 2], ps[:], AF.Copy, r=[pk], w=[zk])
                        p.add('scalar', 'copy', zt[:, 0:2], ztail[:, ch, :], r=['ztail'], w=[zk])
                        p.add('scalar', 'copy', ztail[:, ch, :], zt[:, NT:NT + 2], r=[zk], w=['ztail'])
                        c1, ck = cr.next()
                        p.add(V, 'tensor_scalar', c1[:], zt[:, 2:NT + 2], cwt[:, ch, 2:3], cbt[:, ch:ch + 1], ALU.mult, ALU.add,
                              r=[zk, 'cwt', 'cbt'], w=[ck])
                        p.add(V, 'scalar_tensor_tensor', c1[:], zt[:, 1:NT + 1], cwt[:, ch, 1:2], c1[:], ALU.mult, ALU.add,
                              r=[zk, 'cwt', ck], w=[ck])
                        p.add(V, 'scalar_tensor_tensor', c1[:], zt[:, 0:NT], cwt[:, ch, 0:1], c1[:], ALU.mult, ALU.add,
                              r=[zk, 'cwt', ck], w=[ck])
                        cres.append((c1, ck))
                    (ca, cak), (cv_, cvk) = cres
                    p.act(ca[:], ca[:], AF.Silu, r=[cak], w=[cak])
                    p.add(V, 'tensor_tensor', actb[:, j, hb * NT:(hb + 1) * NT], ca[:], cv_[:], ALU.mult, r=[cak, cvk], w=['actb'])
        p.pop_scope()
        p.push_scope()
        f = p.sb("f", [128, 16, TS], F32)
        xr = Ring(p, "xm", 2, [128, NT], F32)
        orr = Ring(p, "or", 2, [128, NT], F32)
        sqr = Ring(p, "sq", 2, [128, NT], F32)
        rstd = p.sb("rstd", [128, NT], F32)
        wr = Ring(p, "wd", 3, [128, 44 * 128], BF16)
        for oc in range(16):
            wt, wk = wr.next()
            w4 = wview(wt, 44, 128)
            p.dma(w4, wdn3[:, :, oc * 128:(oc + 1) * 128], w=[wk], eng=G)
            for hb in range(NH_):
                ps, pk = pm.next()
                for j in range(44):
                    p.mm(ps[:], w4[:, j, :], actb[:, j, hb * NT:(hb + 1) * NT], start=(j == 0), stop=(j == 43), r=[wk, 'actb'], w=[pk])
                p.act(f[:, oc, hb * NT:(hb + 1) * NT], ps[:], AF.Copy, r=[pk], w=['f'])
        for hb in range(NH_):
            hs_ = slice(hb * NT, (hb + 1) * NT)
            ts = slice(s0 + hb * NT, s0 + (hb + 1) * NT)
            rms_rstd(p, [f[:, kc, hs_] for kc in range(16)], ['f'], sqr, ps_ss, E['pb'].k[7], onesf, rstd, NT)
            for oc in range(16):
                xt, xk = xr.next(); ot, ok_ = orr.next()
                p.dma(xt[:], xm[oc * 128:(oc + 1) * 128, ts], w=[xk])
                p.add(V, 'scalar_tensor_tensor', ot[:], f[:, oc, hs_], g3t[:, oc:oc + 1], rstd[:], ALU.mult, ALU.mult,
                      r=['f', 'g3t', 'rstd'], w=[ok_])
                p.add(V, 'tensor_tensor', ot[:], ot[:], xt[:], ALU.add, r=[ok_, xk], w=[ok_])
                p.dma(xo[oc * 128:(oc + 1) * 128, ts], ot[:], r=[ok_])
        p.pop_scope()
    p.pop_scope()


def build_fused():
    nc = bass.Bass("TRN2", target_bir_lowering=False)
    E = {}

    def din(name, shape, dt=F32):
        return nc.dram_tensor(name, shape, dt, kind="ExternalInput").ap()

    def dscr(name, shape, dt=F32):
        return nc.dram_tensor(name, shape, dt, kind="Internal").ap()

    E['xin'] = din("xT", [D, T])
    for nm in ['g_pre_mix', 'g_post_mix', 'g_pre_ffn', 'g_post_ffn']:
        E[nm] = din(nm, [2, 128, 16])
    E['w_in'] = din("w_in", [2, D, 8192]); E['w_glu'] = din("w_glu", [2, 1024, 1024])
    E['w_up_ssm'] = din("w_up_ssm", [2, 1024, D]); E['w_up_attn'] = din("w_up_attn", [2, 1024, D])
    E['w_out'] = din("w_out", [2, D, D]); E['w_ffn_up'] = din("w_ffn_up", [2, D, 2 * DFF]); E['w_ffn_down'] = din("w_ffn_down", [2, DFF, D])
    E['cw'] = din("cw", [2, 128, 88, 3]); E['cb'] = din("cb", [2, 128, 88])
    E['ssm'] = [[{k: din("%s_%d_%d" % (k, l, gh), shp) for k, shp in
                  [('lamre', [64, NG]), ('lamim', [64, NG]), ('ldt', [64, NG]), ('bre', [64, NG, 16]), ('bim', [64, NG, 16]),
                   ('cre', [64, NG, 16]), ('cim', [64, NG, 16]), ('dsk', [128, NG])]} for gh in range(2)] for l in range(2)]
    E['kk'] = din("kk", [64, 33]); E['identf'] = din("identf", [128, 128]); E['identb'] = din("identb", [128, 128], BF16)
    E['tmask'] = din("tmask", [128, 128]); E['selE'] = din("selE", [16, 16, 128], BF16)
    E['causal'] = din("causal", [128, 2, 256], BF16); E['pastm'] = din("pastm", [128, 16, 16])
    E['xout'] = nc.dram_tensor("xo", [D, T], F32, kind="ExternalOutput").ap()
    E['xnext'] = dscr("xnext", [D, T])
    E['uP'] = dscr("uP", [64, 128, 512], BF16); E['qT'] = dscr("qT", [1024, T], BF16); E['kT'] = dscr("kT", [1024, T], BF16)
    E['v'] = dscr("v", [T, 1024], BF16); E['sg'] = dscr("sg", [4096, T]); E['yP'] = dscr("yP", [64, 128, 512])
    E['aT'] = dscr("aT", [1024, T], BF16); E['xm'] = dscr("xm", [D, T]); E['hfx'] = dscr("hfx", [D, T + 2], BF16)
    p = Prog(nc)
    E['pb'] = PRing(p, "pb", 8)
    zt = p.sb("zt", [128, 16, 2], BF16)
    p.add('vector', 'memset', zt[:], 0.0, w=['zt'])
    p.dma(E['hfx'].rearrange("(kc p) t -> p kc t", p=128)[:, :, 0:2], zt[:], r=['zt'])
    for l in range(2):
        E['xcur'] = E['xin'] if l == 0 else E['xnext']
        emit_A(p, E, l)
        emit_B(p, E, l)
        emit_C(p, E, l)
        emit_D(p, E, l)
    p.emit()
    return nc


_NC = []


def kernel(**inp):
    c_ = np.ascontiguousarray
    f32 = lambda a: c_(np.asarray(a, np.float32))
    if not _NC:
        _NC.append(build_fused())
    nc = _NC[0]
    x = f32(inp['x'])
    shared = dict(consts_B())
    for nm in ['g_pre_mix', 'g_post_mix', 'g_pre_ffn', 'g_post_ffn']:
        shared[nm] = c_(np.stack([_glay(inp[nm][l]) for l in range(2)]))
    shared['w_in'] = f32(inp['w_in']); shared['w_glu'] = f32(inp['w_glu'])
    shared['w_up_ssm'] = f32(inp['w_up_ssm']); shared['w_up_attn'] = f32(inp['w_up_attn'])
    shared['w_out'] = f32(inp['w_out']); shared['w_ffn_up'] = f32(inp['w_ffn_up']); shared['w_ffn_down'] = f32(inp['w_ffn_down'])
    shared['cw'] = c_(np.stack([f32(inp['conv_w'][l]).reshape(3, 88, 128).transpose(2, 1, 0) for l in range(2)]))
    shared['cb'] = c_(np.stack([f32(inp['conv_b'][l]).reshape(88, 128).T for l in range(2)]))
    for l in range(2):
        for gh in range(2):
            for k, v in ssm_params_B(inp, l, gh).items():
                shared["%s_%d_%d" % (k, l, gh)] = v
    nb = x.shape[0]
    maps = []
    for b in range(nb):
        m = dict(shared)
        m['xT'] = c_(x[b].T)
        maps.append(m)
    res = run_bass_kernel_spmd(nc, maps, core_ids=list(range(nb))).results
    return c_(np.stack([res[b]['xo'].T for b in range(nb)]))
```

```python
import numpy as np
import ml_dtypes
from contextlib import ExitStack
import concourse.bass as bass
import concourse.mybir as mybir
from concourse.bass_utils import run_bass_kernel_spmd

F32 = mybir.dt.float32
BF16 = mybir.dt.bfloat16
ALU = mybir.AluOpType
AF = mybir.ActivationFunctionType
AX = mybir.AxisListType
ENGS = ['tensor', 'vector', 'scalar', 'gpsimd', 'sync']


class _Op:
    __slots__ = ('eng', 'fn', 'dma', 'deps', 'signal', 'sigval', 'dsem', 'dval', 'pre')

    def __init__(self, eng, fn, dma):
        self.eng = eng
        self.fn = fn
        self.dma = dma
        self.deps = []
        self.signal = False
        self.sigval = 0
        self.dsem = None
        self.dval = 0
        self.pre = None


class Prog:
    def __init__(self, nc, ndma=12):
        self.nc = nc
        self.ops = {e: [] for e in ENGS}
        self.last_w = {}
        self.readers = {}
        self.ndma = ndma
        self.dma_count = {e: 0 for e in ENGS}
        self.es = ExitStack()
        self.scopes = []
        self._uid = 0
        self._par = {}
        self.nbar = 0

    def sb(self, name, shape, dt):
        st = self.scopes[-1] if self.scopes else self.es
        self._uid += 1
        return st.enter_context(self.nc.sbuf_tensor("%s_%d" % (name, self._uid), shape, dt))

    def parity(self, e):
        k = id(e)
        if k not in self._par:
            self._par[k] = e.partition_id() % 2
        return self._par[k]

    def push_scope(self):
        self.scopes.append(ExitStack())

    def pop_scope(self):
        self.barrier()
        self.scopes.pop().close()

    def barrier(self):
        self.nbar += 1
        for e in ENGS:
            o = _Op(e, None, False)
            o.pre = ('bar', self.nbar)
            self.ops[e].append(o)
        self.last_w = {}
        self.readers = {}

    def ps(self, name, shape, dt=F32):
        return self.es.enter_context(self.nc.psum_tensor(name, shape, dt))

    def _add(self, eng, fn, r, w, dma=False):
        o = _Op(eng, fn, dma)
        deps = {}
        for b in r:
            lw = self.last_w.get(b)
            if lw is not None:
                deps[id(lw)] = (lw, True)
        for b in w:
            lw = self.last_w.get(b)
            if lw is not None and id(lw) not in deps:
                deps[id(lw)] = (lw, False)
            rd = self.readers.get(b)
            if rd:
                for x in rd[0].values():
                    if id(x) not in deps:
                        deps[id(x)] = (x, False)
                for x in rd[1]:
                    if id(x) not in deps:
                        deps[id(x)] = (x, False)
        for d, raw in deps.values():
            if d is o:
                continue
            if not d.dma and not dma and d.eng == eng:
                if not raw or eng == 'tensor':
                    continue
            o.deps.append(d)
        for b in r:
            rd = self.readers.setdefault(b, ({}, []))
            if dma:
                rd[1].append(o)
            else:
                rd[0][eng] = o
        for b in w:
            self.last_w[b] = o
            self.readers[b] = ({}, [])
        self.ops[eng].append(o)
        return o

    def add(self, eng, meth, *args, r=(), w=(), **kw):
        return self._add(eng, lambda e: getattr(e, meth)(*args, **kw), r, w)

    def dma(self, out, in_, r=(), w=(), eng='sync', **kw):
        return self._add(eng, lambda e: e.dma_start(out=out, in_=in_, **kw), r, w, dma=True)

    def mm(self, out, lhsT, rhs, start=True, stop=True, r=(), w=(), **kw):
        return self.add('tensor', 'matmul', out, lhsT, rhs, start=start, stop=stop, r=r, w=w, **kw)

    def act(self, out, in_, func, r=(), w=(), **kw):
        return self.add('scalar', 'activation', out, in_, func, r=r, w=w, **kw)

    def emit(self):
        nc = self.nc
        es = self.es
        sem = {e: es.enter_context(nc.semaphore('s_' + e)) for e in ENGS}
        bsem = es.enter_context(nc.semaphore('s_bar'))
        dsems = {}
        for e in ENGS:
            if any(o.dma for o in self.ops[e]):
                dsems[e] = [es.enter_context(nc.semaphore('d_%s_%d' % (e, i))) for i in range(self.ndma)]
        for e in ENGS:
            for o in self.ops[e]:
                for d in o.deps:
                    if not d.dma:
                        d.signal = True
        for e in ENGS:
            c = 0
            j = 0
            for o in self.ops[e]:
                if o.fn is None:
                    continue
                if o.dma:
                    o.dsem = dsems[e][j % self.ndma]
                    o.dval = 16 * (j // self.ndma + 1)
                    o.pre = (o.dsem, o.dval - 16)
                    j += 1
                elif o.signal:
                    c += 1
                    o.sigval = c
        self.sig_counts = {e: sum(1 for o in self.ops[e] if o.signal) for e in ENGS}
        block = es.enter_context(nc.Block())
        all_dma_final = {}
        for e in ENGS:
            for o in self.ops[e]:
                if o.dma:
                    all_dma_final[id(o.dsem)] = (o.dsem, max(o.dval, all_dma_final.get(id(o.dsem), (None, 0))[1]))

        def run(e, engobj):
            waited = {}

            def wait(s, v):
                if v <= 0:
                    return
                if waited.get(id(s), 0) >= v:
                    return
                waited[id(s)] = v
                engobj.wait_ge(s, v)

            mydma = {}
            for o in self.ops[e]:
                if o.fn is None:
                    for s_, v_ in mydma.values():
                        wait(s_, v_)
                    engobj.drain().then_inc(bsem, 1)
                    engobj.wait_ge(bsem, len(ENGS) * o.pre[1])
                    continue
                if o.dma:
                    mydma[id(o.dsem)] = (o.dsem, o.dval)
                for d in o.deps:
                    if d.dma:
                        wait(d.dsem, d.dval)
                    else:
                        wait(sem[d.eng], d.sigval)
                if o.dma:
                    wait(*o.pre)
                    o.fn(engobj).then_inc(o.dsem, 16)
                else:
                    ins = o.fn(engobj)
                    if o.signal:
                        ins.then_inc(sem[e], 1)
            if e == 'sync':
                for s, v in all_dma_final.values():
                    wait(s, v)

        @block.sync
        def _(eng):
            run('sync', eng)

        @block.tensor
        def _(eng):
            run('tensor', eng)

        @block.vector
        def _(eng):
            run('vector', eng)

        @block.scalar
        def _(eng):
            run('scalar', eng)

        @block.gpsimd
        def _(eng):
            run('gpsimd', eng)

        es.close()


D = 2048
T = 4096
NT = 512
S = 4096
EPS = 1e-6
DFF = 5632
NEGM = -30000.0


class Ring:
    def __init__(self, p, name, n, shape, dt):
        self.t = [p.sb("%s%d" % (name, i), shape, dt) for i in range(n)]
        self.k = [(name, i) for i in range(n)]
        self.i = 0

    def next(self):
        j = self.i % len(self.t)
        self.i += 1
        return self.t[j], self.k[j]


class PRing:
    def __init__(self, p, name, n, shape=(128, NT)):
        self.t = [p.ps("%s%d" % (name, i), list(shape)) for i in range(n)]
        self.k = [(name, i) for i in range(n)]
        self.i = 0

    def next(self):
        j = self.i % len(self.t)
        self.i += 1
        return self.t[j], self.k[j]


class SubRing:
    def __init__(self, ring, idx):
        self.t = [ring.t[i] for i in idx]
        self.k = [ring.k[i] for i in idx]
        self.i = 0

    def next(self):
        j = self.i % len(self.t)
        self.i += 1
        return self.t[j], self.k[j]


def wview(t, kc, ncol):
    return t[:, 0:kc * ncol].rearrange("p (k o) -> p k o", o=ncol)


def rms_rstd(p, xs_list, rkeys, sqr, ps_ss, pk, onesf, rstd, n):
    m = len(xs_list)
    for i, xa in enumerate(xs_list):
        sq, sk = sqr.next()
        p.act(sq[:, 0:n], xa, AF.Square, r=rkeys, w=[sk])
        p.mm(ps_ss[:, 0:n], onesf[:], sq[:, 0:n], start=(i == 0), stop=(i == m - 1), r=[sk, 'onesf'], w=[pk])
    rstd_from_ss(p, ps_ss, pk, rstd, n)


def rstd_from_ss(p, ps_ss, pk, rstd, n):
    p.add('vector', 'tensor_scalar', rstd[:, 0:n], ps_ss[:, 0:n], 1.0 / D, EPS, ALU.mult, ALU.add, r=[pk], w=['rstd'])
    p.act(rstd[:, 0:n], rstd[:, 0:n], AF.Sqrt, r=['rstd'], w=['rstd'])
    p.add('vector', 'reciprocal', rstd[:, 0:n], rstd[:, 0:n], r=['rstd'], w=['rstd'])


def emit_A(p, E, l):
    xT = E['xcur']; g = E['g_pre_mix'][l]; w_in = E['w_in'][l]
    uP = E['uP']; qT = E['qT']; kT = E['kT']; v = E['v']; sg = E['sg']
    p.push_scope()
    onesf = p.sb("onesf", [128, 128], F32)
    p.add('vector', 'memset', onesf[:], 1.0, w=['onesf'])
    gt = p.sb("gt", [128, 16], F32)
    p.dma(gt[:], g, w=['gt'])
    xs = p.sb("xs", [128, 16, NT], F32)
    h = p.sb("h", [128, 16, 2048], BF16)
    h2 = p.sb("h2", [128, 16, 2048], BF16)
    sqr = Ring(p, "sq", 2, [128, NT], F32)
    rstd = p.sb("rstd", [128, NT], F32)
    wr = Ring(p, "w", 3, [128, 16 * 256], BF16)
    evf = Ring(p, "evf", 3, [128, NT], F32)
    evb = Ring(p, "evb", 3, [128, NT], BF16)
    ubr = Ring(p, "ub", 2, [128, 8, 64], BF16)
    ps_ss = E['pb'].t[7]
    pm = SubRing(E['pb'], [0, 1, 2, 3])
    w3 = w_in.rearrange("(kc p) o -> p kc o", p=128)
    xT3 = xT.rearrange("(kc p) t -> p kc t", p=128)
    tog = [0]

    def evac(out, in_, r, w):
        tog[0] ^= 1
        if tog[0]:
            p.add('vector', 'tensor_copy', out, in_, r=r, w=w)
        else:
            p.add('scalar', 'copy', out, in_, r=r, w=w)

    ST = 2048
    hbufs = [h, h2]

    def h_phase(st):
        hcur = hbufs[st % 2]
        hk = 'h%d' % (st % 2)
        for sub in range(ST // NT):
            t0 = st * ST + sub * NT
            p.dma(xs[:], xT3[:, :, t0:t0 + NT], w=['xs'])
            rms_rstd(p, [xs[:, kc, :] for kc in range(16)], ['xs'], sqr, ps_ss, E['pb'].k[7], onesf, rstd, NT)
            for kc in range(16):
                p.add('vector', 'scalar_tensor_tensor', hcur[:, kc, sub * NT:(sub + 1) * NT], xs[:, kc, :], gt[:, kc:kc + 1], rstd[:],
                      ALU.mult, ALU.mult, r=['xs', 'gt', 'rstd'], w=[hk])

    h_phase(0)
    for st in range(T // ST):
        h = hbufs[st % 2]
        hkey = 'h%d' % (st % 2)
        for cg in range(32):
            if cg == 6 and st + 1 < T // ST:
                h_phase(st + 1)
            wt, wk = wr.next()
            w4 = wview(wt, 16, 256)
            p.dma(w4, w3[:, :, cg * 256:(cg + 1) * 256], w=[wk], eng='gpsimd')
            if 12 <= cg < 16:
                for ts in range(ST // 128):
                    ps, pk = pm.next()
                    for kc in range(16):
                        p.mm(ps[:, 0:256], h[:, kc, ts * 128:(ts + 1) * 128], w4[:, kc, :], start=(kc == 0),
                             stop=(kc == 15), r=[hkey, wk], w=[pk])
                    eb, ek = evb.next()
                    evac(eb[:, 0:256], ps[:, 0:256], [pk], [ek])
                    r0 = st * ST + ts * 128
                    p.dma(v[r0:r0 + 128, (cg - 12) * 256:(cg - 11) * 256], eb[:, 0:256], r=[ek])
                continue
            for o2 in range(2):
                oc = cg * 2 + o2
                for sub in range(ST // NT):
                    t0 = st * ST + sub * NT
                    tt = t0 // NT
                    ps, pk = pm.next()
                    for kc in range(16):
                        p.mm(ps[:], w4[:, kc, o2 * 128:(o2 + 1) * 128], h[:, kc, sub * NT:(sub + 1) * NT], start=(kc == 0),
                             stop=(kc == 15), r=[hkey, wk], w=[pk])
                    if oc < 8:
                        ub, uk = ubr.next()
                        p.add('vector', 'tensor_copy', ub[:].rearrange("p s c -> p c s"),
                              ps[:].rearrange("p (c s) -> p c s", s=8), r=[pk], w=[uk])
                        for gl in range(8):
                            gg = oc * 8 + gl
                            p.dma(uP[gg].rearrange("(s m) c -> m s c", m=16)[:, :, tt * 64:(tt + 1) * 64],
                                  ub[gl * 16:(gl + 1) * 16, :, :], r=[uk])
                    elif oc < 24:
                        eb, ek = evb.next()
                        evac(eb[:], ps[:], [pk], [ek])
                        dst = qT if oc < 16 else kT
                        c = (oc - 8) % 8
                        p.dma(dst[c * 128:(c + 1) * 128, t0:t0 + NT], eb[:], r=[ek])
                    else:
                        ef, fk = evf.next()
                        p.act(ef[:], ps[:], AF.Sigmoid, r=[pk], w=[fk])
                        c = oc - 32
                        p.dma(sg[c * 128:(c + 1) * 128, t0:t0 + NT], ef[:], r=[fk])
    p.pop_scope()


NG = 32
NH = 4
KK_EXP = [0, -1, -2, -3, -4, -5, -6, -7] + list(range(8)) + [7, 6, 5, 4, 3, 2, 1, 0] + list(range(1, 9)) + [8]
I32 = mybir.dt.int32
TWO_PI = 6.283185307179586
ATT_SCALE = 1.0 / (128 ** 0.5)


def emit_B(p, E, l):
    uP_all = E['uP']; qT = E['qT']; kT = E['kT']; vv = E['v']; yP_all = E['yP']; aT = E['aT']
    c_identf = E['identf']; c_identb = E['identb']; c_tmask = E['tmask']; c_sel = E['selE']
    c_causal = E['causal']; c_pastm = E['pastm']; kk = E['kk']
    p.push_scope()

    def ld(name, src, shape, dt=F32, eng='sync'):
        t = p.sb(name, shape, dt)
        p.dma(t[:], src, w=[name], eng=eng)
        return t

    identf = ld("identf_t", c_identf, [128, 128]); identb = ld("identb_t", c_identb, [128, 128], BF16)
    tmask = ld("tmask_t", c_tmask, [128, 128]); selE = ld("selE_t", c_sel, [16, 16, 128], BF16)
    causal = ld("causal_t", c_causal, [128, 2, 256], BF16); pastm = ld("pastm_t", c_pastm, [128, 16, 16])
    onesb = p.sb("onesb", [128, 128], BF16)
    p.add('vector', 'memset', onesb[:], 1.0, w=['onesb'])
    pb = E['pb']

    PERS = []
    for gh in range(2):
        sp = E['ssm'][l][gh]
        lamre = sp['lamre']; lamim = sp['lamim']; ldt = sp['ldt']; bre = sp['bre']; bim = sp['bim']
        cre = sp['cre']; cim = sp['cim']; dsk = sp['dsk']
        TT = p.sb("TT", [128, NG, 128], BF16); P1 = p.sb("P1", [128, NG, 128], BF16)
        QQre = p.sb("QQre", [64, NG, 128], BF16); QQnim = p.sb("QQnim", [64, NG, 128], BF16)
        C1 = p.sb("C1", [64, 64], F32); C2 = p.sb("C2", [64, 64], F32)
        p.push_scope()
        t_lre = ld("t_lre", lamre, [64, NG]); t_lim = ld("t_lim", lamim, [64, NG]); t_ldt = ld("t_ldt", ldt, [64, NG])
        t_bre = ld("t_bre", bre, [64, NG, 16]); t_bim = ld("t_bim", bim, [64, NG, 16])
        t_cre = ld("t_cre", cre, [64, NG, 16]); t_cim = ld("t_cim", cim, [64, NG, 16])
        t_dsk = ld("t_dsk", dsk, [128, NG]); t_kk = ld("t_kk", kk, [64, 33])
        V = 'vector'
        sh3 = [64, NG, 33]
        dt_ = p.sb("dt_", [64, NG], F32); ar = p.sb("ar", [64, NG], F32); ai = p.sb("ai", [64, NG], F32)
        p.act(dt_[:], t_ldt[:], AF.Exp, r=['t_ldt'], w=['dt_'])
        p.add(V, 'tensor_tensor', ar[:], t_lre[:], dt_[:], ALU.mult, r=['t_lre', 'dt_'], w=['ar'])
        p.add(V, 'tensor_tensor', ai[:], t_lim[:], dt_[:], ALU.mult, r=['t_lim', 'dt_'], w=['ai'])
        th = p.sb("th", sh3, F32); mag = p.sb("mag", sh3, F32)
        pwre = p.sb("pwre", sh3, F32); pwim = p.sb("pwim", sh3, F32)
        ni = p.sb("ni", sh3, I32); nf = p.sb("nf", sh3, F32); yy = p.sb("yy", sh3, F32)
        kkb = t_kk[:].unsqueeze(1).to_broadcast(sh3)
        p.add(V, 'tensor_tensor', mag[:], ar[:].unsqueeze(2).to_broadcast(sh3), kkb, ALU.mult, r=['ar', 't_kk'], w=['mag'])
        p.act(mag[:], mag[:], AF.Exp, r=['mag'], w=['mag'])
        p.add(V, 'tensor_tensor', th[:], ai[:].unsqueeze(2).to_broadcast(sh3), kkb, ALU.mult, r=['ai', 't_kk'], w=['th'])

        def sin_of(dst, dkey, off):
            p.add(V, 'tensor_scalar', yy[:], th[:], 1.0 / TWO_PI, 32.5 + off, ALU.mult, ALU.add, r=['th'], w=['yy'])
            p.add(V, 'tensor_copy', ni[:], yy[:], r=['yy'], w=['ni'])
            p.add(V, 'tensor_copy', nf[:], ni[:], r=['ni'], w=['nf'])
            p.add(V, 'tensor_tensor', yy[:], yy[:], nf[:], ALU.subtract, r=['yy', 'nf'], w=['yy'])
            p.add(V, 'scalar_tensor_tensor', nf[:], yy[:], 0.0, yy[:], ALU.is_lt, ALU.add, r=['yy'], w=['nf'])
            p.add(V, 'tensor_scalar', nf[:], nf[:], TWO_PI, -TWO_PI / 2, ALU.mult, ALU.add, r=['nf'], w=['nf'])
            p.act(nf[:], nf[:], AF.Sin, r=['nf'], w=['nf'])
            p.add(V, 'tensor_tensor', dst[:], nf[:], mag[:], ALU.mult, r=['nf', 'mag'], w=[dkey])

        sin_of(pwim, 'pwim', 0.0)
        sin_of(pwre, 'pwre', 0.25)
        nr = p.sb("nr", [64, NG], F32); dd = p.sb("dd", [64, NG], F32); t0_ = p.sb("t0_", [64, NG], F32)
        qre = p.sb("qre", [64, NG], F32); qim = p.sb("qim", [64, NG], F32)
        p.add(V, 'tensor_scalar', nr[:], pwre[:, :, 9], -1.0, None, ALU.add, r=['pwre'], w=['nr'])
        nim = pwim[:, :, 9]
        p.add(V, 'tensor_tensor', dd[:], t_lre[:], t_lre[:], ALU.mult, r=['t_lre'], w=['dd'])
        p.add(V, 'tensor_tensor', t0_[:], t_lim[:], t_lim[:], ALU.mult, r=['t_lim'], w=['t0_'])
        p.add(V, 'tensor_tensor', dd[:], dd[:], t0_[:], ALU.add, r=['dd', 't0_'], w=['dd'])
        p.add(V, 'reciprocal', dd[:], dd[:], r=['dd'], w=['dd'])
        p.add(V, 'tensor_tensor', qre[:], nr[:], t_lre[:], ALU.mult, r=['nr', 't_lre'], w=['qre'])
        p.add(V, 'tensor_tensor', t0_[:], nim, t_lim[:], ALU.mult, r=['pwim', 't_lim'], w=['t0_'])
        p.add(V, 'tensor_tensor', qre[:], qre[:], t0_[:], ALU.add, r=['qre', 't0_'], w=['qre'])
        p.add(V, 'tensor_tensor', qre[:], qre[:], dd[:], ALU.mult, r=['qre', 'dd'], w=['qre'])
        p.add(V, 'tensor_tensor', qim[:], nim, t_lre[:], ALU.mult, r=['pwim', 't_lre'], w=['qim'])
        p.add(V, 'tensor_tensor', t0_[:], nr[:], t_lim[:], ALU.mult, r=['nr', 't_lim'], w=['t0_'])
        p.add(V, 'tensor_tensor', qim[:], qim[:], t0_[:], ALU.subtract, r=['qim', 't0_'], w=['qim'])
        p.add(V, 'tensor_tensor', qim[:], qim[:], dd[:], ALU.mult, r=['qim', 'dd'], w=['qim'])
        sh16 = [64, NG, 16]
        bbre = p.sb("bbre", sh16, F32); bbim = p.sb("bbim", sh16, F32); t16 = p.sb("t16", sh16, F32)
        qreb = qre[:].unsqueeze(2).to_broadcast(sh16); qimb = qim[:].unsqueeze(2).to_broadcast(sh16)
        p.add(V, 'tensor_tensor', bbre[:], t_bre[:], qreb, ALU.mult, r=['t_bre', 'qre'], w=['bbre'])
        p.add(V, 'tensor_tensor', t16[:], t_bim[:], qimb, ALU.mult, r=['t_bim', 'qim'], w=['t16'])
        p.add(V, 'tensor_tensor', bbre[:], bbre[:], t16[:], ALU.subtract, r=['bbre', 't16'], w=['bbre'])
        p.add(V, 'tensor_tensor', bbim[:], t_bim[:], qreb, ALU.mult, r=['t_bim', 'qre'], w=['bbim'])
        p.add(V, 'tensor_tensor', t16[:], t_bre[:], qimb, ALU.mult, r=['t_bre', 'qim'], w=['t16'])
        p.add(V, 'tensor_tensor', bbim[:], bbim[:], t16[:], ALU.add, r=['bbim', 't16'], w=['bbim'])
        sh4 = [64, NG, 8, 16]
        LL = p.sb("LL", [128, NG, 128], BF16); RR = p.sb("RR", [128, NG, 128], BF16); PP = p.sb("PP", [128, NG, 128], BF16)
        imtmp = p.sb("imtmp", [64, NG, 128], BF16)
        ta = p.sb("ta", sh4, F32); tb = p.sb("tb", sh4, F32)

        def cprod(i0, vre, vim, vkeys, out_re, rekey, out_im, imkey, neg_im):
            pr = pwre[:, :, i0:i0 + 8].unsqueeze(3).to_broadcast(sh4)
            pi_ = pwim[:, :, i0:i0 + 8].unsqueeze(3).to_broadcast(sh4)
            vr = vre[:].unsqueeze(2).to_broadcast(sh4)
            vi = vim[:].unsqueeze(2).to_broadcast(sh4)
            o_re = out_re.rearrange("p g (j m) -> p g j m", m=16)
            o_im = out_im.rearrange("p g (j m) -> p g j m", m=16)
            p.add(V, 'tensor_tensor', ta[:], pr, vr, ALU.mult, r=['pwre'] + vkeys, w=['ta'])
            p.add('gpsimd', 'tensor_tensor', tb[:], pi_, vi, ALU.mult, r=['pwim'] + vkeys, w=['tb'])
            p.add(V, 'tensor_tensor', o_re, ta[:], tb[:], ALU.subtract, r=['ta', 'tb'], w=[rekey])
            p.add(V, 'tensor_tensor', ta[:], pr, vi, ALU.mult, r=['pwre'] + vkeys, w=['ta'])
            p.add('gpsimd', 'tensor_tensor', tb[:], pi_, vr, ALU.mult, r=['pwim'] + vkeys, w=['tb'])
            if neg_im:
                p.add(V, 'scalar_tensor_tensor', o_im, ta[:], -1.0, tb[:], ALU.mult, ALU.subtract, r=['ta', 'tb'], w=[imkey])
            else:
                p.add(V, 'tensor_tensor', o_im, ta[:], tb[:], ALU.add, r=['ta', 'tb'], w=[imkey])

        cprod(0, bbre, bbim, ['bbre', 'bbim'], LL[0:64], 'LL', imtmp[:], 'imtmp', False)
        p.dma(LL[64:128], imtmp[:], r=['imtmp'], w=['LL'])
        cprod(8, t_cre, t_cim, ['t_cre', 't_cim'], RR[0:64], 'RR', imtmp[:], 'imtmp', True)
        p.dma(RR[64:128], imtmp[:], r=['imtmp'], w=['RR'])
        cprod(16, bbre, bbim, ['bbre', 'bbim'], PP[0:64], 'PP', imtmp[:], 'imtmp', False)
        p.dma(PP[64:128], imtmp[:], r=['imtmp'], w=['PP'])
        cprod(24, t_cre, t_cim, ['t_cre', 't_cim'], QQre[:], 'QQre', QQnim[:], 'QQnim', True)
        p.add(V, 'tensor_copy', C1[:, 0:32], pwre[:, :, 32], r=['pwre'], w=['C1'])
        p.add(V, 'tensor_copy', C1[:, 32:64], pwre[:, :, 32], r=['pwre'], w=['C1'])
        p.add(V, 'tensor_scalar', C2[:, 0:32], pwim[:, :, 32], -1.0, None, ALU.mult, r=['pwim'], w=['C2'])
        p.add(V, 'tensor_copy', C2[:, 32:64], pwim[:, :, 32], r=['pwim'], w=['C2'])
        tfr = Ring(p, "tfr", 2, [128, 4, 128], F32)
        for g0 in range(0, NG, 4):
            ps, pk = pb.next()
            for gl in range(4):
                p.mm(ps[:, gl * 128:(gl + 1) * 128], LL[:, g0 + gl, :], RR[:, g0 + gl, :], r=['LL', 'RR'], w=[pk])
            tf, tk = tfr.next()
            p.add(V, 'tensor_tensor', tf[:], ps[:].rearrange("p (g c) -> p g c", c=128),
                  tmask[:].unsqueeze(1).to_broadcast([128, 4, 128]), ALU.mult, r=[pk, 'tmask_t'], w=[tk])
            for gl in range(4):
                p.add(V, 'scalar_tensor_tensor', TT[:, g0 + gl, :], identf[:], t_dsk[:, g0 + gl:g0 + gl + 1], tf[:, gl, :],
                      ALU.mult, ALU.add, r=['identf_t', 't_dsk', tk], w=['TT'])
            ps2, pk2 = pb.next()
            for gl in range(4):
                p.mm(ps2[:, gl * 128:(gl + 1) * 128], PP[:, g0 + gl, :], identb[:], r=['PP', 'identb_t'], w=[pk2])
            p.add('scalar', 'copy', P1[:, g0:g0 + 4, :], ps2[:].rearrange("p (g c) -> p g c", c=128), r=[pk2], w=['P1'])

        p.pop_scope()
        PERS.append((TT, P1, QQre, QQnim, C1, C2))
    SEGC = 64
    U = p.sb("U", [128, NG, 512], BF16)
    hist = p.sb("hist", [64, SEGC + 1, 96], F32)
    Gst = p.sb("Gst", [64, SEGC, 64], F32)
    Hre = p.sb("Hre", [64, NG, SEGC], BF16); Him = p.sb("Him", [64, NG, SEGC], BF16)
    t1 = p.sb("t1", [64, 64], F32); t2 = p.sb("t2", [64, 64], F32); t3 = p.sb("t3", [64, 64], F32)
    ybr = Ring(p, "yb", 2, [128, 4, SEGC], F32)
    pbs = SubRing(pb, [6, 7])

    def ssm_main():
        for gh in range(2):
            TT, P1, QQre, QQnim, C1, C2 = PERS[gh]
            uP = uP_all[gh * NG:(gh + 1) * NG]; yP = yP_all[gh * NG:(gh + 1) * NG]
            for q4 in range(4):
                p.dma(U[:, q4 * 8:(q4 + 1) * 8, :], uP[q4 * 8:(q4 + 1) * 8].rearrange("g p c -> p g c"), w=['U'])
            p.add(V, 'memset', hist[:, 0, :], 0.0, w=['histA', 'histB'])
            yield
            for seg in range(512 // SEGC):
                cs = slice(seg * SEGC, (seg + 1) * SEGC)
                if seg > 0:
                    p.add(V, 'tensor_copy', hist[:, 0, :], hist[:, SEGC, :], r=['histA', 'histB'], w=['histA', 'histB'])
                for g0 in range(0, NG, 4):
                    for ri in range(2):
                        ps, pk = pbs.next()
                        for gl in range(4):
                            p.mm(ps[0:64, gl * SEGC:(gl + 1) * SEGC], P1[:, g0 + gl, ri * 64:(ri + 1) * 64], U[:, g0 + gl, cs],
                                 r=['P1', 'U'], w=[pk])
                        dst = Gst[:, :, ri * 32 + g0:ri * 32 + g0 + 4].rearrange("p c g -> p g c")
                        src = ps[0:64, 0:4 * SEGC].rearrange("p (g c) -> p g c", c=SEGC)
                        if ri == 0:
                            p.add(V, 'tensor_copy', dst, src, r=[pk], w=['Gst'])
                        else:
                            p.add('scalar', 'copy', dst, src, r=[pk], w=['Gst'])
                        yield
                for c in range(SEGC):
                    p.add(V, 'tensor_tensor', t1[:], hist[:, c, 0:64], C1[:], ALU.mult, r=['histA', 'C1'], w=['t1'])
                    p.add(V, 'tensor_tensor', t2[:], hist[:, c, 32:96], C2[:], ALU.mult, r=['histA', 'histB', 'C2'], w=['t2'])
                    p.add(V, 'tensor_tensor', t3[:], t1[:], t2[:], ALU.add, r=['t1', 't2'], w=['t3'])
                    p.add(V, 'tensor_tensor', hist[:, c + 1, 0:64], t3[:], Gst[:, c, :], ALU.add, r=['t3', 'Gst'], w=['histA'])
                    p.add(V, 'tensor_tensor', hist[:, c + 1, 64:96], t3[:, 0:32], Gst[:, c, 0:32], ALU.add,
                          r=['t3', 'Gst'], w=['histB'])
                    yield
                p.add(V, 'tensor_copy', Hre[:], hist[:, 0:SEGC, 0:32].rearrange("p c g -> p g c"), r=['histA'], w=['Hre'])
                p.add('gpsimd', 'tensor_copy', Him[:], hist[:, 0:SEGC, 32:64].rearrange("p c g -> p g c"), r=['histA'], w=['Him'])
                yield
                for g0 in range(0, NG, 4):
                    ps, pk = pbs.next()
                    for gl in range(4):
                        g = g0 + gl
                        o = ps[:, gl * SEGC:(gl + 1) * SEGC]
                        p.mm(o, TT[:, g, :], U[:, g, cs], start=True, stop=False, r=['TT', 'U'], w=[pk])
                        p.mm(o, QQre[:, g, :], Hre[:, g, :], start=False, stop=False, r=['QQre', 'Hre'], w=[pk])
                        p.mm(o, QQnim[:, g, :], Him[:, g, :], start=False, stop=True, r=['QQnim', 'Him'], w=[pk])
                    yb, yk = ybr.next()
                    p.add('scalar', 'copy', yb[:], ps[:, 0:4 * SEGC].rearrange("p (g c) -> p g c", c=SEGC), r=[pk], w=[yk])
                    p.dma(yP[g0:g0 + 4, :, cs].rearrange("g p c -> p g c"), yb[:], r=[yk])
                    yield

    ssm_it = ssm_main()
    Kh = p.sb("Kh", [128, S], BF16); Qh = p.sb("Qh", [128, S], BF16); Vh = p.sb("Vh", [128, 32, 128], BF16)
    ksum = p.sb("ksum", [128, 16], F32); kmh = p.sb("kmh", [128, 16], BF16); kml = p.sb("kml", [128, 16], BF16)
    kmhf = p.sb("kmhf", [128, 16], F32)
    maskT = p.sb("maskT", [16, S], BF16)
    gmr = Ring(p, "gm", 2, [128, 16], F32); mxr = Ring(p, "mx", 2, [128, 8], F32); thr_r = Ring(p, "thr", 2, [128, 1], F32)
    bir = Ring(p, "bi", 2, [128, 16], F32)
    ptr = Ring(p, "pt", 4, [128, 512], BF16)
    rlr = Ring(p, "rl", 2, [128, 512], F32); obr = Ring(p, "ob", 2, [128, 512], BF16)
    psr = SubRing(pb, [0, 1, 2]); por = SubRing(pb, [3]); plr = SubRing(pb, [4]); pgr = SubRing(pb, [5])
    for hh in range(8):
        hs = slice(hh * 128, (hh + 1) * 128)
        p.dma(Kh[:], kT[hs, :], w=['Kh'])
        p.dma(Qh[:], qT[hs, :], w=['Qh'])
        p.dma(Vh[:], vv[:, hs].rearrange("(kt p) d -> p kt d", p=128), w=['Vh'])
        p.add(V, 'tensor_reduce', ksum[:], Kh[:].rearrange("p (j l) -> p j l", l=256), AX.X, ALU.add, r=['Kh'], w=['ksum'])
        p.add(V, 'tensor_scalar', ksum[:], ksum[:], 1.0 / 256, None, ALU.mult, r=['ksum'], w=['ksum'])
        p.add(V, 'tensor_copy', kmh[:], ksum[:], r=['ksum'], w=['kmh'])
        p.add(V, 'tensor_copy', kmhf[:], kmh[:], r=['kmh'], w=['kmhf'])
        p.add(V, 'tensor_tensor', kmhf[:], ksum[:], kmhf[:], ALU.subtract, r=['ksum', 'kmhf'], w=['kmhf'])
        p.add(V, 'tensor_copy', kml[:], kmhf[:], r=['kmhf'], w=['kml'])
        for qs in range(32):
            n = qs // 2
            ps, pk = pgr.next()
            p.mm(ps[:, 0:16], Qh[:, qs * 128:(qs + 1) * 128], kmh[:], start=True, stop=False, r=['Qh', 'kmh'], w=[pk])
            p.mm(ps[:, 0:16], Qh[:, qs * 128:(qs + 1) * 128], kml[:], start=False, stop=True, r=['Qh', 'kml'], w=[pk])
            gm, gk = gmr.next(); mx, mk = mxr.next(); th_, tk_ = thr_r.next(); bi, bk = bir.next()
            p.add(V, 'tensor_tensor', gm[:], ps[:, 0:16], pastm[:, n, :], ALU.add, r=[pk, 'pastm_t'], w=[gk])
            p.add(V, 'max', mx[:], gm[:], r=[gk], w=[mk])
            p.add(V, 'tensor_scalar_max', th_[:], mx[:, 2:3], -10000.0, r=[mk], w=[tk_])
            p.add(V, 'tensor_scalar', bi[:], gm[:], th_[:, 0:1], -NEGM, ALU.is_ge, ALU.mult, r=[gk, tk_], w=[bk])
            p.add(V, 'tensor_scalar', bi[:], bi[:], NEGM, None, ALU.add, r=[bk], w=[bk])
            p.add(V, 'memset', bi[:, n:n + 1], 0.0, w=[bk])
            ps2, pk2 = psr.next()
            p.mm(ps2[0:16, 0:128], bi[:], identf[:], r=[bk, 'identf_t'], w=[pk2])
            p.add('scalar', 'copy', maskT[:, qs * 128:(qs + 1) * 128], ps2[0:16, 0:128], r=[pk2], w=['maskT'])
            next(ssm_it, None)
        for np_ in range(8):
            n0 = 2 * np_
            qsl = slice(n0 * 256, (n0 + 2) * 256)
            po, pok = por.next()
            pl, plk = plr.next()
            nkc = 2 * n0 + 2
            tot = nkc + 2
            pend = []

            def stage1(kt):
                j = kt // 2
                cs_ = slice(0, 512) if kt < nkc else slice(256, 512)
                qcols = slice(n0 * 256, (n0 + 2) * 256) if kt < nkc else slice((n0 + 1) * 256, (n0 + 2) * 256)
                ps, pk = psr.next()
                p.mm(ps[:, cs_], Kh[:, kt * 128:(kt + 1) * 128], Qh[:, qcols], start=True, stop=False, r=['Kh', 'Qh'], w=[pk])
                p.mm(ps[:, cs_], selE[:, j, :], maskT[:, qcols], start=False, stop=(j < n0), r=['selE_t', 'maskT'], w=[pk])
                dcol = slice(0, 256) if j == n0 else slice(256, 512)
                if j >= n0:
                    p.mm(ps[:, dcol], identb[:], causal[:, kt % 2, :], start=False, stop=True, r=['identb_t', 'causal_t'], w=[pk])
                pt, ptk = ptr.next()
                p.act(pt[:, cs_], ps[:, cs_], AF.Exp, scale=ATT_SCALE, r=[pk], w=[ptk])
                pend.append((kt, cs_, pt, ptk))

            def stage2():
                kt, cs_, pt, ptk = pend.pop(0)
                p.mm(po[:, cs_], Vh[:, kt, :], pt[:, cs_], start=(kt == 0), stop=(kt == tot - 1), r=['Vh', ptk], w=[pok])
                p.mm(pl[:, cs_], onesb[:], pt[:, cs_], start=(kt == 0), stop=(kt == tot - 1), r=['onesb', ptk], w=[plk])

            for kt in range(tot):
                stage1(kt)
                next(ssm_it, None)
                if len(pend) > 2:
                    stage2()
            while pend:
                stage2()
            rl, rk = rlr.next(); ob, ok_ = obr.next()
            p.add(V, 'reciprocal', rl[:], pl[:], r=[plk], w=[rk])
            p.add(V, 'tensor_tensor', ob[:], po[:], rl[:], ALU.mult, r=[pok, rk], w=[ok_])
            p.dma(aT[hs, qsl], ob[:], r=[ok_])
    for _ in ssm_it:
        pass
    p.pop_scope()


def _bf(a):
    return np.ascontiguousarray(a).astype(ml_dtypes.bfloat16)


def consts_B():
    sm = np.arange(128) // 16
    tmask = (sm[None, :] >= sm[:, None]).astype(np.float32)
    selE = np.zeros((16, 16, 128), np.float32)
    for j in range(16):
        selE[j, j, :] = 1.0
    k = np.arange(128)[:, None, None]
    a = np.arange(2)[None, :, None]
    q = np.arange(256)[None, None, :]
    causal = np.where(a * 128 + k <= q, 0.0, NEGM).astype(np.float32)
    n = np.arange(16)[None, :, None]
    j = np.arange(16)[None, None, :]
    pastm = np.broadcast_to(np.where(j < n, 0.0, NEGM), (128, 16, 16)).astype(np.float32)
    return {
        "identf": np.eye(128, dtype=np.float32), "identb": _bf(np.eye(128, dtype=np.float32)),
        "tmask": tmask, "selE": _bf(selE), "causal": _bf(causal), "pastm": np.ascontiguousarray(pastm),
        "kk": np.ascontiguousarray(np.tile(np.array(KK_EXP, np.float32), (64, 1))),
    }


def ssm_params_B(inp, l, gh):
    gs = slice(gh * NG, (gh + 1) * NG)
    c = np.ascontiguousarray
    return {
        "lamre": c(inp['lam_re'][l][gs].T), "lamim": c(inp['lam_im'][l][gs].T),
        "ldt": c(np.broadcast_to(inp['log_dt'][l][gs][None, :], (64, NG))),
        "bre": c(inp['b_re'][l][gs].transpose(1, 0, 2)), "bim": c(inp['b_im'][l][gs].transpose(1, 0, 2)),
        "cre": c(inp['c_re'][l][gs].transpose(2, 0, 1)), "cim": c(inp['c_im'][l][gs].transpose(2, 0, 1)),
        "dsk": c(np.tile(inp['d_skip'][l].reshape(64, 16)[gs].T, (8, 1))),
    }


def _glay(gv):
    return np.ascontiguousarray(np.asarray(gv, np.float32).reshape(16, 128).T)


def emit_C(p, E, l):
    yP = E['yP']; aT = E['aT']; sg = E['sg']; xT = E['xcur']
    w_glu = E['w_glu'][l]; w_us = E['w_up_ssm'][l]; w_ua = E['w_up_attn'][l]; w_out = E['w_out'][l]
    g1 = E['g_post_mix'][l]; g2 = E['g_pre_ffn'][l]
    xm_o = E['xm']; hf_o = E['hfx'][:, 2:T + 2]
    p.push_scope()
    V = 'vector'; G = 'gpsimd'
    onesf = p.sb("onesf", [128, 128], F32)
    p.add(V, 'memset', onesf[:], 1.0, w=['onesf'])
    g1t = p.sb("g1t", [128, 16], F32); g2t = p.sb("g2t", [128, 16], F32)
    p.dma(g1t[:], g1, w=['g1t']); p.dma(g2t[:], g2, w=['g2t'])
    ybr = Ring(p, "ybuf", 2, [128, 8, 64], F32)
    yall = p.sb("yall", [128, 8, NT], F32); yb = p.sb("ybb", [128, 8, NT], BF16); y2 = p.sb("y2", [128, 8, NT], BF16)
    at = p.sb("at", [128, 8, NT], BF16); mb = p.sb("mb", [128, 16, NT], BF16)
    mo = p.sb("mo", [128, 16, NT], F32); xs = p.sb("xs", [128, 16, NT], F32); hfb = p.sb("hfb", [128, 16, NT], BF16)
    tr = Ring(p, "tmp", 4, [128, NT], F32)
    sgr = Ring(p, "sgt", 4, [128, NT], F32)
    sqr = Ring(p, "sq", 2, [128, NT], F32)
    rstd = p.sb("rstd", [128, NT], F32)
    wr = Ring(p, "w", 4, [128, 16 * 256], BF16)
    ps_ss = E['pb'].t[7]
    pm = SubRing(E['pb'], [0, 1, 2, 3, 4, 5])
    xT3 = xT.rearrange("(kc p) t -> p kc t", p=128)
    aT3 = aT.rearrange("(kc p) t -> p kc t", p=128)

    def wload(wd, kc, cg):
        wt, wk = wr.next()
        w4 = wview(wt, kc, 256)
        p.dma(w4, wd.rearrange("(kc p) o -> p kc o", p=128)[:, :, cg * 256:(cg + 1) * 256], w=[wk], eng=G)
        return w4, wk

    def ph_gelu(tt):
        t0 = tt * NT
        ts = slice(t0, t0 + NT)
        for kc in range(8):
            ybf, yk = ybr.next()
            for gl in range(8):
                p.dma(ybf[gl * 16:(gl + 1) * 16, :, :],
                      yP[kc * 8 + gl].rearrange("(t m) c -> m t c", m=16)[:, :, tt * 64:(tt + 1) * 64], w=[yk])
            xv = ybf[:].rearrange("p t c -> p c t")
            a, ak = tr.next()
            av = a[:].rearrange("p (c t) -> p c t", t=8)
            p.add(V, 'tensor_tensor', av, xv, xv, ALU.mult, r=[yk], w=[ak])
            p.add(V, 'tensor_scalar', a[:], a[:], 0.044715, 1.0, ALU.mult, ALU.add, r=[ak], w=[ak])
            p.add(V, 'tensor_tensor', av, av, xv, ALU.mult, r=[ak, yk], w=[ak])
            p.act(a[:], a[:], AF.Sigmoid, scale=1.5957691216057308, r=[ak], w=[ak])
            p.add(V, 'tensor_tensor', yall[:, kc, :].rearrange("p (c t) -> p c t", t=8), av, xv, ALU.mult, r=[ak, yk], w=['yall'])
            p.add(V, 'tensor_copy', yb[:, kc, :], yall[:, kc, :], r=['yall'], w=['yb'])

    def ph_mix(tt):
        t0 = tt * NT
        ts = slice(t0, t0 + NT)
        for cg in range(4):
            w4, wk = wload(w_glu, 8, cg)
            for o2 in range(2):
                oc = cg * 2 + o2
                ps, pk = pm.next()
                for kc in range(8):
                    p.mm(ps[:], w4[:, kc, o2 * 128:(o2 + 1) * 128], yb[:, kc, :], start=(kc == 0), stop=(kc == 7), r=[wk, 'yb'], w=[pk])
                s_, sk = tr.next()
                p.act(s_[:], ps[:], AF.Sigmoid, r=[pk], w=[sk])
                p.add(V, 'tensor_tensor', y2[:, oc, :], yall[:, oc, :], s_[:], ALU.mult, r=['yall', sk], w=['y2'])
        p.dma(at[:], aT3[:, :, ts], w=['at'])
        for cg in range(8):
            w1, k1 = wload(w_us, 8, cg)
            w2, k2 = wload(w_ua, 8, cg)
            for o2 in range(2):
                oc = cg * 2 + o2
                osl = slice(o2 * 128, (o2 + 1) * 128)
                sa, sak = sgr.next(); sb_, sbk = sgr.next()
                p.dma(sa[:], sg[oc * 128:(oc + 1) * 128, ts], w=[sak])
                p.dma(sb_[:], sg[2048 + oc * 128:2048 + (oc + 1) * 128, ts], w=[sbk])
                ps1, pk1 = pm.next(); ps2, pk2 = pm.next()
                for kc in range(8):
                    p.mm(ps1[:], w1[:, kc, osl], y2[:, kc, :], start=(kc == 0), stop=(kc == 7), r=[k1, 'y2'], w=[pk1])
                for kc in range(8):
                    p.mm(ps2[:], w2[:, kc, osl], at[:, kc, :], start=(kc == 0), stop=(kc == 7), r=[k2, 'at'], w=[pk2])
                m1, mk1 = tr.next(); m2, mk2 = tr.next()
                p.add(V, 'tensor_tensor', m1[:], ps1[:], sa[:], ALU.mult, r=[pk1, sak], w=[mk1])
                p.add(V, 'tensor_tensor', m2[:], ps2[:], sb_[:], ALU.mult, r=[pk2, sbk], w=[mk2])
                p.add(V, 'tensor_tensor', mb[:, oc, :], m1[:], m2[:], ALU.add, r=[mk1, mk2], w=['mb'])

    def ph_out(tt):
        t0 = tt * NT
        ts = slice(t0, t0 + NT)
        p.dma(xs[:], xT3[:, :, ts], w=['xs'])
        for cg in range(8):
            w4, wk = wload(w_out, 16, cg)
            for o2 in range(2):
                oc = cg * 2 + o2
                ps, pk = pm.next()
                for kc in range(16):
                    p.mm(ps[:], w4[:, kc, o2 * 128:(o2 + 1) * 128], mb[:, kc, :], start=(kc == 0), stop=(kc == 15), r=[wk, 'mb'], w=[pk])
                p.act(mo[:, oc, :], ps[:], AF.Copy, r=[pk], w=['mo'])
        rms_rstd(p, [mo[:, kc, :] for kc in range(16)], ['mo'], sqr, ps_ss, E['pb'].k[7], onesf, rstd, NT)
        for oc in range(16):
            p.add(V, 'scalar_tensor_tensor', mo[:, oc, :], mo[:, oc, :], g1t[:, oc:oc + 1], rstd[:], ALU.mult, ALU.mult,
                  r=['mo', 'g1t', 'rstd'], w=['mo'])
            p.add(V, 'tensor_tensor', mo[:, oc, :], mo[:, oc, :], xs[:, oc, :], ALU.add, r=['mo', 'xs'], w=['mo'])
        p.dma(xm_o.rearrange("(kc p) t -> p kc t", p=128)[:, :, ts], mo[:], r=['mo'])
        rms_rstd(p, [mo[:, kc, :] for kc in range(16)], ['mo'], sqr, ps_ss, E['pb'].k[7], onesf, rstd, NT)
        for oc in range(16):
            p.add(V, 'scalar_tensor_tensor', hfb[:, oc, :], mo[:, oc, :], g2t[:, oc:oc + 1], rstd[:], ALU.mult, ALU.mult,
                  r=['mo', 'g2t', 'rstd'], w=['hfb'])
        p.dma(hf_o.rearrange("(kc p) t -> p kc t", p=128)[:, :, ts], hfb[:], r=['hfb'])

    NTI = T // NT
    ph_gelu(0)
    for tt in range(NTI):
        ph_mix(tt)
        if tt + 1 < NTI:
            ph_gelu(tt + 1)
        ph_out(tt)
    p.pop_scope()


def emit_D(p, E, l):
    hfx = E['hfx']; xm = E['xm']; w_up = E['w_ffn_up'][l]; w_dn = E['w_ffn_down'][l]
    cw = E['cw'][l]; cb = E['cb'][l]; g3 = E['g_post_ffn'][l]
    xo = E['xout'] if l == 1 else E['xnext']
    TS = 1024
    NH_ = TS // NT
    p.push_scope()
    V = 'vector'; G = 'gpsimd'
    onesf = p.sb("onesf", [128, 128], F32)
    p.add(V, 'memset', onesf[:], 1.0, w=['onesf'])
    g3t = p.sb("g3t", [128, 16], F32); cwt = p.sb("cwt", [128, 88, 3], F32); cbt = p.sb("cbt", [128, 88], F32)
    p.dma(g3t[:], g3, w=['g3t']); p.dma(cwt[:], cw, w=['cwt']); p.dma(cbt[:], cb, w=['cbt'])
    ztail = p.sb("ztail", [128, 88, 2], F32)
    p.add(V, 'memset', ztail[:], 0.0, w=['ztail'])
    actb = p.sb("actb", [128, 44, TS], BF16)
    ps_ss = E['pb'].t[7]
    pm = SubRing(E['pb'], [0, 1, 2, 3, 4, 5, 6])
    hfx3 = hfx.rearrange("(kc p) t -> p kc t", p=128)
    wup3 = w_up.rearrange("(kc p) o -> p kc o", p=128)
    wdn3 = w_dn.rearrange("(kc p) o -> p kc o", p=128)
    for st in range(T // TS):
        s0 = st * TS
        p.push_scope()
        hx = p.sb("hx", [128, 16, TS + 2], BF16)
        zr = Ring(p, "z", 4, [128, NT + 2], F32)
        cr = Ring(p, "cv", 4, [128, NT], F32)
        wr = Ring(p, "w", 3, [128, 2 * 16 * 256], BF16)
        p.dma(hx[:], hfx3[:, :, s0:s0 + TS + 2], w=['hx'])
        for jj in range(22):
            wt, wk = wr.next()
            w5 = wt[:].rearrange("p (a k o) -> p a k o", a=2, o=256)
            p.dma(w5[:, 0], wup3[:, :, jj * 256:(jj + 1) * 256], w=[wk], eng=G)
            p.dma(w5[:, 1], wup3[:, :, DFF + jj * 256:DFF + (jj + 1) * 256], w=[wk], eng=G)
            for o2 in range(2):
                j = jj * 2 + o2
                osl = slice(o2 * 128, (o2 + 1) * 128)
                for hb in range(NH_):
                    cres = []
                    for which in range(2):
                        ch = which * 44 + j
                        ps, pk = pm.next()
                        for kc in range(16):
                            p.mm(ps[:], w5[:, which, kc, osl], hx[:, kc, 2 + hb * NT:2 + (hb + 1) * NT], start=(kc == 0),
                                 stop=(kc == 15), r=[wk, 'hx'], w=[pk])
                        zt, zk = zr.next()
                        p.act(zt[:, 2:NT + 2], ps[:], AF.Copy, r=[pk], w=[zk])
                        p.add('scalar', 'copy', zt[:, 0:2], ztail[:, ch, :], r=['ztail'], w=[zk])
                        p.add('scalar', 'copy', ztail[:, ch, :], zt[:, NT:NT + 2], r=[zk], w=['ztail'])
                        c1, ck = cr.next()
                        p.add(V, 'tensor_scalar', c1[:], zt[:, 2:NT + 2], cwt[:, ch, 2:3], cbt[:, ch:ch + 1], ALU.mult, ALU.add,
                              r=[zk, 'cwt', 'cbt'], w=[ck])
                        p.add(V, 'scalar_tensor_tensor', c1[:], zt[:, 1:NT + 1], cwt[:, ch, 1:2], c1[:], ALU.mult, ALU.add,
                              r=[zk, 'cwt', ck], w=[ck])
                        p.add(V, 'scalar_tensor_tensor', c1[:], zt[:, 0:NT], cwt[:, ch, 0:1], c1[:], ALU.mult, ALU.add,
                              r=[zk, 'cwt', ck], w=[ck])
                        cres.append((c1, ck))
                    (ca, cak), (cv_, cvk) = cres
                    p.act(ca[:], ca[:], AF.Silu, r=[cak], w=[cak])
                    p.add(V, 'tensor_tensor', actb[:, j, hb * NT:(hb + 1) * NT], ca[:], cv_[:], ALU.mult, r=[cak, cvk], w=['actb'])
        p.pop_scope()
        p.push_scope()
        f = p.sb("f", [128, 16, TS], F32)
        xr = Ring(p, "xm", 2, [128, NT], F32)
        orr = Ring(p, "or", 2, [128, NT], F32)
        sqr = Ring(p, "sq", 2, [128, NT], F32)
        rstd = p.sb("rstd", [128, NT], F32)
        wr = Ring(p, "wd", 3, [128, 44 * 128], BF16)
        for oc in range(16):
            wt, wk = wr.next()
            w4 = wview(wt, 44, 128)
            p.dma(w4, wdn3[:, :, oc * 128:(oc + 1) * 128], w=[wk], eng=G)
            for hb in range(NH_):
                ps, pk = pm.next()
                for j in range(44):
                    p.mm(ps[:], w4[:, j, :], actb[:, j, hb * NT:(hb + 1) * NT], start=(j == 0), stop=(j == 43), r=[wk, 'actb'], w=[pk])
                p.act(f[:, oc, hb * NT:(hb + 1) * NT], ps[:], AF.Copy, r=[pk], w=['f'])
        for hb in range(NH_):
            hs_ = slice(hb * NT, (hb + 1) * NT)
            ts = slice(s0 + hb * NT, s0 + (hb + 1) * NT)
            rms_rstd(p, [f[:, kc, hs_] for kc in range(16)], ['f'], sqr, ps_ss, E['pb'].k[7], onesf, rstd, NT)
            for oc in range(16):
                xt, xk = xr.next(); ot, ok_ = orr.next()
                p.dma(xt[:], xm[oc * 128:(oc + 1) * 128, ts], w=[xk])
                p.add(V, 'scalar_tensor_tensor', ot[:], f[:, oc, hs_], g3t[:, oc:oc + 1], rstd[:], ALU.mult, ALU.mult,
                      r=['f', 'g3t', 'rstd'], w=[ok_])
                p.add(V, 'tensor_tensor', ot[:], ot[:], xt[:], ALU.add, r=[ok_, xk], w=[ok_])
                p.dma(xo[oc * 128:(oc + 1) * 128, ts], ot[:], r=[ok_])
        p.pop_scope()
    p.pop_scope()


def build_fused():
    nc = bass.Bass("TRN2", target_bir_lowering=False)
    E = {}

    def din(name, shape, dt=F32):
        return nc.dram_tensor(name, shape, dt, kind="ExternalInput").ap()

    def dscr(name, shape, dt=F32):
        return nc.dram_tensor(name, shape, dt, kind="Internal").ap()

    E['xin'] = din("xT", [D, T])
    for nm in ['g_pre_mix', 'g_post_mix', 'g_pre_ffn', 'g_post_ffn']:
        E[nm] = din(nm, [2, 128, 16])
    E['w_in'] = din("w_in", [2, D, 8192]); E['w_glu'] = din("w_glu", [2, 1024, 1024])
    E['w_up_ssm'] = din("w_up_ssm", [2, 1024, D]); E['w_up_attn'] = din("w_up_attn", [2, 1024, D])
    E['w_out'] = din("w_out", [2, D, D]); E['w_ffn_up'] = din("w_ffn_up", [2, D, 2 * DFF]); E['w_ffn_down'] = din("w_ffn_down", [2, DFF, D])
    E['cw'] = din("cw", [2, 128, 88, 3]); E['cb'] = din("cb", [2, 128, 88])
    E['ssm'] = [[{k: din("%s_%d_%d" % (k, l, gh), shp) for k, shp in
                  [('lamre', [64, NG]), ('lamim', [64, NG]), ('ldt', [64, NG]), ('bre', [64, NG, 16]), ('bim', [64, NG, 16]),
                   ('cre', [64, NG, 16]), ('cim', [64, NG, 16]), ('dsk', [128, NG])]} for gh in range(2)] for l in range(2)]
    E['kk'] = din("kk", [64, 33]); E['identf'] = din("identf", [128, 128]); E['identb'] = din("identb", [128, 128], BF16)
    E['tmask'] = din("tmask", [128, 128]); E['selE'] = din("selE", [16, 16, 128], BF16)
    E['causal'] = din("causal", [128, 2, 256], BF16); E['pastm'] = din("pastm", [128, 16, 16])
    E['xout'] = nc.dram_tensor("xo", [D, T], F32, kind="ExternalOutput").ap()
    E['xnext'] = dscr("xnext", [D, T])
    E['uP'] = dscr("uP", [64, 128, 512], BF16); E['qT'] = dscr("qT", [1024, T], BF16); E['kT'] = dscr("kT", [1024, T], BF16)
    E['v'] = dscr("v", [T, 1024], BF16); E['sg'] = dscr("sg", [4096, T]); E['yP'] = dscr("yP", [64, 128, 512])
    E['aT'] = dscr("aT", [1024, T], BF16); E['xm'] = dscr("xm", [D, T]); E['hfx'] = dscr("hfx", [D, T + 2], BF16)
    p = Prog(nc)
    E['pb'] = PRing(p, "pb", 8)
    zt = p.sb("zt", [128, 16, 2], BF16)
    p.add('vector', 'memset', zt[:], 0.0, w=['zt'])
    p.dma(E['hfx'].rearrange("(kc p) t -> p kc t", p=128)[:, :, 0:2], zt[:], r=['zt'])
    for l in range(2):
        E['xcur'] = E['xin'] if l == 0 else E['xnext']
        emit_A(p, E, l)
        emit_B(p, E, l)
        emit_C(p, E, l)
        emit_D(p, E, l)
    p.emit()
    return nc


_NC = []


def kernel(**inp):
    c_ = np.ascontiguousarray
    f32 = lambda a: c_(np.asarray(a, np.float32))
    if not _NC:
        _NC.append(build_fused())
    nc = _NC[0]
    x = f32(inp['x'])
    shared = dict(consts_B())
    for nm in ['g_pre_mix', 'g_post_mix', 'g_pre_ffn', 'g_post_ffn']:
        shared[nm] = c_(np.stack([_glay(inp[nm][l]) for l in range(2)]))
    shared['w_in'] = f32(inp['w_in']); shared['w_glu'] = f32(inp['w_glu'])
    shared['w_up_ssm'] = f32(inp['w_up_ssm']); shared['w_up_attn'] = f32(inp['w_up_attn'])
    shared['w_out'] = f32(inp['w_out']); shared['w_ffn_up'] = f32(inp['w_ffn_up']); shared['w_ffn_down'] = f32(inp['w_ffn_down'])
    shared['cw'] = c_(np.stack([f32(inp['conv_w'][l]).reshape(3, 88, 128).transpose(2, 1, 0) for l in range(2)]))
    shared['cb'] = c_(np.stack([f32(inp['conv_b'][l]).reshape(88, 128).T for l in range(2)]))
    for l in range(2):
        for gh in range(2):
            for k, v in ssm_params_B(inp, l, gh).items():
                shared["%s_%d_%d" % (k, l, gh)] = v
    nb = x.shape[0]
    maps = []
    for b in range(nb):
        m = dict(shared)
        m['xT'] = c_(x[b].T)
        maps.append(m)
    res = run_bass_kernel_spmd(nc, maps, core_ids=list(range(nb))).results
    return c_(np.stack([res[b]['xo'].T for b in range(nb)]))
```
